# Optimizing a Trainium2 kernel written in Bass

```python
import math
import jax, jax.numpy as jnp
from jax import lax
import numpy as np

D_MODEL = 1024
BATCH = 16
SEQ = 4096
DEPTH = 4

MEM_LEN = 256
N_EVEN = (DEPTH + 1) // 2
N_ODD = DEPTH // 2
EPS = 1e-6
ROPE_THETA = 10000.0
Q_BLOCK = 128

D_FF = 2816
CONV_DIM = 512
CONV_WIDTH = 31
MLA_HEADS = 8
Q_LORA = 256
KV_LORA = 128
QK_NOPE = 64
QK_ROPE = 32
V_HEAD = 64
MLA_OUT = MLA_HEADS * V_HEAD
EVEN_IN = 2 * CONV_DIM + Q_LORA + KV_LORA + QK_ROPE
EVEN_MIX = CONV_DIM + MLA_OUT
RET_HEADS = 4
RET_QK = 256
RET_V = 512
RET_CHUNK = 128
ODD_MIX = RET_HEADS * RET_V
ODD_IN = 2 * RET_HEADS * RET_QK + 2 * ODD_MIX
X_HEADS = 4
X_HEAD_DIM = D_MODEL // X_HEADS

kernel_name = 'hybrid_conv_mla_retention_trunk'


def rms_norm(x, g):
    xf = x.astype(jnp.float32)
    y = xf * lax.rsqrt(jnp.mean(xf * xf, axis=-1, keepdims=True) + EPS)
    return (y * g.astype(jnp.float32)).astype(x.dtype)


def layer_norm(x, g, b):
    xf = x.astype(jnp.float32)
    mu = jnp.mean(xf, axis=-1, keepdims=True)
    xc = xf - mu
    var = jnp.mean(xc * xc, axis=-1, keepdims=True)
    return (xc * lax.rsqrt(var + EPS) * g.astype(jnp.float32) + b.astype(jnp.float32)).astype(x.dtype)


def rope_tables(positions, dim):
    inv = ROPE_THETA ** (-jnp.arange(0, dim, 2, dtype=jnp.float32) / dim)
    ang = positions.astype(jnp.float32)[..., None] * inv
    return jnp.cos(ang), jnp.sin(ang)


def apply_rope(x, cos, sin):
    x1, x2 = jnp.split(x, 2, axis=-1)
    c = cos[:, :, None, :].astype(x.dtype)
    s = sin[:, :, None, :].astype(x.dtype)
    return jnp.concatenate([x1 * c - x2 * s, x1 * s + x2 * c], axis=-1)


def swiglu(h, w_gate, w_up, w_down):
    return (jax.nn.silu(h @ w_gate) * (h @ w_up)) @ w_down


def causal_depthwise_conv(a, w, b):
    y = lax.conv_general_dilated(
        a, w[:, None, :].astype(a.dtype), window_strides=(1,),
        padding=[(CONV_WIDTH - 1, 0)], dimension_numbers=('NWC', 'WIO', 'NWC'),
        feature_group_count=a.shape[-1])
    return y + b.astype(a.dtype)


def causal_block_attention(q, k, v, scale):
    B, S, H, Dq = q.shape
    nb = S // Q_BLOCK
    kf = k.astype(jnp.float32)
    vf = v.astype(jnp.float32)
    qb = q.astype(jnp.float32).reshape(B, nb, Q_BLOCK, H, Dq).transpose(1, 0, 2, 3, 4)
    kpos = jnp.arange(S)

    def one_block(args):
        qi, i = args
        s = jnp.einsum('bqhd,bkhd->bhqk', qi, kf) * scale
        qpos = i * Q_BLOCK + jnp.arange(Q_BLOCK)
        s = jnp.where(kpos[None, :] <= qpos[:, None], s, -jnp.inf)
        p = jax.nn.softmax(s, axis=-1)
        return jnp.einsum('bhqk,bkhd->bqhd', p, vf)

    out = lax.map(one_block, (qb, jnp.arange(nb)))
    return out.transpose(1, 0, 2, 3, 4).reshape(B, S, H, v.shape[-1]).astype(v.dtype)


def mla_heads(z_q, z_kv, z_kr, cos, sin, q_a_norm, w_q_b, kv_a_norm, w_kv_b,
              q_nope_norm, k_nope_norm, q_rope_norm, k_rope_norm):
    B, S, _ = z_q.shape
    q = (rms_norm(z_q, q_a_norm) @ w_q_b).reshape(B, S, MLA_HEADS, QK_NOPE + QK_ROPE)
    kv = (rms_norm(z_kv, kv_a_norm) @ w_kv_b).reshape(B, S, MLA_HEADS, QK_NOPE + V_HEAD)
    q_nope, q_rope = jnp.split(q, [QK_NOPE], axis=-1)
    k_nope, v = jnp.split(kv, [QK_NOPE], axis=-1)
    q_nope = rms_norm(q_nope, q_nope_norm)
    k_nope = rms_norm(k_nope, k_nope_norm)
    q_rope = apply_rope(rms_norm(q_rope, q_rope_norm), cos, sin)
    k_rope = apply_rope(rms_norm(z_kr[:, :, None, :], k_rope_norm), cos, sin)
    qf = jnp.concatenate([q_nope, q_rope], axis=-1)
    kf = jnp.concatenate([k_nope, jnp.broadcast_to(k_rope, (B, S, MLA_HEADS, QK_ROPE))], axis=-1)
    o = causal_block_attention(qf, kf, v, (QK_NOPE + QK_ROPE) ** -0.5)
    return o.reshape(B, S, MLA_OUT)


def conv_mla_mixer(h, cos_m, sin_m, w_in, conv_w, conv_b, conv_ln_g, conv_ln_b,
                   q_a_norm, w_q_b, kv_a_norm, w_kv_b, q_nope_norm, k_nope_norm,
                   q_rope_norm, k_rope_norm, w_out):
    z = h @ w_in
    z_val, z_gate, z_q, z_kv, z_kr = jnp.split(
        z, [CONV_DIM, 2 * CONV_DIM, 2 * CONV_DIM + Q_LORA, 2 * CONV_DIM + Q_LORA + KV_LORA], axis=-1)
    a = z_val * jax.nn.sigmoid(z_gate)
    a = jax.nn.silu(layer_norm(causal_depthwise_conv(a, conv_w, conv_b), conv_ln_g, conv_ln_b))
    m = mla_heads(z_q, z_kv, z_kr, cos_m, sin_m, q_a_norm, w_q_b, kv_a_norm, w_kv_b,
                  q_nope_norm, k_nope_norm, q_rope_norm, k_rope_norm)
    return jnp.concatenate([a, m], axis=-1) @ w_out


def retention_chunkwise(q, k, v):
    B, S, H, Dk = q.shape
    Dv = v.shape[-1]
    n = S // RET_CHUNK
    log_g = jnp.log1p(-jnp.exp2(-5.0 - jnp.arange(H, dtype=jnp.float32)))
    idx = jnp.arange(RET_CHUNK, dtype=jnp.float32)
    diff = idx[:, None] - idx[None, :]
    decay = jnp.where(diff >= 0, jnp.exp(log_g[:, None, None] * jnp.maximum(diff, 0.0)), 0.0)
    xi = jnp.exp(log_g[:, None] * (idx + 1.0))[None, :, :, None]
    zeta = jnp.exp(log_g[:, None] * (RET_CHUNK - 1.0 - idx))[None, :, :, None]
    g_chunk = jnp.exp(log_g * RET_CHUNK)[None, :, None, None]

    def to_chunks(t):
        return t.astype(jnp.float32).reshape(B, n, RET_CHUNK, H, t.shape[-1]).transpose(1, 0, 3, 2, 4)

    def step(state, inp):
        qc, kc, vc = inp
        scores = jnp.einsum('bhid,bhjd->bhij', qc, kc) * decay
        out = (jnp.einsum('bhij,bhjv->bhiv', scores, vc)
               + jnp.einsum('bhid,bhdv->bhiv', qc, state) * xi)
        state = state * g_chunk + jnp.einsum('bhjd,bhjv->bhdv', kc * zeta, vc)
        return state, out

    state0 = jnp.zeros((B, H, Dk, Dv), jnp.float32)
    _, out = lax.scan(step, state0, (to_chunks(q), to_chunks(k), to_chunks(v)))
    return out.transpose(1, 0, 3, 2, 4).reshape(B, S, H, Dv)


def retention_mixer(h, cos_r, sin_r, w_in, gn_g, gn_b, w_out):
    B, S, _ = h.shape
    z = h @ w_in
    q, k, v, g = jnp.split(z, [RET_HEADS * RET_QK, 2 * RET_HEADS * RET_QK,
                               2 * RET_HEADS * RET_QK + ODD_MIX], axis=-1)
    q = apply_rope(q.reshape(B, S, RET_HEADS, RET_QK), cos_r, sin_r)
    k = apply_rope(k.reshape(B, S, RET_HEADS, RET_QK), cos_r, sin_r) * (RET_QK ** -0.5)
    v = v.reshape(B, S, RET_HEADS, RET_V)
    o = retention_chunkwise(q, k, v).astype(h.dtype)
    o = layer_norm(o, gn_g.reshape(RET_HEADS, RET_V), gn_b.reshape(RET_HEADS, RET_V))
    return (jax.nn.silu(g) * o.reshape(B, S, ODD_MIX)) @ w_out


def memory_cross_attention(h, m, wq, wk, wv, wo, q_norm, k_norm):
    B, S, _ = h.shape
    M = m.shape[1]
    q = rms_norm((h @ wq).reshape(B, S, X_HEADS, X_HEAD_DIM), q_norm)
    k = rms_norm((m @ wk).reshape(B, M, X_HEADS, X_HEAD_DIM), k_norm)
    v = (m @ wv).reshape(B, M, X_HEADS, X_HEAD_DIM)
    s = jnp.einsum('bshd,bmhd->bhsm', q.astype(jnp.float32), k.astype(jnp.float32)) * (X_HEAD_DIM ** -0.5)
    p = jax.nn.softmax(s, axis=-1)
    o = jnp.einsum('bhsm,bmhd->bshd', p, v.astype(jnp.float32)).astype(h.dtype)
    return o.reshape(B, S, D_MODEL) @ wo


def setup_inputs(seed: int = 0) -> dict:
    key = jax.random.key(seed)
    ks = iter(jax.random.split(key, 64))

    def w(shape, fan_in):
        return jax.random.normal(next(ks), shape, jnp.float32) * (fan_in ** -0.5)

    def gain(shape):
        return 1.0 + 0.02 * jax.random.normal(next(ks), shape, jnp.float32)

    def bias(shape):
        return 0.01 * jax.random.normal(next(ks), shape, jnp.float32)

    L, E, O, D = DEPTH, N_EVEN, N_ODD, D_MODEL
    x = jax.random.normal(next(ks), (BATCH, SEQ, D), jnp.float32)
    mem = jax.random.normal(next(ks), (BATCH, MEM_LEN, D), jnp.float32)
    offset = jax.random.randint(next(ks), (BATCH, 1), 0, 1024, dtype=jnp.int32)
    positions = jnp.arange(SEQ, dtype=jnp.int32)[None, :] + offset
    return {
        'x': x, 'mem': mem, 'positions': positions,
        'ffn1_norm': gain((L, D)), 'ffn1_w_gate': w((L, D, D_FF), D),
        'ffn1_w_up': w((L, D, D_FF), D), 'ffn1_w_down': w((L, D_FF, D), D_FF),
        'ffn2_norm': gain((L, D)), 'ffn2_w_gate': w((L, D, D_FF), D),
        'ffn2_w_up': w((L, D, D_FF), D), 'ffn2_w_down': w((L, D_FF, D), D_FF),
        'mix_norm': gain((L, D)), 'xattn_norm': gain((L, D)), 'mem_norm': gain((L, D)),
        'xattn_wq': w((L, D, D), D), 'xattn_wk': w((L, D, D), D),
        'xattn_wv': w((L, D, D), D), 'xattn_wo': w((L, D, D), D),
        'xattn_q_norm': gain((L, X_HEAD_DIM)), 'xattn_k_norm': gain((L, X_HEAD_DIM)),
        'ev_w_in': w((E, D, EVEN_IN), D),
        'ev_conv_w': w((E, CONV_WIDTH, CONV_DIM), CONV_WIDTH), 'ev_conv_b': bias((E, CONV_DIM)),
        'ev_conv_ln_g': gain((E, CONV_DIM)), 'ev_conv_ln_b': bias((E, CONV_DIM)),
        'ev_q_a_norm': gain((E, Q_LORA)), 'ev_w_q_b': w((E, Q_LORA, MLA_HEADS * (QK_NOPE + QK_ROPE)), Q_LORA),
        'ev_kv_a_norm': gain((E, KV_LORA)), 'ev_w_kv_b': w((E, KV_LORA, MLA_HEADS * (QK_NOPE + V_HEAD)), KV_LORA),
        'ev_q_nope_norm': gain((E, QK_NOPE)), 'ev_k_nope_norm': gain((E, QK_NOPE)),
        'ev_q_rope_norm': gain((E, QK_ROPE)), 'ev_k_rope_norm': gain((E, QK_ROPE)),
        'ev_w_out': w((E, EVEN_MIX, D), EVEN_MIX),
        'od_w_in': w((O, D, ODD_IN), D), 'od_gn_g': gain((O, ODD_MIX)), 'od_gn_b': bias((O, ODD_MIX)),
        'od_w_out': w((O, ODD_MIX, D), ODD_MIX),
    }


def reference(x, mem, positions,
              ffn1_norm, ffn1_w_gate, ffn1_w_up, ffn1_w_down,
              ffn2_norm, ffn2_w_gate, ffn2_w_up, ffn2_w_down,
              mix_norm, xattn_norm, mem_norm,
              xattn_wq, xattn_wk, xattn_wv, xattn_wo, xattn_q_norm, xattn_k_norm,
              ev_w_in, ev_conv_w, ev_conv_b, ev_conv_ln_g, ev_conv_ln_b,
              ev_q_a_norm, ev_w_q_b, ev_kv_a_norm, ev_w_kv_b,
              ev_q_nope_norm, ev_k_nope_norm, ev_q_rope_norm, ev_k_rope_norm, ev_w_out,
              od_w_in, od_gn_g, od_gn_b, od_w_out):
    cos_m, sin_m = rope_tables(positions, QK_ROPE)
    cos_r, sin_r = rope_tables(positions, RET_QK)
    for l in range(DEPTH):
        x = x + 0.5 * swiglu(rms_norm(x, ffn1_norm[l]), ffn1_w_gate[l], ffn1_w_up[l], ffn1_w_down[l])
        h = rms_norm(x, mix_norm[l])
        if l % 2 == 0:
            e = l // 2
            x = x + conv_mla_mixer(h, cos_m, sin_m, ev_w_in[e], ev_conv_w[e], ev_conv_b[e],
                                   ev_conv_ln_g[e], ev_conv_ln_b[e], ev_q_a_norm[e], ev_w_q_b[e],
                                   ev_kv_a_norm[e], ev_w_kv_b[e], ev_q_nope_norm[e], ev_k_nope_norm[e],
                                   ev_q_rope_norm[e], ev_k_rope_norm[e], ev_w_out[e])
        else:
            o = l // 2
            x = x + retention_mixer(h, cos_r, sin_r, od_w_in[o], od_gn_g[o], od_gn_b[o], od_w_out[o])
        x = x + memory_cross_attention(rms_norm(x, xattn_norm[l]), rms_norm(mem, mem_norm[l]),
                                       xattn_wq[l], xattn_wk[l], xattn_wv[l], xattn_wo[l],
                                       xattn_q_norm[l], xattn_k_norm[l])
        x = x + 0.5 * swiglu(rms_norm(x, ffn2_norm[l]), ffn2_w_gate[l], ffn2_w_up[l], ffn2_w_down[l])
    return x
```

```python
import numpy as np
import concourse.bass as bass
import concourse.mybir as mybir
from concourse.bass_utils import run_bass_kernel_spmd

F32 = mybir.dt.float32
BF16 = mybir.dt.bfloat16
I32 = mybir.dt.int32
U8 = mybir.dt.uint8
ALU = mybir.AluOpType
AF = mybir.ActivationFunctionType
AX = mybir.AxisListType

ENGS = ("pe", "act", "dve", "pool", "sp")
EPOCH = 20000
DT_SIZE = {F32: 4, BF16: 2, I32: 4, U8: 1}


class Buf:
    __slots__ = ("name", "w", "r")

    def __init__(self, name=""):
        self.name = name
        self.w = None
        self.r = {}


class Sched:
    def __init__(self, nc):
        self.nc = nc
        self.eobj = {"pe": nc.tensor, "act": nc.scalar, "dve": nc.vector,
                     "pool": nc.gpsimd, "sp": nc.sync}
        self.ops = {e: [] for e in ENGS}
        self.streams = {}
        self.sem_handles = {}
        self.nsem = 0

    def _sem(self, key):
        h = self.sem_handles.get(key)
        if h is None:
            h = self.nc.alloc_semaphore("s%d" % self.nsem)
            self.nsem += 1
            self.sem_handles[key] = h
        return h

    @staticmethod
    def _add(deps, key, val):
        if deps.get(key, -1) < val:
            deps[key] = val

    def _deps(self, e, reads, writes):
        deps = {}
        for b in reads:
            if b.w is not None:
                self._add(deps, *b.w)
        for b in writes:
            if b.w is not None:
                self._add(deps, *b.w)
            for k, v in b.r.items():
                self._add(deps, k, v)
        if e == "pe":
            deps.pop(("e", "pe"), None)
        return deps

    def _mark(self, me, reads, writes):
        for b in reads:
            if b.r.get(me[0], -1) < me[1]:
                b.r[me[0]] = me[1]
        for b in writes:
            b.w = me
            b.r = {}

    def op(self, e, fn, reads=(), writes=()):
        idx = len(self.ops[e])
        deps = self._deps(e, reads, writes)
        self.ops[e].append([fn, deps, None])
        self._mark((("e", e), idx), reads, writes)

    def dma(self, q, out_ap, in_ap, reads=(), writes=(), stream="ld", ring=6, **kw):
        st = self.streams.setdefault(stream, [0, ring])
        i = st[0]
        st[0] += 1
        slot = i % st[1]
        gen = i // st[1]
        key = ("d", stream, slot)
        deps = self._deps(q, reads, writes)
        if gen > 0:
            self._add(deps, key, 16 * gen)

        def fn(eng, out_ap=out_ap, in_ap=in_ap, kw=kw):
            return eng.dma_start(out=out_ap, in_=in_ap, **kw)
        self.ops[q].append([fn, deps, key])
        self._mark((key, 16 * (gen + 1)), reads, writes)

    def barrier(self):
        last = {}
        for e in ENGS:
            for i in range(len(self.ops[e]) - 1, -1, -1):
                if self.ops[e][i][2] is None:
                    last[("e", e)] = i
                    break
        for name, (cnt, ring) in self.streams.items():
            for slot in range(min(cnt, ring)):
                n = (cnt - 1 - slot) // ring + 1
                last[("d", name, slot)] = 16 * n
        for e in ENGS:
            d = dict(last)
            if e == "pe":
                d.pop(("e", "pe"), None)
            self.ops[e].append([lambda eng: eng.nop(), d, None])

    def finalize(self):
        nc = self.nc
        need = {e: set() for e in ENGS}
        for e in ENGS:
            for fn, deps, dk in self.ops[e]:
                for k, v in deps.items():
                    if k[0] == "e":
                        need[k[1]].add(v)
        rank = {}
        for e in ENGS:
            r = 0
            for i in sorted(need[e]):
                rank[(e, i)] = r
                r += 1

        def resolve(k, v):
            if k[0] == "e":
                r = rank[(k[1], v)]
                return self._sem(("e", k[1], r // EPOCH)), r % EPOCH + 1
            return self._sem(k), v

        for e in ENGS:
            for fn, deps, dk in self.ops[e]:
                for k, v in deps.items():
                    resolve(k, v)
                if dk is not None:
                    self._sem(dk)

        def emit(e, eng):
            seen = {}
            for i, (fn, deps, dk) in enumerate(self.ops[e]):
                for k, v in deps.items():
                    sem, val = resolve(k, v)
                    sk = id(sem)
                    if seen.get(sk, -1) >= val:
                        continue
                    seen[sk] = val
                    eng.wait_ge(sem, val)
                ins = fn(eng)
                if dk is not None:
                    ins.then_inc(self._sem(dk), 16)
                elif (e, i) in rank:
                    r = rank[(e, i)]
                    ins.then_inc(self._sem(("e", e, r // EPOCH)), 1)

        with nc.Block() as block:
            @block.tensor
            def _(eng):
                emit("pe", eng)

            @block.scalar
            def _(eng):
                emit("act", eng)

            @block.vector
            def _(eng):
                emit("dve", eng)

            @block.gpsimd
            def _(eng):
                emit("pool", eng)

            @block.sync
            def _(eng):
                emit("sp", eng)
        for h in self.sem_handles.values():
            nc.gpsimd.sem_clear(h)
        nc.all_engine_barrier()


class Arena:
    def __init__(self, nc, nbytes):
        self.t = nc.alloc_sbuf_tensor("arena", [128, nbytes], U8)
        self.n = nbytes
        self.off = 0
        self.marks = []

    def alloc(self, shape, dt):
        n = int(np.prod(shape[1:])) * DT_SIZE[dt]
        n_al = (n + 63) // 64 * 64
        assert self.off + n_al <= self.n, ("SBUF arena overflow", self.off, n_al, self.n)
        v = self.t[0:shape[0], self.off:self.off + n].bitcast(dt)
        self.off += n_al
        if len(shape) == 3:
            v = v.rearrange("p (a b) -> p a b", a=shape[1])
        elif len(shape) == 4:
            v = v.rearrange("p (a b c) -> p a b c", a=shape[1], b=shape[2])
        return v

    def mark(self):
        return self.off

    def reset(self, m):
        self.off = m


D = 1024
DFF = 2816
NFC = DFF // 128
EPS = 1e-6
TT = 512
MEM = 256
TWO_PI = 6.283184


def host_consts():
    import ml_dtypes
    bf = ml_dtypes.bfloat16
    c = {}
    c["c_ident"] = np.eye(128, dtype=np.float32).astype(bf)
    c["c_ones"] = np.ones((128, 128), np.float32).astype(bf)
    bd64 = np.zeros((128, 128), np.float32)
    bd64[:64, :64] = 1
    bd64[64:, 64:] = 1
    c["c_bd64"] = bd64.astype(bf)
    bd32 = np.zeros((64, 64), np.float32)
    bd32[:32, :32] = 1
    bd32[32:, 32:] = 1
    c["c_bd32"] = bd32.astype(bf)
    rot = np.zeros((64, 64), np.float32)
    for m in range(64):
        if m % 32 < 16:
            rot[m + 16, m] = -1.0
        else:
            rot[m - 16, m] = 1.0
    c["c_rot"] = rot
    sel = np.zeros((65, 64), np.float32)
    sel[64, :] = 1.0
    c["c_sel"] = sel
    mask = np.zeros((128, 128), np.float32)
    for j in range(128):
        mask[j, j:] = 1.0
    c["c_mask"] = mask.astype(bf)
    inv_m = (10000.0 ** (-np.arange(0, 32, 2, dtype=np.float32) / 32)).astype(np.float32)
    inv_r = (10000.0 ** (-np.arange(0, 256, 2, dtype=np.float32) / 256)).astype(np.float32)
    tab = np.zeros((128, 2), np.float32)
    tab[:, 0] = inv_m[np.arange(128) % 16] / (2 * np.pi)
    tab[:, 1] = inv_r / (2 * np.pi)
    c["c_inv"] = tab
    H = 4
    log_g = np.log1p(-np.exp2(-5.0 - np.arange(H, dtype=np.float64)))
    idx = np.arange(128, dtype=np.float64)
    diff = idx[None, :] - idx[:, None]
    dec = np.where(diff >= 0, np.exp(log_g[:, None, None] * np.maximum(diff, 0.0)), 0.0)
    c["c_decay"] = np.ascontiguousarray(dec.transpose(1, 0, 2)).astype(np.float32)
    xi = np.exp(log_g[:, None] * (idx + 1.0))
    c["c_xi"] = np.tile(xi[None, :, None, :], (128, 1, 4, 1)).reshape(128, 4, 512).astype(np.float32)
    zeta = np.exp(log_g[:, None] * (128 - 1.0 - idx))
    c["c_zeta"] = np.ascontiguousarray(zeta.T).astype(np.float32)
    c["_gchunk"] = [float(np.exp(log_g[h] * 128)) for h in range(H)]
    return c


class Builder:
    def __init__(self, n_seq, S, depth=4, phases=None):
        self.n_seq, self.S, self.depth = n_seq, S, depth
        self.NT = n_seq * S
        self.TPS = S // TT
        self.phases = phases
        nc = bass.Bass("TRN2", target_bir_lowering=False)
        self.nc = nc
        self.S_ = Sched(nc)
        self.arena = Arena(nc, 212000)
        self.PS = [nc.alloc_psum_tensor("ps%d" % i, [128, 512], F32)[:, :] for i in range(8)]
        self.bPS = [Buf("ps%d" % i) for i in range(8)]
        self.cast_rr = 0
        self.stage_i = 0
        self.gch = host_consts()["_gchunk"]

    def inp(self, name, shape, dt=F32):
        return self.nc.dram_tensor(name, list(shape), dt, kind="ExternalInput").ap()

    def scratch(self, name, shape, dt):
        return self.nc.dram_tensor(name, list(shape), dt).ap()

    def mm(self, out, lhsT, rhs, start, stop, reads, writes):
        self.S_.op("pe", lambda eng: eng.matmul(out=out, lhsT=lhsT, rhs=rhs, start=start, stop=stop),
                   reads, writes)

    def act(self, out, in_, func, reads, writes, **kw):
        self.S_.op("act", lambda eng: eng.activation(out=out, in_=in_, func=func, **kw), reads, writes)

    def tt(self, e, out, in0, in1, op, reads, writes):
        self.S_.op(e, lambda eng: eng.tensor_tensor(out=out, in0=in0, in1=in1, op=op), reads, writes)

    def ts(self, out, in0, s1, s2, op0, op1, reads, writes, e="dve"):
        if s2 is None:
            self.S_.op(e, lambda eng: eng.tensor_scalar(out=out, in0=in0, scalar1=s1, scalar2=None, op0=op0),
                       reads, writes)
        else:
            self.S_.op(e, lambda eng: eng.tensor_scalar(out=out, in0=in0, scalar1=s1, scalar2=s2,
                                                        op0=op0, op1=op1), reads, writes)

    def stt(self, out, in0, scalar, in1, op0, op1, reads, writes, e="dve"):
        self.S_.op(e, lambda eng: eng.scalar_tensor_tensor(out=out, in0=in0, scalar=scalar, in1=in1,
                                                           op0=op0, op1=op1), reads, writes)

    def cp(self, e, out, in_, reads, writes):
        if e == "act":
            self.S_.op(e, lambda eng: eng.copy(out=out, in_=in_), reads, writes)
        else:
            self.S_.op(e, lambda eng: eng.tensor_copy(out=out, in_=in_), reads, writes)

    def recip(self, out, in_, reads, writes):
        self.S_.op("dve", lambda eng: eng.reciprocal(out=out, in_=in_), reads, writes)

    def memset(self, e, out, val, writes):
        self.S_.op(e, lambda eng: eng.memset(out, val), (), writes)

    def dma(self, q, out, in_, reads=(), writes=(), stream="ld", ring=4, **kw):
        self.S_.dma(q, out, in_, reads=reads, writes=writes, stream=stream, ring=ring, **kw)

    def cast_op(self, out, in_, reads, writes, scale_ap=None):
        S = self.S_
        if scale_ap is None:
            e = ("dve", "pool", "act")[self.cast_rr % 3]
        else:
            e = ("dve", "act")[self.cast_rr % 2]
        self.cast_rr += 1
        if scale_ap is None:
            self.cp(e, out, in_, reads, writes)
        elif e == "act":
            self.act(out, in_, AF.Copy, reads, writes, scale=scale_ap)
        else:
            self.ts(out, in_, scale_ap, None, ALU.mult, None, reads, writes)

    def new_stage(self, A, n=2, cols=1024):
        return [(A.alloc([128, cols], F32), Buf("stg")) for _ in range(n)]

    def stage_load(self, stage, src):
        k = self.stage_i
        self.stage_i += 1
        sb, sbuf = stage[k % len(stage)]
        p, n = src.shape
        q = ("sp", "pool")[k % 2]
        self.dma(q, sb[0:p, 0:n], src, writes=(sbuf,), stream="wld", ring=4)
        return sb[0:p, 0:n], sbuf

    def load_rows(self, stage, dst, dbuf, src, gain=None, gbuf=None):
        p, n = src.shape
        cols = stage[0][0].shape[1]
        for c0 in range(0, n, cols):
            w = min(cols, n - c0)
            sv, sbuf = self.stage_load(stage, src[:, c0:c0 + w])
            rd = (sbuf,) if gain is None else (sbuf, gbuf)
            self.cast_op(dst[:, c0:c0 + w], sv, rd, (dbuf,), gain)

    def load_const(self, A, dram, shape, dt):
        t = A.alloc(list(shape), dt)
        b = Buf("const")
        self.dma("sp", t, dram, writes=(b,), stream="misc", ring=4)
        return t, b

    def load_col(self, A, vec, nchunk, npart=128):
        t = A.alloc([128, nchunk], F32)
        b = Buf("col")
        self.dma("sp", t[0:npart, :], vec.rearrange("(c p) -> p c", p=npart), writes=(b,), stream="misc",
                 ring=4, allow_slow_non_contiguous=True)
        return t, b

    def load_col_rep(self, A, vec, n, reps, scale=None):
        t = A.alloc([128, 1], F32)
        b = Buf("colr")
        for r in range(reps):
            self.dma("sp", t[r * n:(r + 1) * n, :], vec.rearrange("(p o) -> p o", o=1), writes=(b,),
                     stream="misc", ring=4, allow_slow_non_contiguous=True)
        if scale is not None:
            self.ts(t[0:n * reps, :], t[0:n * reps, :], float(scale), None, ALU.mult, None, (b,), (b,))
        return t, b

    def norm_ctx(self, A, use_ln):
        C = {"use_ln": use_ln}
        C["XN"] = [(A.alloc([128, D], F32), Buf("xn")) for _ in range(2)]
        C["HN"] = [(A.alloc([128, D], BF16), Buf("hn")) for _ in range(4)]
        C["HT"] = [(A.alloc([128, 8, TT], BF16), Buf("ht")) for _ in range(2)]
        C["SS"] = [(A.alloc([128, 16], F32), Buf("ss")) for _ in range(2)]
        C["ident"], C["bid"] = self.load_const(A, self.c["c_ident"], [128, 128], BF16)
        C["x"] = 0
        C["pt"] = 0
        return C

    def norm1(self, C, t, src, nsub=4):
        ss, bss = C["SS"][t % 2]
        for j in range(nsub):
            xn, bxn = C["XN"][C["x"] % 2]
            C["x"] += 1
            hn, bhn = C["HN"][j]
            self.dma("sp", xn, src(j), writes=(bxn,), stream="xn", ring=2)
            self.act(hn, xn, AF.Square, (bxn,), (bhn, bss), accum_out=ss[:, j:j + 1])
        if C["use_ln"]:
            self.act(ss[:, 8:8 + nsub], ss[:, 0:nsub], AF.Ln, (bss,), (bss,), scale=1.0 / D, bias=EPS)
            self.act(ss[:, 12:12 + nsub], ss[:, 8:8 + nsub], AF.Exp, (bss,), (bss,), scale=-0.5)
        else:
            self.ts(ss[:, 4:4 + nsub], ss[:, 0:nsub], 1.0 / D, EPS, ALU.mult, ALU.add, (bss,), (bss,))
            self.act(ss[:, 8:8 + nsub], ss[:, 4:4 + nsub], AF.Sqrt, (bss,), (bss,))
            self.recip(ss[:, 12:12 + nsub], ss[:, 8:8 + nsub], (bss,), (bss,))
        for j in range(nsub):
            xn, bxn = C["XN"][C["x"] % 2]
            C["x"] += 1
            hn, bhn = C["HN"][j]
            self.dma("sp", xn, src(j), writes=(bxn,), stream="xn", ring=2)
            self.act(hn, xn, AF.Copy, (bxn, bss), (bhn,), scale=ss[:, 12 + j:13 + j])

    def norm2(self, C, t, nsub=4):
        ht, bht = C["HT"][t % 2]
        PS, bPS = self.PS, self.bPS
        ident, bid = C["ident"], C["bid"]
        for j in range(nsub):
            hn, bhn = C["HN"][j]
            pi = 6 + C["pt"] % 2
            C["pt"] += 1
            pt = PS[pi].bitcast(BF16).rearrange("p (c t) -> p c t", c=8)
            for c in range(8):
                self.S_.op("pe", lambda eng, pt=pt, hn=hn, c=c: eng.transpose(
                    out=pt[:, c, :], in_=hn[:, c * 128:(c + 1) * 128], identity=ident),
                    reads=(bhn, bid), writes=(bPS[pi],))
            self.cp("dve", ht[:, :, j * 128:(j + 1) * 128], pt, (bPS[pi],), (bht,))
        return ht, bht

    def resid_ctx(self, A):
        return {"XR": [(A.alloc([128, D], F32), Buf("xr")) for _ in range(2)]}

    def out_proj(self, R, t, js, steps, xin, xout, scale, banks=(4, 5)):
        PS, bPS = self.PS, self.bPS
        n = len(steps)
        for j in js:
            r0 = t * TT + j * 128
            xr, bxr = R["XR"][j % 2]
            self.dma("pool", xr, xin[r0:r0 + 128, :], writes=(bxr,), stream="xr", ring=2)
            for h in range(2):
                po = banks[h]
                for i, (lf, rf, rd) in enumerate(steps):
                    self.mm(PS[po], lf(j), rf(h), i == 0, i == n - 1, rd, (bPS[po],))
                self.stt(xr[:, h * 512:(h + 1) * 512], PS[po], float(scale), xr[:, h * 512:(h + 1) * 512],
                         ALU.mult, ALU.add, (bPS[po], bxr), (bxr,))
            self.dma("sp", xout[r0:r0 + 128, :], xr, reads=(bxr,), stream="xst", ring=2)

    def temp_pool(self, A, n32, n16):
        self.T32 = [(A.alloc([128, TT], F32), Buf("t32")) for _ in range(n32)]
        self.T16 = [(A.alloc([128, TT], BF16), Buf("t16")) for _ in range(n16)]
        self.t32_i = 0
        self.t16_i = 0

    def t32(self):
        r = self.T32[self.t32_i % len(self.T32)]
        self.t32_i += 1
        return r

    def t16(self):
        r = self.T16[self.t16_i % len(self.T16)]
        self.t16_i += 1
        return r

    def rstd_fm(self, ps_sum, bps, P, inv_n):
        L, bL = self.t32()
        self.act(L[0:P, :], ps_sum, AF.Ln, (bps,), (bL,), scale=float(inv_n), bias=EPS)
        self.act(L[0:P, :], L[0:P, :], AF.Exp, (bL,), (bL,), scale=-0.5)
        return L, bL

    def fm_norm(self, srcs, P, G, bG, nbank, inv_n, outs, gcol=None, bgcol=None):
        PS, bPS = self.PS, self.bPS
        xs = []
        for i, (ps, bps) in enumerate(srcs):
            X, bX = self.t32()
            Q, bQ = self.t16()
            self.act(X[0:P, :], ps, AF.Copy, (bps,), (bX,))
            self.act(Q[0:P, :], ps, AF.Square, (bps,), (bQ,))
            xs.append((X, bX, Q, bQ))
        pn = PS[nbank][0:P, :]
        for i, (X, bX, Q, bQ) in enumerate(xs):
            self.mm(pn, G, Q[0:P, :], i == 0, i == len(xs) - 1, (bG, bQ), (bPS[nbank],))
        Rr, bR = self.rstd_fm(pn, bPS[nbank], P, inv_n)
        for (X, bX, Q, bQ), (o, bo) in zip(xs, outs):
            if gcol is None:
                self.tt("dve", o, X[0:P, :], Rr[0:P, :], ALU.mult, (bX, bR), (bo,))
            else:
                self.stt(o, X[0:P, :], gcol, Rr[0:P, :], ALU.mult, ALU.mult, (bX, bR, bgcol), (bo,))

    def ffn_phase(self, xin, xout, norm_g, wg, wu, wd):
        S, A = self.S_, self.arena
        m0 = A.mark()
        WG = A.alloc([128, 8, DFF], BF16)
        WU = A.alloc([128, 8, DFF], BF16)
        WD = A.alloc([128, NFC, D], BF16)
        bWG, bWU, bWD = Buf("WG"), Buf("WU"), Buf("WD")
        stage = self.new_stage(A, 2)
        gcol, bg = self.load_col(A, norm_g, 8)
        C = self.norm_ctx(A, use_ln=False)
        R = self.resid_ctx(A)
        ACTT = A.alloc([128, NFC, TT], BF16)
        bACT = [Buf("act%d" % c) for c in range(NFC)]
        SG = [(A.alloc([128, TT], BF16), Buf("sg")) for _ in range(2)]
        for c in range(8):
            self.load_rows(stage, WG[:, c, :], bWG, wg[c * 128:(c + 1) * 128, :], gcol[:, c:c + 1], bg)
        for c in range(8):
            self.load_rows(stage, WU[:, c, :], bWU, wu[c * 128:(c + 1) * 128, :], gcol[:, c:c + 1], bg)
        for c in range(NFC):
            self.load_rows(stage, WD[:, c, :], bWD, wd[c * 128:(c + 1) * 128, :])
        PS, bPS = self.PS, self.bPS
        ntiles = self.NT // TT

        def src(t):
            return lambda j: xin[t * TT + j * 128:t * TT + (j + 1) * 128, :]

        def gateup(t):
            ht, bht = C["HT"][t % 2]
            for c in range(NFC):
                pg, pu = c % 2, 2 + c % 2
                for k in range(8):
                    self.mm(PS[pg], WG[:, k, c * 128:(c + 1) * 128], ht[:, k, :], k == 0, k == 7,
                            (bWG, bht), (bPS[pg],))
                for k in range(8):
                    self.mm(PS[pu], WU[:, k, c * 128:(c + 1) * 128], ht[:, k, :], k == 0, k == 7,
                            (bWU, bht), (bPS[pu],))
                sg, bsg = SG[c % 2]
                self.act(sg, PS[pg], AF.Silu, (bPS[pg],), (bsg,))
                self.tt("dve", ACTT[:, c, :], PS[pu], sg, ALU.mult, (bPS[pu], bsg), (bACT[c],))

        def down(t, js):
            steps = [((lambda j, c=c: ACTT[:, c, j * 128:(j + 1) * 128]),
                      (lambda h, c=c: WD[:, c, h * 512:(h + 1) * 512]),
                      (bWD, bACT[c])) for c in range(NFC)]
            self.out_proj(R, t, js, steps, xin, xout, 0.5)

        self.norm1(C, 0, src(0))
        self.norm2(C, 0)
        for t in range(ntiles):
            gateup(t)
            if t + 1 < ntiles:
                self.norm1(C, t + 1, src(t + 1))
            down(t, (0, 1))
            if t + 1 < ntiles:
                self.norm2(C, t + 1)
            down(t, (2, 3))
        S.barrier()
        A.reset(m0)

    def rope_phase(self, positions):
        S, A = self.S_, self.arena
        m0 = A.mark()
        ns, SQ = self.n_seq, self.S
        self.CM = self.scratch("s_cm", [ns, 64, SQ], F32)
        self.SM = self.scratch("s_sm", [ns, 64, SQ], F32)
        self.CR = self.scratch("s_cr", [ns, 128, SQ], F32)
        self.SR = self.scratch("s_sr", [ns, 128, SQ], F32)
        inv, binv = self.load_const(A, self.c["c_inv"], [128, 2], F32)
        POS = [(A.alloc([128, TT], I32), Buf("pos")) for _ in range(2)]
        PF = [(A.alloc([128, TT], F32), Buf("pf")) for _ in range(2)]
        U = [(A.alloc([128, TT], F32), Buf("u")) for _ in range(2)]
        KI = [(A.alloc([128, TT], I32), Buf("ki")) for _ in range(2)]
        KF = [(A.alloc([128, TT], F32), Buf("kf")) for _ in range(2)]
        OUT = [(A.alloc([128, TT], F32), Buf("ro")) for _ in range(4)]
        n = 0
        for s in range(ns):
            for ti in range(self.TPS):
                c0 = ti * TT
                pos, bpos = POS[(s * self.TPS + ti) % 2]
                pf, bpf = PF[(s * self.TPS + ti) % 2]
                self.dma("sp", pos, positions[s:s + 1, c0:c0 + TT].partition_broadcast(128), writes=(bpos,),
                         stream="misc", ring=4)
                self.cp("dve", pf, pos, (bpos,), (bpf,))
                for typ, P, cdst, sdst in ((0, 64, self.CM, self.SM), (1, 128, self.CR, self.SR)):
                    for off, dst in ((0.25, cdst), (0.0, sdst)):
                        u, bu = U[n % 2]
                        ki, bki = KI[n % 2]
                        kf, bkf = KF[n % 2]
                        o, bo = OUT[n % 4]
                        n += 1
                        self.ts(u[0:P, :], pf[0:P, :], inv[0:P, typ:typ + 1], off, ALU.mult, ALU.add,
                                (bpf, binv), (bu,))
                        self.cp("dve", ki[0:P, :], u[0:P, :], (bu,), (bki,))
                        self.cp("dve", kf[0:P, :], ki[0:P, :], (bki,), (bkf,))
                        self.tt("dve", u[0:P, :], u[0:P, :], kf[0:P, :], ALU.subtract, (bu, bkf), (bu,))
                        self.ts(kf[0:P, :], u[0:P, :], 0.5, None, ALU.is_gt, None, (bu,), (bkf,))
                        self.tt("dve", u[0:P, :], u[0:P, :], kf[0:P, :], ALU.subtract, (bu, bkf), (bu,))
                        self.act(o[0:P, :], u[0:P, :], AF.Sin, (bu,), (bo,), scale=TWO_PI)
                        self.dma("sp", dst[s, :, c0:c0 + TT], o[0:P, :], reads=(bo,), stream="rst", ring=4)
        S.barrier()
        A.reset(m0)

    def xattn_phase(self, xin, xout, mem, xnorm, mnorm, wq, wk, wv, wo, qn, kn):
        S, A = self.S_, self.arena
        PS, bPS = self.PS, self.bPS
        m0 = A.mark()
        stage = self.new_stage(A, 2)
        WQ = A.alloc([128, 8, D], BF16)
        WO = A.alloc([128, 8, D], BF16)
        WK = A.alloc([128, 8, D], BF16)
        WV = A.alloc([128, 8, D], BF16)
        bWQ, bWO, bWK, bWV = Buf("wq"), Buf("wo"), Buf("wk"), Buf("wv")
        gx, bgx = self.load_col(A, xnorm, 8)
        gm, bgm = self.load_col(A, mnorm, 8)
        gq, bgq = self.load_col(A, qn, 2)
        gk, bgk = self.load_col(A, kn, 2)
        self.ts(gk[:, 0:2], gk[:, 0:2], 1.0 / 16.0, None, ALU.mult, None, (bgk,), (bgk,))
        ones, bones = self.load_const(A, self.c["c_ones"], [128, 128], BF16)
        C = self.norm_ctx(A, use_ln=True)
        R = self.resid_ctx(A)
        self.temp_pool(A, 6, 4)
        for c in range(8):
            self.load_rows(stage, WK[:, c, :], bWK, wk[c * 128:(c + 1) * 128, :], gm[:, c:c + 1], bgm)
            self.load_rows(stage, WV[:, c, :], bWV, wv[c * 128:(c + 1) * 128, :], gm[:, c:c + 1], bgm)
        for c in range(8):
            self.load_rows(stage, WQ[:, c, :], bWQ, wq[c * 128:(c + 1) * 128, :], gx[:, c:c + 1], bgx)
            self.load_rows(stage, WO[:, c, :], bWO, wo[c * 128:(c + 1) * 128, :])
        KN = [(A.alloc([128, 8, MEM], BF16), Buf("kn")) for _ in range(self.n_seq)]
        VM = [(A.alloc([128, 2, D], BF16), Buf("vm")) for _ in range(self.n_seq)]
        QN = (A.alloc([128, 8, TT], BF16), Buf("qn"))
        PT = [(A.alloc([128, TT], BF16), Buf("p")) for _ in range(4)]
        OT = A.alloc([128, 8, TT], BF16)
        bOT = [Buf("ot%d" % c) for c in range(8)]
        RD = (A.alloc([128, TT], F32), Buf("rd"))
        for s in range(self.n_seq):
            self.norm1(C, s, lambda j, s=s: mem[s, j * 128:(j + 1) * 128, :], nsub=2)
            mt, bmt = self.norm2(C, s, nsub=2)
            kn_t, bkn = KN[s]
            vm_t, bvm = VM[s]
            for hh in range(4):
                srcs = []
                for dc in range(2):
                    c = 2 * hh + dc
                    b = dc
                    for k in range(8):
                        self.mm(PS[b][:, 0:MEM], WK[:, k, c * 128:(c + 1) * 128], mt[:, k, 0:MEM], k == 0, k == 7,
                                (bWK, bmt), (bPS[b],))
                    srcs.append((PS[b][:, 0:MEM], bPS[b]))
                xs = []
                for (ps, bps) in srcs:
                    X, bX = self.t32()
                    Q, bQ = self.t16()
                    self.act(X[:, 0:MEM], ps, AF.Copy, (bps,), (bX,))
                    self.act(Q[:, 0:MEM], ps, AF.Square, (bps,), (bQ,))
                    xs.append((X, bX, Q, bQ))
                for i, (X, bX, Q, bQ) in enumerate(xs):
                    self.mm(PS[2][:, 0:MEM], ones, Q[:, 0:MEM], i == 0, i == 1, (bones, bQ), (bPS[2],))
                L, bL = self.t32()
                self.act(L[:, 0:MEM], PS[2][:, 0:MEM], AF.Ln, (bPS[2],), (bL,), scale=1.0 / 256, bias=EPS)
                self.act(L[:, 0:MEM], L[:, 0:MEM], AF.Exp, (bL,), (bL,), scale=-0.5)
                for dc, (X, bX, Q, bQ) in enumerate(xs):
                    self.stt(kn_t[:, 2 * hh + dc, :], X[:, 0:MEM], gk[:, dc:dc + 1], L[:, 0:MEM], ALU.mult, ALU.mult,
                             (bX, bL, bgk), (bkn,))
            for mc in range(2):
                for h in range(2):
                    b = 4 + h
                    for k in range(8):
                        self.mm(PS[b], mt[:, k, mc * 128:(mc + 1) * 128], WV[:, k, h * 512:(h + 1) * 512],
                                k == 0, k == 7, (bWV, bmt), (bPS[b],))
                    self.cp("dve", vm_t[:, mc, h * 512:(h + 1) * 512], PS[b], (bPS[b],), (bvm,))
        ntiles = self.NT // TT

        def src(t):
            return lambda j: xin[t * TT + j * 128:t * TT + (j + 1) * 128, :]

        def attend(t, slot):
            s = t // self.TPS
            ht, bht = C["HT"][slot % 2]
            kn_t, bkn = KN[s]
            vm_t, bvm = VM[s]
            qn_t, bqn = QN
            for hh in range(4):
                srcs = []
                for dc in range(2):
                    c = 2 * hh + dc
                    b = dc
                    for k in range(8):
                        self.mm(PS[b], WQ[:, k, c * 128:(c + 1) * 128], ht[:, k, :], k == 0, k == 7,
                                (bWQ, bht), (bPS[b],))
                    srcs.append((PS[b], bPS[b]))
                xs = []
                for (ps, bps) in srcs:
                    X, bX = self.t32()
                    Q, bQ = self.t16()
                    self.act(X, ps, AF.Copy, (bps,), (bX,))
                    self.act(Q, ps, AF.Square, (bps,), (bQ,))
                    xs.append((X, bX, Q, bQ))
                for i, (X, bX, Q, bQ) in enumerate(xs):
                    self.mm(PS[2], ones, Q, i == 0, i == 1, (bones, bQ), (bPS[2],))
                L, bL = self.rstd_fm(PS[2], bPS[2], 128, 1.0 / 256)
                for dc, (X, bX, Q, bQ) in enumerate(xs):
                    self.stt(qn_t[:, 2 * hh + dc, :], X, gq[:, dc:dc + 1], L, ALU.mult, ALU.mult,
                             (bX, bL, bgq), (bqn,))
                ps_ = []
                for mc in range(2):
                    b = 3 + mc
                    for dc in range(2):
                        self.mm(PS[b], kn_t[:, 2 * hh + dc, mc * 128:(mc + 1) * 128], qn_t[:, 2 * hh + dc, :],
                                dc == 0, dc == 1, (bkn, bqn), (bPS[b],))
                    p, bp = PT[(2 * hh + mc) % 4]
                    self.act(p, PS[b], AF.Exp, (bPS[b],), (bp,))
                    ps_.append((p, bp))
                for mc, (p, bp) in enumerate(ps_):
                    self.mm(PS[5], ones, p, mc == 0, mc == 1, (bones, bp), (bPS[5],))
                rd, brd = RD
                self.recip(rd, PS[5], (bPS[5],), (brd,))
                for dc in range(2):
                    c = 2 * hh + dc
                    b = dc
                    for mc, (p, bp) in enumerate(ps_):
                        self.mm(PS[b], vm_t[:, mc, c * 128:(c + 1) * 128], p, mc == 0, mc == 1,
                                (bvm, bp), (bPS[b],))
                    self.tt("dve", OT[:, c, :], PS[b], rd, ALU.mult, (bPS[b], brd), (bOT[c],))

        def outp(t, js):
            steps = [((lambda j, c=c: OT[:, c, j * 128:(j + 1) * 128]),
                      (lambda h, c=c: WO[:, c, h * 512:(h + 1) * 512]),
                      (bWO, bOT[c])) for c in range(8)]
            self.out_proj(R, t, js, steps, xin, xout, 1.0, banks=(3, 4))

        base = self.n_seq
        self.norm1(C, base + 0, src(0))
        self.norm2(C, base + 0)
        for t in range(ntiles):
            attend(t, base + t)
            if t + 1 < ntiles:
                self.norm1(C, base + t + 1, src(t + 1))
            outp(t, (0, 1))
            if t + 1 < ntiles:
                self.norm2(C, base + t + 1)
            outp(t, (2, 3))
        S.barrier()
        A.reset(m0)

    def build(self):
        nc = self.nc
        NT, L, ns = self.NT, self.depth, self.n_seq
        E, O = (L + 1) // 2, L // 2
        hc = host_consts()
        self.c = {}
        for k, v in hc.items():
            if k.startswith("c_"):
                dt = BF16 if v.dtype != np.float32 else F32
                self.c[k] = self.inp(k, v.shape, dt)
        I = {}
        I["x"] = self.inp("x", [NT, D])
        I["mem"] = self.inp("mem", [ns, MEM, D])
        I["positions"] = self.inp("positions", [ns, self.S], I32)
        for nm, shp in PARAM_SHAPES(L, E, O):
            I[nm] = self.inp(nm, shp)
        y = nc.dram_tensor("y", [NT, D], F32, kind="ExternalOutput").ap()
        self.I = I
        phases = self.phases
        if phases is None:
            phases = [("rope",)]
            for l in range(L):
                phases.append(("ffn1", l))
                phases.append(("even", l) if l % 2 == 0 else ("odd", l))
                phases.append(("xattn", l))
                phases.append(("ffn2", l))
        cur = I["x"]
        for ph in phases:
            kind = ph[0]
            if kind == "rope":
                self.rope_phase(I["positions"])
                continue
            l = ph[1]
            if kind in ("ffn1", "ffn2"):
                self.ffn_phase(cur, y, I[kind + "_norm"][l], I[kind + "_w_gate"][l], I[kind + "_w_up"][l],
                               I[kind + "_w_down"][l])
            elif kind == "xattn":
                self.xattn_phase(cur, y, I["mem"], I["xattn_norm"][l], I["mem_norm"][l], I["xattn_wq"][l],
                                 I["xattn_wk"][l], I["xattn_wv"][l], I["xattn_wo"][l], I["xattn_q_norm"][l],
                                 I["xattn_k_norm"][l])
            elif kind == "even":
                self.even_phase(cur, y, l // 2, l)
            elif kind == "odd":
                self.odd_phase(cur, y, l // 2, l)
            cur = y
        self.S_.finalize()
        return nc


def PARAM_SHAPES(L, E, O):
    return [
        ("ffn1_norm", [L, D]), ("ffn1_w_gate", [L, D, DFF]), ("ffn1_w_up", [L, D, DFF]), ("ffn1_w_down", [L, DFF, D]),
        ("ffn2_norm", [L, D]), ("ffn2_w_gate", [L, D, DFF]), ("ffn2_w_up", [L, D, DFF]), ("ffn2_w_down", [L, DFF, D]),
        ("mix_norm", [L, D]), ("xattn_norm", [L, D]), ("mem_norm", [L, D]),
        ("xattn_wq", [L, D, D]), ("xattn_wk", [L, D, D]), ("xattn_wv", [L, D, D]), ("xattn_wo", [L, D, D]),
        ("xattn_q_norm", [L, 256]), ("xattn_k_norm", [L, 256]),
        ("ev_w_in", [E, D, 1440]), ("ev_conv_w", [E, 31, 512]), ("ev_conv_b", [E, 512]),
        ("ev_conv_ln_g", [E, 512]), ("ev_conv_ln_b", [E, 512]), ("ev_q_a_norm", [E, 256]),
        ("ev_w_q_b", [E, 256, 768]), ("ev_kv_a_norm", [E, 128]), ("ev_w_kv_b", [E, 128, 1024]),
        ("ev_q_nope_norm", [E, 64]), ("ev_k_nope_norm", [E, 64]), ("ev_q_rope_norm", [E, 32]),
        ("ev_k_rope_norm", [E, 32]), ("ev_w_out", [E, D, D]),
        ("od_w_in", [O, D, 6144]), ("od_gn_g", [O, 2048]), ("od_gn_b", [O, 2048]), ("od_w_out", [O, 2048, D]),
    ]


N_CORES = 8


def kernel(**inputs):
    x = np.ascontiguousarray(inputs["x"], dtype=np.float32)
    B, S, _ = x.shape
    ns = B // N_CORES
    L = inputs["ffn1_norm"].shape[0]
    b = Builder(ns, S, depth=L)
    nc = b.build()
    hc = {k: v for k, v in host_consts().items() if k.startswith("c_")}
    shared = {}
    for nm, shp in PARAM_SHAPES(L, (L + 1) // 2, L // 2):
        shared[nm] = np.ascontiguousarray(inputs[nm], dtype=np.float32)
    mem = np.ascontiguousarray(inputs["mem"], dtype=np.float32)
    pos = np.ascontiguousarray(inputs["positions"], dtype=np.int32)
    in_maps = []
    for c in range(N_CORES):
        m = dict(shared)
        m.update(hc)
        m["x"] = x[c * ns:(c + 1) * ns].reshape(ns * S, D)
        m["mem"] = mem[c * ns:(c + 1) * ns]
        m["positions"] = pos[c * ns:(c + 1) * ns]
        in_maps.append(m)
    res = run_bass_kernel_spmd(nc, in_maps, core_ids=list(range(N_CORES)))
    out = np.concatenate([r["y"].reshape(ns, S, D) for r in res.results], axis=0)
    return out.astype(np.float32)


def _even_phase(self, xin, xout, e, l):
    S, A = self.S_, self.arena
    PS, bPS = self.PS, self.bPS
    I = self.I
    ns, SQ, TPS = self.n_seq, self.S, self.TPS
    ntiles = self.NT // TT
    NB = SQ // 128
    SCALE = 96.0 ** -0.5
    if not hasattr(self, "ev_scr"):
        self.ev_scr = dict(
            AT=self.scratch("s_at", [ntiles, 128, 4 * TT], BF16),
            QN=self.scratch("s_qn", [ntiles, 128, 4 * TT], BF16),
            QR=self.scratch("s_qr", [ntiles, 64, 4 * TT], BF16),
            KN=self.scratch("s_kn", [ns, 128, 4, SQ], BF16),
            KR=self.scratch("s_kr", [ns, 64, SQ], BF16),
            V=self.scratch("s_v", [ns, 128, NB, 512], BF16),
        )
    scr = self.ev_scr

    m0 = A.mark()
    stage = self.new_stage(A, 2)
    gmix, bgmix = self.load_col(A, I["mix_norm"][l], 8)
    WIN = A.alloc([128, 8, 1472], BF16)
    bWIN = Buf("win")
    w_in = I["ev_w_in"][e]
    for k in range(8):
        self.load_rows(stage, WIN[:, k, 0:1440], bWIN, w_in[k * 128:(k + 1) * 128, :], gmix[:, k:k + 1], bgmix)
        self.load_rows(stage, WIN[:, k, 1440:1472], bWIN, w_in[k * 128:(k + 1) * 128, 1408:1440],
                       gmix[:, k:k + 1], bgmix)
    gqa, bgqa = self.load_col(A, I["ev_q_a_norm"][e], 2)
    WQN = A.alloc([128, 2, 4, 128], BF16)
    WQR = A.alloc([128, 2, 4, 64], BF16)
    bWQ = Buf("wqb")
    for c in range(2):
        sv, sb = self.stage_load(stage, I["ev_w_q_b"][e][c * 128:(c + 1) * 128, :])
        svh = sv.rearrange("p (h e) -> p h e", e=96)
        self.ts(WQN[:, c].rearrange("p a b -> p (a b)").rearrange("p (h e) -> p h e", e=64), svh[:, :, 0:64],
                gqa[:, c:c + 1], None, ALU.mult, None, (sb, bgqa), (bWQ,))
        self.ts(WQR[:, c].rearrange("p a b -> p (a b)").rearrange("p (h e) -> p h e", e=32), svh[:, :, 64:96],
                gqa[:, c:c + 1], None, ALU.mult, None, (sb, bgqa), (bWQ,))
    gkva, bgkva = self.load_col(A, I["ev_kv_a_norm"][e], 1)
    WKN = A.alloc([128, 4, 128], BF16)
    WVV = A.alloc([128, 512], BF16)
    bWKV = Buf("wkvb")
    sv, sb = self.stage_load(stage, I["ev_w_kv_b"][e])
    svh = sv.rearrange("p (h e) -> p h e", e=128)
    self.ts(WKN.rearrange("p a b -> p (a b)").rearrange("p (h e) -> p h e", e=64), svh[:, :, 0:64],
            gkva[:, 0:1], None, ALU.mult, None, (sb, bgkva), (bWKV,))
    self.ts(WVV.rearrange("p (h e) -> p h e", e=64), svh[:, :, 64:128],
            gkva[:, 0:1], None, ALU.mult, None, (sb, bgkva), (bWKV,))
    ident, bid0 = self.load_const(A, self.c["c_ident"], [128, 128], BF16)
    id32 = A.alloc([128, 128], F32)
    bid32 = Buf("id32")
    self.cp("dve", id32, ident, (bid0,), (bid32,))
    CW = A.alloc([128, 4, 31], F32)
    bCW = Buf("cw")
    for cc in range(4):
        self.dma("sp", CW[:, cc, :], I["ev_conv_w"][e][:, cc * 128:(cc + 1) * 128].rearrange("j p -> p j"),
                 writes=(bCW,), stream="misc", ring=4, allow_slow_non_contiguous=True)
    DIAG = A.alloc([128, 4, 31, 128], BF16)
    bDG = Buf("diag")
    for cc in range(4):
        for j in range(31):
            if (cc * 31 + j) % 2 == 0:
                self.ts(DIAG[:, cc, j, :], id32, CW[:, cc, j:j + 1], None, ALU.mult, None, (bid32, bCW), (bDG,))
            else:
                self.act(DIAG[:, cc, j, :], id32, AF.Copy, (bid32, bCW), (bDG,), scale=CW[:, cc, j:j + 1])
    cb, bcb = self.load_col(A, I["ev_conv_b"][e], 4)
    lng, blng = self.load_col(A, I["ev_conv_ln_g"][e], 4)
    lnb, blnb = self.load_col(A, I["ev_conv_ln_b"][e], 4)
    gqn, bgqn = self.load_col_rep(A, I["ev_q_nope_norm"][e], 64, 2, SCALE)
    gkn, bgkn = self.load_col_rep(A, I["ev_k_nope_norm"][e], 64, 2)
    gqr, bgqr = self.load_col_rep(A, I["ev_q_rope_norm"][e], 32, 2, SCALE)
    gkr, bgkr = self.load_col_rep(A, I["ev_k_rope_norm"][e], 32, 2)
    ones, bones = self.load_const(A, self.c["c_ones"], [128, 128], BF16)
    bd64, bbd64 = self.load_const(A, self.c["c_bd64"], [128, 128], BF16)
    bd32, bbd32 = self.load_const(A, self.c["c_bd32"], [64, 64], BF16)
    rot, brot = self.load_const(A, self.c["c_rot"], [64, 64], F32)
    C = self.norm_ctx(A, use_ln=True)
    self.temp_pool(A, 8, 6)
    ABF = [(A.alloc([128, 30 + TT], BF16), Buf("abf")) for _ in range(4)]
    Y32 = [(A.alloc([128, TT], F32), Buf("y32")) for _ in range(4)]
    YBF = [(A.alloc([128, TT], BF16), Buf("ybf")) for _ in range(4)]
    YSQ = [(A.alloc([128, TT], BF16), Buf("ysq")) for _ in range(4)]
    M32 = (A.alloc([128, TT], F32), Buf("m32"))
    ATt = [(A.alloc([128, 4, TT], BF16), Buf("at")) for _ in range(2)]
    ZQN = [(A.alloc([128, TT], BF16), Buf("zqn")) for _ in range(2)]
    QNt = [(A.alloc([128, 4, TT], BF16), Buf("qnt")) for _ in range(2)]
    QRt = [(A.alloc([128, 4, TT], BF16), Buf("qrt")) for _ in range(2)]
    ZKVN = (A.alloc([128, TT], BF16), Buf("zkvn"))
    KNt = [(A.alloc([128, 4, TT], BF16), Buf("knt")) for _ in range(2)]
    VTt = [(A.alloc([128, 4, 512], BF16), Buf("vtt")) for _ in range(2)]
    KRt = [(A.alloc([128, TT], BF16), Buf("krt")) for _ in range(2)]
    CT = [(A.alloc([128, TT], F32), Buf("ct")) for _ in range(2)]
    ST = [(A.alloc([128, TT], F32), Buf("st")) for _ in range(2)]
    RN = (A.alloc([128, TT], F32), Buf("rn"))

    def src(t):
        return lambda j: xin[t * TT + j * 128:t * TT + (j + 1) * 128, :]

    def rope64(ps, bps, gcol, bgcol, ct, bct, st, bst, out, bout):
        X, bX = self.t32()
        Q, bQ = self.t16()
        self.act(X[0:64, :], ps, AF.Copy, (bps,), (bX,))
        self.act(Q[0:64, :], ps, AF.Square, (bps,), (bQ,))
        self.mm(PS[7][0:64, :], bd32, Q[0:64, :], True, True, (bbd32, bQ), (bPS[7],))
        Rr, bR = self.rstd_fm(PS[7][0:64, :], bPS[7], 64, 1.0 / 32)
        rn, brn = RN
        self.stt(rn[0:64, :], X[0:64, :], gcol[0:64, 0:1], Rr[0:64, :], ALU.mult, ALU.mult, (bX, bR, bgcol), (brn,))
        self.mm(PS[7][0:64, :], rot, rn[0:64, :], True, True, (brot, brn), (bPS[7],))
        T1, bT1 = self.t32()
        T2, bT2 = self.t32()
        self.tt("pool", T1[0:64, :], rn[0:64, :], ct[0:64, :], ALU.mult, (brn, bct), (bT1,))
        self.tt("dve", T2[0:64, :], PS[7][0:64, :], st[0:64, :], ALU.mult, (bPS[7], bst), (bT2,))
        self.tt("pool", out, T1[0:64, :], T2[0:64, :], ALU.add, (bT1, bT2), (bout,))

    def e1_tile(t, slot):
        s, ti = t // TPS, t % TPS
        c0 = ti * TT
        ht, bht = C["HT"][slot % 2]
        ct, bct = CT[t % 2]
        st, bst = ST[t % 2]
        self.dma("sp", ct[0:64, :], self.CM[s, :, c0:c0 + TT], writes=(bct,), stream="tab", ring=4)
        self.dma("sp", st[0:64, :], self.SM[s, :, c0:c0 + TT], writes=(bst,), stream="tab", ring=4)
        for cc in range(4):
            ab, bab = ABF[cc]
            if ti == 0:
                self.memset("pool", ab[:, 0:30], 0.0, (bab,))
            pv, pg = cc % 2, 2 + cc % 2
            for k in range(8):
                self.mm(PS[pv], WIN[:, k, cc * 128:(cc + 1) * 128], ht[:, k, :], k == 0, k == 7, (bWIN, bht), (bPS[pv],))
            for k in range(8):
                self.mm(PS[pg], WIN[:, k, 512 + cc * 128:512 + (cc + 1) * 128], ht[:, k, :], k == 0, k == 7,
                        (bWIN, bht), (bPS[pg],))
            T1, bT1 = self.t32()
            self.act(T1, PS[pg], AF.Exp, (bPS[pg],), (bT1,), scale=-1.0)
            self.ts(T1, T1, 1.0, None, ALU.add, None, (bT1,), (bT1,))
            self.recip(T1, T1, (bT1,), (bT1,))
            self.tt("dve", ab[:, 30:30 + TT], PS[pv], T1, ALU.mult, (bPS[pv], bT1), (bab,))
        for cc in range(4):
            ab, bab = ABF[cc]
            py = 4 + cc % 2
            for j in range(31):
                self.mm(PS[py], DIAG[:, cc, j, :], ab[:, j:j + TT], j == 0, j == 30, (bDG, bab), (bPS[py],))
            y32, by32 = Y32[cc]
            self.ts(y32, PS[py], cb[:, cc:cc + 1], None, ALU.add, None, (bPS[py], bcb), (by32,))
            ybf, bybf = YBF[cc]
            ysq, bysq = YSQ[cc]
            self.cp("pool", ybf, y32, (by32,), (bybf,))
            self.tt("pool", ysq, y32, y32, ALU.mult, (by32,), (bysq,))
            self.cp("pool", ab[:, 0:30], ab[:, TT:TT + 30], (bab,), (bab,))
        for cc in range(4):
            self.mm(PS[0], ones, YBF[cc][0], cc == 0, cc == 3, (bones, YBF[cc][1]), (bPS[0],))
        for cc in range(4):
            self.mm(PS[1], ones, YSQ[cc][0], cc == 0, cc == 3, (bones, YSQ[cc][1]), (bPS[1],))
        m32, bm32 = M32
        self.act(m32, PS[0], AF.Copy, (bPS[0],), (bm32,), scale=1.0 / 512)
        V, bV = self.t32()
        self.tt("pool", V, m32, m32, ALU.mult, (bm32,), (bV,))
        self.stt(V, PS[1], 1.0 / 512, V, ALU.mult, ALU.subtract, (bPS[1], bV), (bV,))
        self.act(V, V, AF.Ln, (bV,), (bV,), bias=EPS)
        self.act(V, V, AF.Exp, (bV,), (bV,), scale=-0.5)
        at, bat = ATt[t % 2]
        for cc in range(4):
            y32, by32 = Y32[cc]
            Z, bZ = self.t32()
            self.tt("pool", Z, y32, m32, ALU.subtract, (by32, bm32), (bZ,))
            self.tt("pool", Z, Z, V, ALU.mult, (bZ, bV), (bZ,))
            self.ts(Z, Z, lng[:, cc:cc + 1], lnb[:, cc:cc + 1], ALU.mult, ALU.add, (bZ, blng, blnb), (bZ,))
            E_, bE = self.t32()
            self.act(E_, Z, AF.Exp, (bZ,), (bE,), scale=-1.0)
            self.ts(E_, E_, 1.0, None, ALU.add, None, (bE,), (bE,))
            self.recip(E_, E_, (bE,), (bE,))
            self.tt("pool", at[:, cc, :], Z, E_, ALU.mult, (bZ, bE), (bat,))
        self.dma("sp", scr["AT"][t].rearrange("p (a b) -> p a b", a=4), at, reads=(bat,), stream="est", ring=4)
        srcs = []
        for c in range(2):
            for k in range(8):
                self.mm(PS[c], WIN[:, k, 1024 + c * 128:1024 + (c + 1) * 128], ht[:, k, :], k == 0, k == 7,
                        (bWIN, bht), (bPS[c],))
            srcs.append((PS[c], bPS[c]))
        self.fm_norm(srcs, 128, ones, bones, 2, 1.0 / 256, [(ZQN[0][0], ZQN[0][1]), (ZQN[1][0], ZQN[1][1])])
        qnt, bqnt = QNt[t % 2]
        for pr in range(4):
            b = pr % 2
            for c in range(2):
                self.mm(PS[b], WQN[:, c, pr, :], ZQN[c][0], c == 0, c == 1, (bWQ, ZQN[c][1]), (bPS[b],))
            self.fm_norm([(PS[b], bPS[b])], 128, bd64, bbd64, 2 + pr % 2, 1.0 / 64, [(qnt[:, pr, :], bqnt)],
                         gqn[:, 0:1], bgqn)
        qrt, bqrt = QRt[t % 2]
        for pr in range(4):
            b = 4 + pr % 2
            for c in range(2):
                self.mm(PS[b][0:64, :], WQR[:, c, pr, :], ZQN[c][0], c == 0, c == 1, (bWQ, ZQN[c][1]), (bPS[b],))
            rope64(PS[b][0:64, :], bPS[b], gqr, bgqr, ct, bct, st, bst, qrt[0:64, pr, :], bqrt)
        self.dma("sp", scr["QN"][t].rearrange("p (a b) -> p a b", a=4), qnt, reads=(bqnt,), stream="est", ring=4)
        self.dma("sp", scr["QR"][t].rearrange("p (a b) -> p a b", a=4), qrt[0:64], reads=(bqrt,), stream="est", ring=4)
        for k in range(8):
            self.mm(PS[0], WIN[:, k, 1280:1408], ht[:, k, :], k == 0, k == 7, (bWIN, bht), (bPS[0],))
        zk, bzk = ZKVN
        self.fm_norm([(PS[0], bPS[0])], 128, ones, bones, 2, 1.0 / 128, [(zk, bzk)])
        knt, bknt = KNt[t % 2]
        for pr in range(4):
            b = pr % 2
            self.mm(PS[b], WKN[:, pr, :], zk, True, True, (bWKV, bzk), (bPS[b],))
            self.fm_norm([(PS[b], bPS[b])], 128, bd64, bbd64, 2 + pr % 2, 1.0 / 64, [(knt[:, pr, :], bknt)],
                         gkn[:, 0:1], bgkn)
        self.dma("sp", scr["KN"][s, :, :, c0:c0 + TT], knt, reads=(bknt,), stream="est", ring=4)
        vtt, bvtt = VTt[t % 2]
        for j in range(4):
            b = 4 + j % 2
            self.mm(PS[b], zk[:, j * 128:(j + 1) * 128], WVV, True, True, (bWKV, bzk), (bPS[b],))
            self.cp("act" if j % 2 else "dve", vtt[:, j, :], PS[b], (bPS[b],), (bvtt,))
        self.dma("sp", scr["V"][s, :, ti * 4:ti * 4 + 4, :], vtt, reads=(bvtt,), stream="est", ring=4)
        for k in range(8):
            self.mm(PS[6][0:64, :], WIN[:, k, 1408:1472], ht[:, k, :], k == 0, k == 7, (bWIN, bht), (bPS[6],))
        krt, bkrt = KRt[t % 2]
        rope64(PS[6][0:64, :], bPS[6], gkr, bgkr, ct, bct, st, bst, krt[0:64, :], bkrt)
        self.dma("sp", scr["KR"][s, :, c0:c0 + TT], krt[0:64, :], reads=(bkrt,), stream="est", ring=4)

    self.norm1(C, 0, src(0))
    self.norm2(C, 0)
    for t in range(ntiles):
        if t + 1 < ntiles:
            self.norm1(C, t + 1, src(t + 1))
        e1_tile(t, t)
        if t + 1 < ntiles:
            self.norm2(C, t + 1)
    S.barrier()
    A.reset(m0)

    m0 = A.mark()
    stage = self.new_stage(A, 2)
    WOC = A.alloc([128, 4, D], BF16)
    WOM = A.alloc([128, 8, D], BF16)
    bWOC, bWOM = Buf("woc"), Buf("wom")
    w_out = I["ev_w_out"][e]
    for cc in range(4):
        self.load_rows(stage, WOC[:, cc, :], bWOC, w_out[cc * 128:(cc + 1) * 128, :])
    for h in range(8):
        self.load_rows(stage, WOM[0:64, h, :], bWOM, w_out[512 + h * 64:512 + (h + 1) * 64, :])
    mask, bmask = self.load_const(A, self.c["c_mask"], [128, 128], BF16)
    sel, bsel = self.load_const(A, self.c["c_sel"], [65, 64], F32)
    R = self.resid_ctx(A)
    KNs = A.alloc([128, 4, SQ], BF16)
    KRs = A.alloc([128, SQ], BF16)
    VA = A.alloc([128, NB, 8, 65], BF16)
    bKN, bKR, bVA = Buf("kns"), Buf("krs"), Buf("va")
    VST = [(A.alloc([128, 8, 512], BF16), Buf("vst")) for _ in range(2)]
    QNl = [(A.alloc([128, 4, TT], BF16), Buf("qnl")) for _ in range(2)]
    QRl = [(A.alloc([128, 4, TT], BF16), Buf("qrl")) for _ in range(2)]
    ATl = [(A.alloc([128, 4, TT], BF16), Buf("atl")) for _ in range(2)]
    PTl = [(A.alloc([128, TT], BF16), Buf("pt")) for _ in range(3)]
    OS = [(A.alloc([128, TT], F32), Buf("os")) for _ in range(2)]
    RD = [(A.alloc([128, TT], F32), Buf("rd")) for _ in range(2)]
    OT = [(A.alloc([128, 8, TT], BF16), Buf("ot")) for _ in range(2)]
    pcount = 0
    for s in range(ns):
        self.dma("sp", KNs, scr["KN"][s], writes=(bKN,), stream="kv", ring=4)
        self.dma("sp", KRs[0:64, :], scr["KR"][s], writes=(bKR,), stream="kv", ring=4)
        self.memset("pool", VA, 1.0, (bVA,))
        for g in range(NB // 8):
            vs, bvs = VST[g % 2]
            self.dma("sp", vs, scr["V"][s, :, g * 8:(g + 1) * 8, :], writes=(bvs,), stream="kv", ring=4)
            self.cp("pool", VA[:, g * 8:(g + 1) * 8, :, 0:64], vs.rearrange("p b (h e) -> p b h e", e=64),
                    (bvs,), (bVA,))
        for ti in range(TPS):
            t = s * TPS + ti
            qn, bqn = QNl[t % 2]
            qr, bqr = QRl[t % 2]
            at, bat = ATl[t % 2]
            ot, bot = OT[t % 2]
            self.dma("sp", qn, scr["QN"][t].rearrange("p (a b) -> p a b", a=4), writes=(bqn,), stream="ql", ring=4)
            self.dma("sp", qr[0:64], scr["QR"][t].rearrange("p (a b) -> p a b", a=4), writes=(bqr,), stream="ql", ring=4)
            self.dma("sp", at, scr["AT"][t].rearrange("p (a b) -> p a b", a=4), writes=(bat,), stream="ql", ring=4)
            nkb = 4 * ti + 4
            for h in range(8):
                pr, hh = h // 2, h % 2
                po = 2 + h % 2
                for kb in range(nkb):
                    n0 = max(0, kb - 4 * ti) * 128
                    pb = pcount % 2
                    p, bp = PTl[pcount % 3]
                    pcount += 1
                    self.mm(PS[pb][:, n0:TT], KNs[hh * 64:(hh + 1) * 64, pr, kb * 128:(kb + 1) * 128],
                            qn[hh * 64:(hh + 1) * 64, pr, n0:TT], True, False, (bKN, bqn), (bPS[pb],))
                    self.mm(PS[pb][:, n0:TT], KRs[hh * 32:(hh + 1) * 32, kb * 128:(kb + 1) * 128],
                            qr[hh * 32:(hh + 1) * 32, pr, n0:TT], False, True, (bKR, bqr), (bPS[pb],))
                    self.act(p[:, n0:TT], PS[pb][:, n0:TT], AF.Exp, (bPS[pb],), (bp,))
                    if kb >= 4 * ti:
                        self.tt("pool", p[:, n0:n0 + 128], p[:, n0:n0 + 128], mask, ALU.mult, (bp, bmask), (bp,))
                    self.mm(PS[po][0:65, n0:TT], VA[:, kb, h, :], p[:, n0:TT], kb == 0, kb == nkb - 1,
                            (bVA, bp), (bPS[po],))
                os_, bos = OS[h % 2]
                rd, brd = RD[h % 2]
                self.cp("act", os_[0:65, :], PS[po][0:65, :], (bPS[po],), (bos,))
                pd = 4 + h % 2
                self.mm(PS[pd][0:64, :], sel, os_[0:65, :], True, True, (bsel, bos), (bPS[pd],))
                self.recip(rd[0:64, :], PS[pd][0:64, :], (bPS[pd],), (brd,))
                self.tt("pool", ot[0:64, h, :], os_[0:64, :], rd[0:64, :], ALU.mult, (bos, brd), (bot,))
            steps = [((lambda j, cc=cc, at=at: at[:, cc, j * 128:(j + 1) * 128]),
                      (lambda hf, cc=cc: WOC[:, cc, hf * 512:(hf + 1) * 512]), (bWOC, bat)) for cc in range(4)]
            steps += [((lambda j, h=h, ot=ot: ot[0:64, h, j * 128:(j + 1) * 128]),
                       (lambda hf, h=h: WOM[0:64, h, hf * 512:(hf + 1) * 512]), (bWOM, bot)) for h in range(8)]
            self.out_proj(R, t, (0, 1, 2, 3), steps, xin, xout, 1.0, banks=(6, 7))
    S.barrier()
    A.reset(m0)


Builder.even_phase = _even_phase


def _odd_phase(self, xin, xout, o, l):
    S, A = self.S_, self.arena
    PS, bPS = self.PS, self.bPS
    I = self.I
    ns, SQ, TPS = self.n_seq, self.S, self.TPS
    ntiles = self.NT // TT
    if not hasattr(self, "od_scr"):
        self.od_scr = dict(
            QT=self.scratch("s_rq", [ntiles, 128, 8 * TT], BF16),
            KT=self.scratch("s_rk", [ntiles, 128, 8 * TT], BF16),
            QX=self.scratch("s_rqx", [ntiles, 128, 8 * TT], BF16),
            V=self.scratch("s_rv", [ntiles, 128, 4 * 2048], BF16),
            SG=self.scratch("s_rsg", [ntiles, 128, 4 * 2048], BF16),
        )
    scr = self.od_scr
    w_in = I["od_w_in"][o]

    m0 = A.mark()
    stage = self.new_stage(A, 2)
    gmix, bgmix = self.load_col(A, I["mix_norm"][l], 8)
    gmk = A.alloc([128, 8], F32)
    bgmk = Buf("gmk")
    self.ts(gmk, gmix, 1.0 / 16.0, None, ALU.mult, None, (bgmix,), (bgmk,))
    WIN = A.alloc([128, 8, 6144], BF16)
    bWIN = Buf("win")
    for k in range(8):
        rows = w_in[k * 128:(k + 1) * 128, :]
        self.load_rows(stage, WIN[:, k, 0:1024], bWIN, rows[:, 0:1024], gmix[:, k:k + 1], bgmix)
        self.load_rows(stage, WIN[:, k, 1024:2048], bWIN, rows[:, 1024:2048], gmk[:, k:k + 1], bgmk)
        self.load_rows(stage, WIN[:, k, 2048:6144], bWIN, rows[:, 2048:6144], gmix[:, k:k + 1], bgmix)
    xi, bxi = self.load_const(A, self.c["c_xi"], [128, 4, 512], F32)
    C = self.norm_ctx(A, use_ln=False)
    self.temp_pool(A, 4, 1)
    CT = [(A.alloc([128, TT], F32), Buf("ct")) for _ in range(1)]
    ST = [(A.alloc([128, TT], F32), Buf("st")) for _ in range(1)]
    QTt = [(A.alloc([128, 8, TT], BF16), Buf("qt")) for _ in range(1)]
    KTt = [(A.alloc([128, 8, TT], BF16), Buf("kt")) for _ in range(1)]
    QXt = [(A.alloc([128, 8, TT], BF16), Buf("qx")) for _ in range(1)]
    Q32 = [(A.alloc([128, TT], F32), Buf("q32")) for _ in range(2)]
    VT = [(A.alloc([128, 2048], BF16), Buf("vt")) for _ in range(2)]
    SGT = [(A.alloc([128, 2048], BF16), Buf("sgt")) for _ in range(2)]

    def src(t):
        return lambda j: xin[t * TT + j * 128:t * TT + (j + 1) * 128, :]

    def r1_tile(t, slot):
        s, ti = t // TPS, t % TPS
        c0 = ti * TT
        ht, bht = C["HT"][slot % 2]
        ct, bct = CT[0]
        st, bst = ST[0]
        self.dma("sp", ct, self.CR[s, :, c0:c0 + TT], writes=(bct,), stream="tab", ring=4)
        self.dma("sp", st, self.SR[s, :, c0:c0 + TT], writes=(bst,), stream="tab", ring=4)
        qt, bqt = QTt[0]
        kt, bkt = KTt[0]
        qx, bqx = QXt[0]
        for which, dst, bdst in ((0, qt, bqt), (1, kt, bkt)):
            for h in range(4):
                col = which * 1024 + h * 256
                for dc in range(2):
                    for k in range(8):
                        self.mm(PS[dc], WIN[:, k, col + dc * 128:col + (dc + 1) * 128], ht[:, k, :], k == 0, k == 7,
                                (bWIN, bht), (bPS[dc],))
                T1, b1 = self.t32()
                T2, b2 = self.t32()
                T3, b3 = self.t32()
                T4, b4 = self.t32()
                self.tt("dve", T1, PS[0], ct, ALU.mult, (bPS[0], bct), (b1,))
                self.tt("dve", T2, PS[1], st, ALU.mult, (bPS[1], bst), (b2,))
                self.tt("dve", T3, PS[0], st, ALU.mult, (bPS[0], bst), (b3,))
                self.tt("dve", T4, PS[1], ct, ALU.mult, (bPS[1], bct), (b4,))
                if which == 0:
                    qa, bqa = Q32[0]
                    qb, bqb = Q32[1]
                    self.tt("pool", qa, T1, T2, ALU.subtract, (b1, b2), (bqa,))
                    self.tt("pool", qb, T3, T4, ALU.add, (b3, b4), (bqb,))
                    self.cp("act", dst[:, 2 * h, :], qa, (bqa,), (bdst,))
                    self.cp("act", dst[:, 2 * h + 1, :], qb, (bqb,), (bdst,))
                    self.tt("pool", qx[:, 2 * h, :], qa, xi[:, h, :], ALU.mult, (bqa, bxi), (bqx,))
                    self.tt("pool", qx[:, 2 * h + 1, :], qb, xi[:, h, :], ALU.mult, (bqb, bxi), (bqx,))
                else:
                    self.tt("pool", dst[:, 2 * h, :], T1, T2, ALU.subtract, (b1, b2), (bdst,))
                    self.tt("pool", dst[:, 2 * h + 1, :], T3, T4, ALU.add, (b3, b4), (bdst,))
        self.dma("sp", scr["QT"][t].rearrange("p (a b) -> p a b", a=8), qt, reads=(bqt,), stream="rst1", ring=4)
        self.dma("sp", scr["KT"][t].rearrange("p (a b) -> p a b", a=8), kt, reads=(bkt,), stream="rst1", ring=4)
        self.dma("sp", scr["QX"][t].rearrange("p (a b) -> p a b", a=8), qx, reads=(bqx,), stream="rst1", ring=4)
        for cj in range(4):
            vt, bvt = VT[cj % 2]
            sg, bsg = SGT[cj % 2]
            for h in range(4):
                b = 2 + h % 2
                for k in range(8):
                    self.mm(PS[b], ht[:, k, cj * 128:(cj + 1) * 128], WIN[:, k, 2048 + h * 512:2048 + (h + 1) * 512],
                            k == 0, k == 7, (bWIN, bht), (bPS[b],))
                self.cp("dve" if h % 2 else "act", vt[:, h * 512:(h + 1) * 512], PS[b], (bPS[b],), (bvt,))
            for h in range(4):
                b = 4 + h % 2
                for k in range(8):
                    self.mm(PS[b], ht[:, k, cj * 128:(cj + 1) * 128], WIN[:, k, 4096 + h * 512:4096 + (h + 1) * 512],
                            k == 0, k == 7, (bWIN, bht), (bPS[b],))
                self.act(sg[:, h * 512:(h + 1) * 512], PS[b], AF.Silu, (bPS[b],), (bsg,))
            self.dma("sp", scr["V"][t, :, cj * 2048:(cj + 1) * 2048], vt, reads=(bvt,), stream="rst2", ring=4)
            self.dma("sp", scr["SG"][t, :, cj * 2048:(cj + 1) * 2048], sg, reads=(bsg,), stream="rst2", ring=4)

    self.norm1(C, 0, src(0))
    self.norm2(C, 0)
    for t in range(ntiles):
        if t + 1 < ntiles:
            self.norm1(C, t + 1, src(t + 1))
        r1_tile(t, t)
        if t + 1 < ntiles:
            self.norm2(C, t + 1)
    S.barrier()
    A.reset(m0)

    m0 = A.mark()
    stage = self.new_stage(A, 2)
    WO = A.alloc([128, 16, D], BF16)
    bWO = Buf("wo")
    for c in range(16):
        self.load_rows(stage, WO[:, c, :], bWO, I["od_w_out"][o][c * 128:(c + 1) * 128, :])
    ident, bid = self.load_const(A, self.c["c_ident"], [128, 128], BF16)
    decay, bdec = self.load_const(A, self.c["c_decay"], [128, 4, 128], F32)
    zeta, bzeta = self.load_const(A, self.c["c_zeta"], [128, 4], F32)
    GNG = A.alloc([128, 2048], F32)
    GNB = A.alloc([128, 2048], F32)
    bGN = Buf("gn")
    self.dma("sp", GNG, I["od_gn_g"][o].rearrange("(o n) -> o n", o=1).partition_broadcast(128), writes=(bGN,),
             stream="misc", ring=4)
    self.dma("sp", GNB, I["od_gn_b"][o].rearrange("(o n) -> o n", o=1).partition_broadcast(128), writes=(bGN,),
             stream="misc", ring=4)
    R = self.resid_ctx(A)
    ST32 = A.alloc([128, 4, 2, 512], F32)
    STB = A.alloc([128, 4, 2, 512], BF16)
    bST = [Buf("st32_%d" % h) for h in range(4)]
    bSTB = [Buf("stb_%d" % h) for h in range(4)]
    QTl = [(A.alloc([128, 8, TT], BF16), Buf("qtl")) for _ in range(2)]
    KTl = [(A.alloc([128, 8, TT], BF16), Buf("ktl")) for _ in range(2)]
    QXl = [(A.alloc([128, 8, TT], BF16), Buf("qxl")) for _ in range(2)]
    Vl = [(A.alloc([128, 2048], BF16), Buf("vl")) for _ in range(2)]
    SGl = [(A.alloc([128, 2048], BF16), Buf("sgl")) for _ in range(2)]
    KZ = [(A.alloc([128, 256], BF16), Buf("kz")) for _ in range(2)]
    PTl = [(A.alloc([128, 128], BF16), Buf("ptl")) for _ in range(2)]
    O32 = [(A.alloc([128, 512], F32), Buf("o32")) for _ in range(3)]
    SQJ = (A.alloc([128, 512], BF16), Buf("sqj"))
    STAT = [(A.alloc([128, 8], F32), Buf("stat")) for _ in range(3)]
    OG = [(A.alloc([128, 2048], BF16), Buf("og")) for _ in range(2)]
    OGT = [(A.alloc([128, 16, TT], BF16), Buf("ogt")) for _ in range(2)]
    cnt = 0
    for s in range(ns):
        for h in range(4):
            self.memset("pool", ST32[:, h], 0.0, (bST[h],))
            self.memset("pool", STB[:, h], 0.0, (bSTB[h],))
        for ti in range(TPS):
            t = s * TPS + ti
            qt, bqt = QTl[t % 2]
            kt, bkt = KTl[t % 2]
            qx, bqx = QXl[t % 2]
            ogt, bogt = OGT[t % 2]
            self.dma("sp", qt, scr["QT"][t].rearrange("p (a b) -> p a b", a=8), writes=(bqt,), stream="rl", ring=4)
            self.dma("sp", kt, scr["KT"][t].rearrange("p (a b) -> p a b", a=8), writes=(bkt,), stream="rl", ring=4)
            self.dma("sp", qx, scr["QX"][t].rearrange("p (a b) -> p a b", a=8), writes=(bqx,), stream="rl", ring=4)
            for cj in range(4):
                cs = slice(cj * 128, (cj + 1) * 128)
                vl, bvl = Vl[cj % 2]
                sg, bsg = SGl[cj % 2]
                og, bog = OG[cj % 2]
                self.dma("sp", vl, scr["V"][t, :, cj * 2048:(cj + 1) * 2048], writes=(bvl,), stream="rl2", ring=4)
                self.dma("sp", sg, scr["SG"][t, :, cj * 2048:(cj + 1) * 2048], writes=(bsg,), stream="rl2", ring=4)
                for h in range(4):
                    cnt += 1
                    pz = 6 + cnt % 2
                    ptz = PS[pz].bitcast(BF16)
                    for dc in range(2):
                        self.S_.op("pe", lambda eng, ptz=ptz, kt=kt, h=h, dc=dc, cs=cs: eng.transpose(
                            out=ptz[:, dc * 128:(dc + 1) * 128], in_=kt[:, 2 * h + dc, cs], identity=ident),
                            reads=(bkt, bid), writes=(bPS[pz],))
                    kz, bkz = KZ[cnt % 2]
                    self.act(kz, ptz[:, 0:256], AF.Copy, (bPS[pz], bzeta), (bkz,), scale=zeta[:, h:h + 1])
                    pb = 0 + cnt % 2
                    for dc in range(2):
                        self.mm(PS[pb][:, 0:128], kt[:, 2 * h + dc, cs], qt[:, 2 * h + dc, cs], dc == 0, dc == 1,
                                (bkt, bqt), (bPS[pb],))
                    p, bp = PTl[cnt % 2]
                    self.tt("dve", p, PS[pb][:, 0:128], decay[:, h, :], ALU.mult, (bPS[pb], bdec), (bp,))
                    po = 2 + cnt % 2
                    self.mm(PS[po], p, vl[:, h * 512:(h + 1) * 512], True, False, (bp, bvl), (bPS[po],))
                    for dc in range(2):
                        self.mm(PS[po], qx[:, 2 * h + dc, cs], STB[:, h, dc, :], False, dc == 1, (bqx, bSTB[h]),
                                (bPS[po],))
                    for dc in range(2):
                        pu = 4 + dc
                        self.mm(PS[pu], kz[:, dc * 128:(dc + 1) * 128], vl[:, h * 512:(h + 1) * 512], True, True,
                                (bkz, bvl), (bPS[pu],))
                        self.stt(ST32[:, h, dc, :], ST32[:, h, dc, :], float(self.gch[h]), PS[pu], ALU.mult, ALU.add,
                                 (bPS[pu], bST[h]), (bST[h],))
                    self.cp("pool", STB[:, h], ST32[:, h], (bST[h],), (bSTB[h],))
                    o32, bo32 = O32[cnt % 3]
                    stt_, bstat = STAT[cnt % 3]
                    sq, bsq = SQJ
                    self.act(o32, PS[po], AF.Copy, (bPS[po],), (bo32, bstat), accum_out=stt_[:, 0:1])
                    self.act(sq, PS[po], AF.Square, (bPS[po],), (bsq, bstat), accum_out=stt_[:, 1:2])
                    self.ts(stt_[:, 2:3], stt_[:, 0:1], 1.0 / 512, None, ALU.mult, None, (bstat,), (bstat,))
                    self.tt("dve", stt_[:, 3:4], stt_[:, 2:3], stt_[:, 2:3], ALU.mult, (bstat,), (bstat,))
                    self.stt(stt_[:, 4:5], stt_[:, 1:2], 1.0 / 512, stt_[:, 3:4], ALU.mult, ALU.subtract,
                             (bstat,), (bstat,))
                    self.ts(stt_[:, 4:5], stt_[:, 4:5], EPS, None, ALU.add, None, (bstat,), (bstat,))
                    self.act(stt_[:, 5:6], stt_[:, 4:5], AF.Sqrt, (bstat,), (bstat,))
                    self.recip(stt_[:, 6:7], stt_[:, 5:6], (bstat,), (bstat,))
                    self.ts(o32, o32, stt_[:, 2:3], stt_[:, 6:7], ALU.subtract, ALU.mult, (bo32, bstat), (bo32,))
                    self.tt("pool", o32, o32, GNG[:, h * 512:(h + 1) * 512], ALU.mult, (bo32, bGN), (bo32,))
                    self.tt("pool", o32, o32, GNB[:, h * 512:(h + 1) * 512], ALU.add, (bo32, bGN), (bo32,))
                    self.tt("pool", og[:, h * 512:(h + 1) * 512], o32, sg[:, h * 512:(h + 1) * 512], ALU.mult,
                            (bo32, bsg), (bog,))
                for g in range(2):
                    pz = 6 + g
                    ptz = PS[pz].bitcast(BF16).rearrange("p (c t) -> p c t", c=8)
                    for c in range(8):
                        self.S_.op("pe", lambda eng, ptz=ptz, og=og, c=c, g=g: eng.transpose(
                            out=ptz[:, c, :], in_=og[:, (g * 8 + c) * 128:(g * 8 + c + 1) * 128], identity=ident),
                            reads=(bog, bid), writes=(bPS[pz],))
                    self.cp("dve" if g else "act", ogt[:, g * 8:(g + 1) * 8, cs], ptz, (bPS[pz],), (bogt,))
            steps = [((lambda j, c=c, ogt=ogt: ogt[:, c, j * 128:(j + 1) * 128]),
                      (lambda hf, c=c: WO[:, c, hf * 512:(hf + 1) * 512]), (bWO, bogt)) for c in range(16)]
            self.out_proj(R, t, (0, 1, 2, 3), steps, xin, xout, 1.0, banks=(4, 5))
    S.barrier()
    A.reset(m0)


Builder.odd_phase = _odd_phase
```

```python
import numpy as np
import concourse.bass as bass
import concourse.mybir as mybir
from concourse.bass_utils import run_bass_kernel_spmd

F32 = mybir.dt.float32
BF16 = mybir.dt.bfloat16
I32 = mybir.dt.int32
U8 = mybir.dt.uint8
ALU = mybir.AluOpType
AF = mybir.ActivationFunctionType
AX = mybir.AxisListType

ENGS = ("pe", "act", "dve", "pool", "sp")
EPOCH = 20000
DT_SIZE = {F32: 4, BF16: 2, I32: 4, U8: 1}


class Buf:
    __slots__ = ("name", "w", "r")

    def __init__(self, name=""):
        self.name = name
        self.w = None
        self.r = {}


class Sched:
    def __init__(self, nc):
        self.nc = nc
        self.eobj = {"pe": nc.tensor, "act": nc.scalar, "dve": nc.vector,
                     "pool": nc.gpsimd, "sp": nc.sync}
        self.ops = {e: [] for e in ENGS}
        self.streams = {}
        self.sem_handles = {}
        self.nsem = 0

    def _sem(self, key):
        h = self.sem_handles.get(key)
        if h is None:
            h = self.nc.alloc_semaphore("s%d" % self.nsem)
            self.nsem += 1
            self.sem_handles[key] = h
        return h

    @staticmethod
    def _add(deps, key, val):
        if deps.get(key, -1) < val:
            deps[key] = val

    def _deps(self, e, reads, writes):
        deps = {}
        for b in reads:
            if b.w is not None:
                self._add(deps, *b.w)
        for b in writes:
            if b.w is not None:
                self._add(deps, *b.w)
            for k, v in b.r.items():
                self._add(deps, k, v)
        if e == "pe":
            deps.pop(("e", "pe"), None)
        return deps

    def _mark(self, me, reads, writes):
        for b in reads:
            if b.r.get(me[0], -1) < me[1]:
                b.r[me[0]] = me[1]
        for b in writes:
            b.w = me
            b.r = {}

    def op(self, e, fn, reads=(), writes=()):
        idx = len(self.ops[e])
        deps = self._deps(e, reads, writes)
        self.ops[e].append([fn, deps, None])
        self._mark((("e", e), idx), reads, writes)

    def dma(self, q, out_ap, in_ap, reads=(), writes=(), stream="ld", ring=6, **kw):
        st = self.streams.setdefault(stream, [0, ring])
        i = st[0]
        st[0] += 1
        slot = i % st[1]
        gen = i // st[1]
        key = ("d", stream, slot)
        deps = self._deps(q, reads, writes)
        if gen > 0:
            self._add(deps, key, 16 * gen)

        def fn(eng, out_ap=out_ap, in_ap=in_ap, kw=kw):
            return eng.dma_start(out=out_ap, in_=in_ap, **kw)
        self.ops[q].append([fn, deps, key])
        self._mark((key, 16 * (gen + 1)), reads, writes)

    def barrier(self):
        last = {}
        for e in ENGS:
            for i in range(len(self.ops[e]) - 1, -1, -1):
                if self.ops[e][i][2] is None:
                    last[("e", e)] = i
                    break
        for name, (cnt, ring) in self.streams.items():
            for slot in range(min(cnt, ring)):
                n = (cnt - 1 - slot) // ring + 1
                last[("d", name, slot)] = 16 * n
        for e in ENGS:
            d = dict(last)
            if e == "pe":
                d.pop(("e", "pe"), None)
            self.ops[e].append([lambda eng: eng.nop(), d, None])

    def finalize(self):
        nc = self.nc
        need = {e: set() for e in ENGS}
        for e in ENGS:
            for fn, deps, dk in self.ops[e]:
                for k, v in deps.items():
                    if k[0] == "e":
                        need[k[1]].add(v)
        rank = {}
        for e in ENGS:
            r = 0
            for i in sorted(need[e]):
                rank[(e, i)] = r
                r += 1

        def resolve(k, v):
            if k[0] == "e":
                r = rank[(k[1], v)]
                return self._sem(("e", k[1], r // EPOCH)), r % EPOCH + 1
            return self._sem(k), v

        for e in ENGS:
            for fn, deps, dk in self.ops[e]:
                for k, v in deps.items():
                    resolve(k, v)
                if dk is not None:
                    self._sem(dk)

        def emit(e, eng):
            seen = {}
            for i, (fn, deps, dk) in enumerate(self.ops[e]):
                for k, v in deps.items():
                    sem, val = resolve(k, v)
                    sk = id(sem)
                    if seen.get(sk, -1) >= val:
                        continue
                    seen[sk] = val
                    eng.wait_ge(sem, val)
                ins = fn(eng)
                if dk is not None:
                    ins.then_inc(self._sem(dk), 16)
                elif (e, i) in rank:
                    r = rank[(e, i)]
                    ins.then_inc(self._sem(("e", e, r // EPOCH)), 1)

        with nc.Block() as block:
            @block.tensor
            def _(eng):
                emit("pe", eng)

            @block.scalar
            def _(eng):
                emit("act", eng)

            @block.vector
            def _(eng):
                emit("dve", eng)

            @block.gpsimd
            def _(eng):
                emit("pool", eng)

            @block.sync
            def _(eng):
                emit("sp", eng)
        for h in self.sem_handles.values():
            nc.gpsimd.sem_clear(h)
        nc.all_engine_barrier()


class Arena:
    def __init__(self, nc, nbytes):
        self.t = nc.alloc_sbuf_tensor("arena", [128, nbytes], U8)
        self.n = nbytes
        self.off = 0
        self.marks = []

    def alloc(self, shape, dt):
        n = int(np.prod(shape[1:])) * DT_SIZE[dt]
        n_al = (n + 63) // 64 * 64
        assert self.off + n_al <= self.n, ("SBUF arena overflow", self.off, n_al, self.n)
        v = self.t[0:shape[0], self.off:self.off + n].bitcast(dt)
        self.off += n_al
        if len(shape) == 3:
            v = v.rearrange("p (a b) -> p a b", a=shape[1])
        elif len(shape) == 4:
            v = v.rearrange("p (a b c) -> p a b c", a=shape[1], b=shape[2])
        return v

    def mark(self):
        return self.off

    def reset(self, m):
        self.off = m


D = 1024
DFF = 2816
NFC = DFF // 128
EPS = 1e-6
TT = 512
MEM = 256
TWO_PI = 6.283184


def host_consts():
    import ml_dtypes
    bf = ml_dtypes.bfloat16
    c = {}
    c["c_ident"] = np.eye(128, dtype=np.float32).astype(bf)
    c["c_ones"] = np.ones((128, 128), np.float32).astype(bf)
    bd64 = np.zeros((128, 128), np.float32)
    bd64[:64, :64] = 1
    bd64[64:, 64:] = 1
    c["c_bd64"] = bd64.astype(bf)
    bd32 = np.zeros((64, 64), np.float32)
    bd32[:32, :32] = 1
    bd32[32:, 32:] = 1
    c["c_bd32"] = bd32.astype(bf)
    rot = np.zeros((64, 64), np.float32)
    for m in range(64):
        if m % 32 < 16:
            rot[m + 16, m] = -1.0
        else:
            rot[m - 16, m] = 1.0
    c["c_rot"] = rot
    sel = np.zeros((65, 64), np.float32)
    sel[64, :] = 1.0
    c["c_sel"] = sel
    mask = np.zeros((128, 128), np.float32)
    for j in range(128):
        mask[j, j:] = 1.0
    c["c_mask"] = mask.astype(bf)
    inv_m = (10000.0 ** (-np.arange(0, 32, 2, dtype=np.float32) / 32)).astype(np.float32)
    inv_r = (10000.0 ** (-np.arange(0, 256, 2, dtype=np.float32) / 256)).astype(np.float32)
    tab = np.zeros((128, 2), np.float32)
    tab[:, 0] = inv_m[np.arange(128) % 16] / (2 * np.pi)
    tab[:, 1] = inv_r / (2 * np.pi)
    c["c_inv"] = tab
    H = 4
    log_g = np.log1p(-np.exp2(-5.0 - np.arange(H, dtype=np.float64)))
    idx = np.arange(128, dtype=np.float64)
    diff = idx[None, :] - idx[:, None]
    dec = np.where(diff >= 0, np.exp(log_g[:, None, None] * np.maximum(diff, 0.0)), 0.0)
    c["c_decay"] = np.ascontiguousarray(dec.transpose(1, 0, 2)).astype(np.float32)
    xi = np.exp(log_g[:, None] * (idx + 1.0))
    c["c_xi"] = np.tile(xi[None, :, None, :], (128, 1, 4, 1)).reshape(128, 4, 512).astype(np.float32)
    zeta = np.exp(log_g[:, None] * (128 - 1.0 - idx))
    c["c_zeta"] = np.ascontiguousarray(zeta.T).astype(np.float32)
    c["_gchunk"] = [float(np.exp(log_g[h] * 128)) for h in range(H)]
    return c


class Builder:
    def __init__(self, n_seq, S, depth=4, phases=None):
        self.n_seq, self.S, self.depth = n_seq, S, depth
        self.NT = n_seq * S
        self.TPS = S // TT
        self.phases = phases
        nc = bass.Bass("TRN2", target_bir_lowering=False)
        self.nc = nc
        self.S_ = Sched(nc)
        self.arena = Arena(nc, 212000)
        self.PS = [nc.alloc_psum_tensor("ps%d" % i, [128, 512], F32)[:, :] for i in range(8)]
        self.bPS = [Buf("ps%d" % i) for i in range(8)]
        self.cast_rr = 0
        self.stage_i = 0
        self.gch = host_consts()["_gchunk"]

    def inp(self, name, shape, dt=F32):
        return self.nc.dram_tensor(name, list(shape), dt, kind="ExternalInput").ap()

    def scratch(self, name, shape, dt):
        return self.nc.dram_tensor(name, list(shape), dt).ap()

    def mm(self, out, lhsT, rhs, start, stop, reads, writes):
        self.S_.op("pe", lambda eng: eng.matmul(out=out, lhsT=lhsT, rhs=rhs, start=start, stop=stop),
                   reads, writes)

    def act(self, out, in_, func, reads, writes, **kw):
        self.S_.op("act", lambda eng: eng.activation(out=out, in_=in_, func=func, **kw), reads, writes)

    def tt(self, e, out, in0, in1, op, reads, writes):
        self.S_.op(e, lambda eng: eng.tensor_tensor(out=out, in0=in0, in1=in1, op=op), reads, writes)

    def ts(self, out, in0, s1, s2, op0, op1, reads, writes, e="dve"):
        if s2 is None:
            self.S_.op(e, lambda eng: eng.tensor_scalar(out=out, in0=in0, scalar1=s1, scalar2=None, op0=op0),
                       reads, writes)
        else:
            self.S_.op(e, lambda eng: eng.tensor_scalar(out=out, in0=in0, scalar1=s1, scalar2=s2,
                                                        op0=op0, op1=op1), reads, writes)

    def stt(self, out, in0, scalar, in1, op0, op1, reads, writes, e="dve"):
        self.S_.op(e, lambda eng: eng.scalar_tensor_tensor(out=out, in0=in0, scalar=scalar, in1=in1,
                                                           op0=op0, op1=op1), reads, writes)

    def cp(self, e, out, in_, reads, writes):
        if e == "act":
            self.S_.op(e, lambda eng: eng.copy(out=out, in_=in_), reads, writes)
        else:
            self.S_.op(e, lambda eng: eng.tensor_copy(out=out, in_=in_), reads, writes)

    def recip(self, out, in_, reads, writes):
        self.S_.op("dve", lambda eng: eng.reciprocal(out=out, in_=in_), reads, writes)

    def memset(self, e, out, val, writes):
        self.S_.op(e, lambda eng: eng.memset(out, val), (), writes)

    def dma(self, q, out, in_, reads=(), writes=(), stream="ld", ring=4, **kw):
        self.S_.dma(q, out, in_, reads=reads, writes=writes, stream=stream, ring=ring, **kw)

    def cast_op(self, out, in_, reads, writes, scale_ap=None):
        S = self.S_
        if scale_ap is None:
            e = ("dve", "pool", "act")[self.cast_rr % 3]
        else:
            e = ("dve", "act")[self.cast_rr % 2]
        self.cast_rr += 1
        if scale_ap is None:
            self.cp(e, out, in_, reads, writes)
        elif e == "act":
            self.act(out, in_, AF.Copy, reads, writes, scale=scale_ap)
        else:
            self.ts(out, in_, scale_ap, None, ALU.mult, None, reads, writes)

    def new_stage(self, A, n=2, cols=1024):
        return [(A.alloc([128, cols], F32), Buf("stg")) for _ in range(n)]

    def stage_load(self, stage, src):
        k = self.stage_i
        self.stage_i += 1
        sb, sbuf = stage[k % len(stage)]
        p, n = src.shape
        q = ("sp", "pool")[k % 2]
        self.dma(q, sb[0:p, 0:n], src, writes=(sbuf,), stream="wld", ring=4)
        return sb[0:p, 0:n], sbuf

    def load_rows(self, stage, dst, dbuf, src, gain=None, gbuf=None):
        p, n = src.shape
        cols = stage[0][0].shape[1]
        for c0 in range(0, n, cols):
            w = min(cols, n - c0)
            sv, sbuf = self.stage_load(stage, src[:, c0:c0 + w])
            rd = (sbuf,) if gain is None else (sbuf, gbuf)
            self.cast_op(dst[:, c0:c0 + w], sv, rd, (dbuf,), gain)

    def load_const(self, A, dram, shape, dt):
        t = A.alloc(list(shape), dt)
        b = Buf("const")
        self.dma("sp", t, dram, writes=(b,), stream="misc", ring=4)
        return t, b

    def load_col(self, A, vec, nchunk, npart=128):
        t = A.alloc([128, nchunk], F32)
        b = Buf("col")
        self.dma("sp", t[0:npart, :], vec.rearrange("(c p) -> p c", p=npart), writes=(b,), stream="misc",
                 ring=4, allow_slow_non_contiguous=True)
        return t, b

    def load_col_rep(self, A, vec, n, reps, scale=None):
        t = A.alloc([128, 1], F32)
        b = Buf("colr")
        for r in range(reps):
            self.dma("sp", t[r * n:(r + 1) * n, :], vec.rearrange("(p o) -> p o", o=1), writes=(b,),
                     stream="misc", ring=4, allow_slow_non_contiguous=True)
        if scale is not None:
            self.ts(t[0:n * reps, :], t[0:n * reps, :], float(scale), None, ALU.mult, None, (b,), (b,))
        return t, b

    def norm_ctx(self, A, use_ln):
        C = {"use_ln": use_ln}
        C["XN"] = [(A.alloc([128, D], F32), Buf("xn")) for _ in range(2)]
        C["HN"] = [(A.alloc([128, D], BF16), Buf("hn")) for _ in range(4)]
        C["HT"] = [(A.alloc([128, 8, TT], BF16), Buf("ht")) for _ in range(2)]
        C["SS"] = [(A.alloc([128, 16], F32), Buf("ss")) for _ in range(2)]
        C["ident"], C["bid"] = self.load_const(A, self.c["c_ident"], [128, 128], BF16)
        C["x"] = 0
        C["pt"] = 0
        return C

    def norm1(self, C, t, src, nsub=4):
        ss, bss = C["SS"][t % 2]
        for j in range(nsub):
            xn, bxn = C["XN"][C["x"] % 2]
            C["x"] += 1
            hn, bhn = C["HN"][j]
            self.dma("sp", xn, src(j), writes=(bxn,), stream="xn", ring=2)
            self.act(hn, xn, AF.Square, (bxn,), (bhn, bss), accum_out=ss[:, j:j + 1])
        if C["use_ln"]:
            self.act(ss[:, 8:8 + nsub], ss[:, 0:nsub], AF.Ln, (bss,), (bss,), scale=1.0 / D, bias=EPS)
            self.act(ss[:, 12:12 + nsub], ss[:, 8:8 + nsub], AF.Exp, (bss,), (bss,), scale=-0.5)
        else:
            self.ts(ss[:, 4:4 + nsub], ss[:, 0:nsub], 1.0 / D, EPS, ALU.mult, ALU.add, (bss,), (bss,))
            self.act(ss[:, 8:8 + nsub], ss[:, 4:4 + nsub], AF.Sqrt, (bss,), (bss,))
            self.recip(ss[:, 12:12 + nsub], ss[:, 8:8 + nsub], (bss,), (bss,))
        for j in range(nsub):
            xn, bxn = C["XN"][C["x"] % 2]
            C["x"] += 1
            hn, bhn = C["HN"][j]
            self.dma("sp", xn, src(j), writes=(bxn,), stream="xn", ring=2)
            self.act(hn, xn, AF.Copy, (bxn, bss), (bhn,), scale=ss[:, 12 + j:13 + j])

    def norm2(self, C, t, nsub=4):
        ht, bht = C["HT"][t % 2]
        PS, bPS = self.PS, self.bPS
        ident, bid = C["ident"], C["bid"]
        for j in range(nsub):
            hn, bhn = C["HN"][j]
            pi = 6 + C["pt"] % 2
            C["pt"] += 1
            pt = PS[pi].bitcast(BF16).rearrange("p (c t) -> p c t", c=8)
            for c in range(8):
                self.S_.op("pe", lambda eng, pt=pt, hn=hn, c=c: eng.transpose(
                    out=pt[:, c, :], in_=hn[:, c * 128:(c + 1) * 128], identity=ident),
                    reads=(bhn, bid), writes=(bPS[pi],))
            self.cp("dve", ht[:, :, j * 128:(j + 1) * 128], pt, (bPS[pi],), (bht,))
        return ht, bht

    def resid_ctx(self, A):
        return {"XR": [(A.alloc([128, D], F32), Buf("xr")) for _ in range(2)]}

    def out_proj(self, R, t, js, steps, xin, xout, scale, banks=(4, 5)):
        PS, bPS = self.PS, self.bPS
        n = len(steps)
        for j in js:
            r0 = t * TT + j * 128
            xr, bxr = R["XR"][j % 2]
            self.dma("pool", xr, xin[r0:r0 + 128, :], writes=(bxr,), stream="xr", ring=2)
            for h in range(2):
                po = banks[h]
                for i, (lf, rf, rd) in enumerate(steps):
                    self.mm(PS[po], lf(j), rf(h), i == 0, i == n - 1, rd, (bPS[po],))
                self.stt(xr[:, h * 512:(h + 1) * 512], PS[po], float(scale), xr[:, h * 512:(h + 1) * 512],
                         ALU.mult, ALU.add, (bPS[po], bxr), (bxr,))
            self.dma("sp", xout[r0:r0 + 128, :], xr, reads=(bxr,), stream="xst", ring=2)

    def temp_pool(self, A, n32, n16):
        self.T32 = [(A.alloc([128, TT], F32), Buf("t32")) for _ in range(n32)]
        self.T16 = [(A.alloc([128, TT], BF16), Buf("t16")) for _ in range(n16)]
        self.t32_i = 0
        self.t16_i = 0

    def t32(self):
        r = self.T32[self.t32_i % len(self.T32)]
        self.t32_i += 1
        return r

    def t16(self):
        r = self.T16[self.t16_i % len(self.T16)]
        self.t16_i += 1
        return r

    def rstd_fm(self, ps_sum, bps, P, inv_n):
        L, bL = self.t32()
        self.act(L[0:P, :], ps_sum, AF.Ln, (bps,), (bL,), scale=float(inv_n), bias=EPS)
        self.act(L[0:P, :], L[0:P, :], AF.Exp, (bL,), (bL,), scale=-0.5)
        return L, bL

    def fm_norm(self, srcs, P, G, bG, nbank, inv_n, outs, gcol=None, bgcol=None):
        PS, bPS = self.PS, self.bPS
        xs = []
        for i, (ps, bps) in enumerate(srcs):
            X, bX = self.t32()
            Q, bQ = self.t16()
            self.act(X[0:P, :], ps, AF.Copy, (bps,), (bX,))
            self.act(Q[0:P, :], ps, AF.Square, (bps,), (bQ,))
            xs.append((X, bX, Q, bQ))
        pn = PS[nbank][0:P, :]
        for i, (X, bX, Q, bQ) in enumerate(xs):
            self.mm(pn, G, Q[0:P, :], i == 0, i == len(xs) - 1, (bG, bQ), (bPS[nbank],))
        Rr, bR = self.rstd_fm(pn, bPS[nbank], P, inv_n)
        for (X, bX, Q, bQ), (o, bo) in zip(xs, outs):
            if gcol is None:
                self.tt("dve", o, X[0:P, :], Rr[0:P, :], ALU.mult, (bX, bR), (bo,))
            else:
                self.stt(o, X[0:P, :], gcol, Rr[0:P, :], ALU.mult, ALU.mult, (bX, bR, bgcol), (bo,))

    def ffn_phase(self, xin, xout, norm_g, wg, wu, wd):
        S, A = self.S_, self.arena
        m0 = A.mark()
        WG = A.alloc([128, 8, DFF], BF16)
        WU = A.alloc([128, 8, DFF], BF16)
        WD = A.alloc([128, NFC, D], BF16)
        bWG, bWU, bWD = Buf("WG"), Buf("WU"), Buf("WD")
        stage = self.new_stage(A, 2)
        gcol, bg = self.load_col(A, norm_g, 8)
        C = self.norm_ctx(A, use_ln=False)
        R = self.resid_ctx(A)
        ACTT = A.alloc([128, NFC, TT], BF16)
        bACT = [Buf("act%d" % c) for c in range(NFC)]
        SG = [(A.alloc([128, TT], BF16), Buf("sg")) for _ in range(2)]
        for c in range(8):
            self.load_rows(stage, WG[:, c, :], bWG, wg[c * 128:(c + 1) * 128, :], gcol[:, c:c + 1], bg)
        for c in range(8):
            self.load_rows(stage, WU[:, c, :], bWU, wu[c * 128:(c + 1) * 128, :], gcol[:, c:c + 1], bg)
        for c in range(NFC):
            self.load_rows(stage, WD[:, c, :], bWD, wd[c * 128:(c + 1) * 128, :])
        PS, bPS = self.PS, self.bPS
        ntiles = self.NT // TT

        def src(t):
            return lambda j: xin[t * TT + j * 128:t * TT + (j + 1) * 128, :]

        def gateup(t):
            ht, bht = C["HT"][t % 2]
            for c in range(NFC):
                pg, pu = c % 2, 2 + c % 2
                for k in range(8):
                    self.mm(PS[pg], WG[:, k, c * 128:(c + 1) * 128], ht[:, k, :], k == 0, k == 7,
                            (bWG, bht), (bPS[pg],))
                for k in range(8):
                    self.mm(PS[pu], WU[:, k, c * 128:(c + 1) * 128], ht[:, k, :], k == 0, k == 7,
                            (bWU, bht), (bPS[pu],))
                sg, bsg = SG[c % 2]
                self.act(sg, PS[pg], AF.Silu, (bPS[pg],), (bsg,))
                self.tt("dve", ACTT[:, c, :], PS[pu], sg, ALU.mult, (bPS[pu], bsg), (bACT[c],))

        def down(t, js):
            steps = [((lambda j, c=c: ACTT[:, c, j * 128:(j + 1) * 128]),
                      (lambda h, c=c: WD[:, c, h * 512:(h + 1) * 512]),
                      (bWD, bACT[c])) for c in range(NFC)]
            self.out_proj(R, t, js, steps, xin, xout, 0.5)

        self.norm1(C, 0, src(0))
        self.norm2(C, 0)
        for t in range(ntiles):
            gateup(t)
            if t + 1 < ntiles:
                self.norm1(C, t + 1, src(t + 1))
            down(t, (0, 1))
            if t + 1 < ntiles:
                self.norm2(C, t + 1)
            down(t, (2, 3))
        S.barrier()
        A.reset(m0)

    def rope_phase(self, positions):
        S, A = self.S_, self.arena
        m0 = A.mark()
        ns, SQ = self.n_seq, self.S
        self.CM = self.scratch("s_cm", [ns, 64, SQ], F32)
        self.SM = self.scratch("s_sm", [ns, 64, SQ], F32)
        self.CR = self.scratch("s_cr", [ns, 128, SQ], F32)
        self.SR = self.scratch("s_sr", [ns, 128, SQ], F32)
        inv, binv = self.load_const(A, self.c["c_inv"], [128, 2], F32)
        POS = [(A.alloc([128, TT], I32), Buf("pos")) for _ in range(2)]
        PF = [(A.alloc([128, TT], F32), Buf("pf")) for _ in range(2)]
        U = [(A.alloc([128, TT], F32), Buf("u")) for _ in range(2)]
        KI = [(A.alloc([128, TT], I32), Buf("ki")) for _ in range(2)]
        KF = [(A.alloc([128, TT], F32), Buf("kf")) for _ in range(2)]
        OUT = [(A.alloc([128, TT], F32), Buf("ro")) for _ in range(4)]
        n = 0
        for s in range(ns):
            for ti in range(self.TPS):
                c0 = ti * TT
                pos, bpos = POS[(s * self.TPS + ti) % 2]
                pf, bpf = PF[(s * self.TPS + ti) % 2]
                self.dma("sp", pos, positions[s:s + 1, c0:c0 + TT].partition_broadcast(128), writes=(bpos,),
                         stream="misc", ring=4)
                self.cp("dve", pf, pos, (bpos,), (bpf,))
                for typ, P, cdst, sdst in ((0, 64, self.CM, self.SM), (1, 128, self.CR, self.SR)):
                    for off, dst in ((0.25, cdst), (0.0, sdst)):
                        u, bu = U[n % 2]
                        ki, bki = KI[n % 2]
                        kf, bkf = KF[n % 2]
                        o, bo = OUT[n % 4]
                        n += 1
                        self.ts(u[0:P, :], pf[0:P, :], inv[0:P, typ:typ + 1], off, ALU.mult, ALU.add,
                                (bpf, binv), (bu,))
                        self.cp("dve", ki[0:P, :], u[0:P, :], (bu,), (bki,))
                        self.cp("dve", kf[0:P, :], ki[0:P, :], (bki,), (bkf,))
                        self.tt("dve", u[0:P, :], u[0:P, :], kf[0:P, :], ALU.subtract, (bu, bkf), (bu,))
                        self.ts(kf[0:P, :], u[0:P, :], 0.5, None, ALU.is_gt, None, (bu,), (bkf,))
                        self.tt("dve", u[0:P, :], u[0:P, :], kf[0:P, :], ALU.subtract, (bu, bkf), (bu,))
                        self.act(o[0:P, :], u[0:P, :], AF.Sin, (bu,), (bo,), scale=TWO_PI)
                        self.dma("sp", dst[s, :, c0:c0 + TT], o[0:P, :], reads=(bo,), stream="rst", ring=4)
        S.barrier()
        A.reset(m0)

    def xattn_phase(self, xin, xout, mem, xnorm, mnorm, wq, wk, wv, wo, qn, kn):
        S, A = self.S_, self.arena
        PS, bPS = self.PS, self.bPS
        m0 = A.mark()
        stage = self.new_stage(A, 2)
        WQ = A.alloc([128, 8, D], BF16)
        WO = A.alloc([128, 8, D], BF16)
        WK = A.alloc([128, 8, D], BF16)
        WV = A.alloc([128, 8, D], BF16)
        bWQ, bWO, bWK, bWV = Buf("wq"), Buf("wo"), Buf("wk"), Buf("wv")
        gx, bgx = self.load_col(A, xnorm, 8)
        gm, bgm = self.load_col(A, mnorm, 8)
        gq, bgq = self.load_col(A, qn, 2)
        gk, bgk = self.load_col(A, kn, 2)
        self.ts(gk[:, 0:2], gk[:, 0:2], 1.0 / 16.0, None, ALU.mult, None, (bgk,), (bgk,))
        ones, bones = self.load_const(A, self.c["c_ones"], [128, 128], BF16)
        C = self.norm_ctx(A, use_ln=True)
        R = self.resid_ctx(A)
        self.temp_pool(A, 6, 4)
        for c in range(8):
            self.load_rows(stage, WK[:, c, :], bWK, wk[c * 128:(c + 1) * 128, :], gm[:, c:c + 1], bgm)
            self.load_rows(stage, WV[:, c, :], bWV, wv[c * 128:(c + 1) * 128, :], gm[:, c:c + 1], bgm)
        for c in range(8):
            self.load_rows(stage, WQ[:, c, :], bWQ, wq[c * 128:(c + 1) * 128, :], gx[:, c:c + 1], bgx)
            self.load_rows(stage, WO[:, c, :], bWO, wo[c * 128:(c + 1) * 128, :])
        KN = [(A.alloc([128, 8, MEM], BF16), Buf("kn")) for _ in range(self.n_seq)]
        VM = [(A.alloc([128, 2, D], BF16), Buf("vm")) for _ in range(self.n_seq)]
        QN = (A.alloc([128, 8, TT], BF16), Buf("qn"))
        PT = [(A.alloc([128, TT], BF16), Buf("p")) for _ in range(4)]
        OT = A.alloc([128, 8, TT], BF16)
        bOT = [Buf("ot%d" % c) for c in range(8)]
        RD = (A.alloc([128, TT], F32), Buf("rd"))
        for s in range(self.n_seq):
            self.norm1(C, s, lambda j, s=s: mem[s, j * 128:(j + 1) * 128, :], nsub=2)
            mt, bmt = self.norm2(C, s, nsub=2)
            kn_t, bkn = KN[s]
            vm_t, bvm = VM[s]
            for hh in range(4):
                srcs = []
                for dc in range(2):
                    c = 2 * hh + dc
                    b = dc
                    for k in range(8):
                        self.mm(PS[b][:, 0:MEM], WK[:, k, c * 128:(c + 1) * 128], mt[:, k, 0:MEM], k == 0, k == 7,
                                (bWK, bmt), (bPS[b],))
                    srcs.append((PS[b][:, 0:MEM], bPS[b]))
                xs = []
                for (ps, bps) in srcs:
                    X, bX = self.t32()
                    Q, bQ = self.t16()
                    self.act(X[:, 0:MEM], ps, AF.Copy, (bps,), (bX,))
                    self.act(Q[:, 0:MEM], ps, AF.Square, (bps,), (bQ,))
                    xs.append((X, bX, Q, bQ))
                for i, (X, bX, Q, bQ) in enumerate(xs):
                    self.mm(PS[2][:, 0:MEM], ones, Q[:, 0:MEM], i == 0, i == 1, (bones, bQ), (bPS[2],))
                L, bL = self.t32()
                self.act(L[:, 0:MEM], PS[2][:, 0:MEM], AF.Ln, (bPS[2],), (bL,), scale=1.0 / 256, bias=EPS)
                self.act(L[:, 0:MEM], L[:, 0:MEM], AF.Exp, (bL,), (bL,), scale=-0.5)
                for dc, (X, bX, Q, bQ) in enumerate(xs):
                    self.stt(kn_t[:, 2 * hh + dc, :], X[:, 0:MEM], gk[:, dc:dc + 1], L[:, 0:MEM], ALU.mult, ALU.mult,
                             (bX, bL, bgk), (bkn,))
            for mc in range(2):
                for h in range(2):
                    b = 4 + h
                    for k in range(8):
                        self.mm(PS[b], mt[:, k, mc * 128:(mc + 1) * 128], WV[:, k, h * 512:(h + 1) * 512],
                                k == 0, k == 7, (bWV, bmt), (bPS[b],))
                    self.cp("dve", vm_t[:, mc, h * 512:(h + 1) * 512], PS[b], (bPS[b],), (bvm,))
        ntiles = self.NT // TT

        def src(t):
            return lambda j: xin[t * TT + j * 128:t * TT + (j + 1) * 128, :]

        def attend(t, slot):
            s = t // self.TPS
            ht, bht = C["HT"][slot % 2]
            kn_t, bkn = KN[s]
            vm_t, bvm = VM[s]
            qn_t, bqn = QN
            for hh in range(4):
                srcs = []
                for dc in range(2):
                    c = 2 * hh + dc
                    b = dc
                    for k in range(8):
                        self.mm(PS[b], WQ[:, k, c * 128:(c + 1) * 128], ht[:, k, :], k == 0, k == 7,
                                (bWQ, bht), (bPS[b],))
                    srcs.append((PS[b], bPS[b]))
                xs = []
                for (ps, bps) in srcs:
                    X, bX = self.t32()
                    Q, bQ = self.t16()
                    self.act(X, ps, AF.Copy, (bps,), (bX,))
                    self.act(Q, ps, AF.Square, (bps,), (bQ,))
                    xs.append((X, bX, Q, bQ))
                for i, (X, bX, Q, bQ) in enumerate(xs):
                    self.mm(PS[2], ones, Q, i == 0, i == 1, (bones, bQ), (bPS[2],))
                L, bL = self.rstd_fm(PS[2], bPS[2], 128, 1.0 / 256)
                for dc, (X, bX, Q, bQ) in enumerate(xs):
                    self.stt(qn_t[:, 2 * hh + dc, :], X, gq[:, dc:dc + 1], L, ALU.mult, ALU.mult,
                             (bX, bL, bgq), (bqn,))
                ps_ = []
                for mc in range(2):
                    b = 3 + mc
                    for dc in range(2):
                        self.mm(PS[b], kn_t[:, 2 * hh + dc, mc * 128:(mc + 1) * 128], qn_t[:, 2 * hh + dc, :],
                                dc == 0, dc == 1, (bkn, bqn), (bPS[b],))
                    p, bp = PT[(2 * hh + mc) % 4]
                    self.act(p, PS[b], AF.Exp, (bPS[b],), (bp,))
                    ps_.append((p, bp))
                for mc, (p, bp) in enumerate(ps_):
                    self.mm(PS[5], ones, p, mc == 0, mc == 1, (bones, bp), (bPS[5],))
                rd, brd = RD
                self.recip(rd, PS[5], (bPS[5],), (brd,))
                for dc in range(2):
                    c = 2 * hh + dc
                    b = dc
                    for mc, (p, bp) in enumerate(ps_):
                        self.mm(PS[b], vm_t[:, mc, c * 128:(c + 1) * 128], p, mc == 0, mc == 1,
                                (bvm, bp), (bPS[b],))
                    self.tt("dve", OT[:, c, :], PS[b], rd, ALU.mult, (bPS[b], brd), (bOT[c],))

        def outp(t, js):
            steps = [((lambda j, c=c: OT[:, c, j * 128:(j + 1) * 128]),
                      (lambda h, c=c: WO[:, c, h * 512:(h + 1) * 512]),
                      (bWO, bOT[c])) for c in range(8)]
            self.out_proj(R, t, js, steps, xin, xout, 1.0, banks=(3, 4))

        base = self.n_seq
        self.norm1(C, base + 0, src(0))
        self.norm2(C, base + 0)
        for t in range(ntiles):
            attend(t, base + t)
            if t + 1 < ntiles:
                self.norm1(C, base + t + 1, src(t + 1))
            outp(t, (0, 1))
            if t + 1 < ntiles:
                self.norm2(C, base + t + 1)
            outp(t, (2, 3))
        S.barrier()
        A.reset(m0)

    def build(self):
        nc = self.nc
        NT, L, ns = self.NT, self.depth, self.n_seq
        E, O = (L + 1) // 2, L // 2
        hc = host_consts()
        self.c = {}
        for k, v in hc.items():
            if k.startswith("c_"):
                dt = BF16 if v.dtype != np.float32 else F32
                self.c[k] = self.inp(k, v.shape, dt)
        I = {}
        I["x"] = self.inp("x", [NT, D])
        I["mem"] = self.inp("mem", [ns, MEM, D])
        I["positions"] = self.inp("positions", [ns, self.S], I32)
        for nm, shp in PARAM_SHAPES(L, E, O):
            I[nm] = self.inp(nm, shp)
        y = nc.dram_tensor("y", [NT, D], F32, kind="ExternalOutput").ap()
        self.I = I
        phases = self.phases
        if phases is None:
            phases = [("rope",)]
            for l in range(L):
                phases.append(("ffn1", l))
                phases.append(("even", l) if l % 2 == 0 else ("odd", l))
                phases.append(("xattn", l))
                phases.append(("ffn2", l))
        cur = I["x"]
        for ph in phases:
            kind = ph[0]
            if kind == "rope":
                self.rope_phase(I["positions"])
                continue
            l = ph[1]
            if kind in ("ffn1", "ffn2"):
                self.ffn_phase(cur, y, I[kind + "_norm"][l], I[kind + "_w_gate"][l], I[kind + "_w_up"][l],
                               I[kind + "_w_down"][l])
            elif kind == "xattn":
                self.xattn_phase(cur, y, I["mem"], I["xattn_norm"][l], I["mem_norm"][l], I["xattn_wq"][l],
                                 I["xattn_wk"][l], I["xattn_wv"][l], I["xattn_wo"][l], I["xattn_q_norm"][l],
                                 I["xattn_k_norm"][l])
            elif kind == "even":
                self.even_phase(cur, y, l // 2, l)
            elif kind == "odd":
                self.odd_phase(cur, y, l // 2, l)
            cur = y
        self.S_.finalize()
        return nc


def PARAM_SHAPES(L, E, O):
    return [
        ("ffn1_norm", [L, D]), ("ffn1_w_gate", [L, D, DFF]), ("ffn1_w_up", [L, D, DFF]), ("ffn1_w_down", [L, DFF, D]),
        ("ffn2_norm", [L, D]), ("ffn2_w_gate", [L, D, DFF]), ("ffn2_w_up", [L, D, DFF]), ("ffn2_w_down", [L, DFF, D]),
        ("mix_norm", [L, D]), ("xattn_norm", [L, D]), ("mem_norm", [L, D]),
        ("xattn_wq", [L, D, D]), ("xattn_wk", [L, D, D]), ("xattn_wv", [L, D, D]), ("xattn_wo", [L, D, D]),
        ("xattn_q_norm", [L, 256]), ("xattn_k_norm", [L, 256]),
        ("ev_w_in", [E, D, 1440]), ("ev_conv_w", [E, 31, 512]), ("ev_conv_b", [E, 512]),
        ("ev_conv_ln_g", [E, 512]), ("ev_conv_ln_b", [E, 512]), ("ev_q_a_norm", [E, 256]),
        ("ev_w_q_b", [E, 256, 768]), ("ev_kv_a_norm", [E, 128]), ("ev_w_kv_b", [E, 128, 1024]),
        ("ev_q_nope_norm", [E, 64]), ("ev_k_nope_norm", [E, 64]), ("ev_q_rope_norm", [E, 32]),
        ("ev_k_rope_norm", [E, 32]), ("ev_w_out", [E, D, D]),
        ("od_w_in", [O, D, 6144]), ("od_gn_g", [O, 2048]), ("od_gn_b", [O, 2048]), ("od_w_out", [O, 2048, D]),
    ]


N_CORES = 8


def kernel(**inputs):
    x = np.ascontiguousarray(inputs["x"], dtype=np.float32)
    B, S, _ = x.shape
    ns = B // N_CORES
    L = inputs["ffn1_norm"].shape[0]
    b = Builder(ns, S, depth=L)
    nc = b.build()
    hc = {k: v for k, v in host_consts().items() if k.startswith("c_")}
    shared = {}
    for nm, shp in PARAM_SHAPES(L, (L + 1) // 2, L // 2):
        shared[nm] = np.ascontiguousarray(inputs[nm], dtype=np.float32)
    mem = np.ascontiguousarray(inputs["mem"], dtype=np.float32)
    pos = np.ascontiguousarray(inputs["positions"], dtype=np.int32)
    in_maps = []
    for c in range(N_CORES):
        m = dict(shared)
        m.update(hc)
        m["x"] = x[c * ns:(c + 1) * ns].reshape(ns * S, D)
        m["mem"] = mem[c * ns:(c + 1) * ns]
        m["positions"] = pos[c * ns:(c + 1) * ns]
        in_maps.append(m)
    res = run_bass_kernel_spmd(nc, in_maps, core_ids=list(range(N_CORES)))
    out = np.concatenate([r["y"].reshape(ns, S, D) for r in res.results], axis=0)
    return out.astype(np.float32)


def _even_phase(self, xin, xout, e, l):
    S, A = self.S_, self.arena
    PS, bPS = self.PS, self.bPS
    I = self.I
    ns, SQ, TPS = self.n_seq, self.S, self.TPS
    ntiles = self.NT // TT
    NB = SQ // 128
    SCALE = 96.0 ** -0.5
    if not hasattr(self, "ev_scr"):
        self.ev_scr = dict(
            AT=self.scratch("s_at", [ntiles, 128, 4 * TT], BF16),
            QN=self.scratch("s_qn", [ntiles, 128, 4 * TT], BF16),
            QR=self.scratch("s_qr", [ntiles, 64, 4 * TT], BF16),
            KN=self.scratch("s_kn", [ns, 128, 4, SQ], BF16),
            KR=self.scratch("s_kr", [ns, 64, SQ], BF16),
            V=self.scratch("s_v", [ns, 128, NB, 512], BF16),
        )
    scr = self.ev_scr

    m0 = A.mark()
    stage = self.new_stage(A, 2)
    gmix, bgmix = self.load_col(A, I["mix_norm"][l], 8)
    WIN = A.alloc([128, 8, 1472], BF16)
    bWIN = Buf("win")
    w_in = I["ev_w_in"][e]
    for k in range(8):
        self.load_rows(stage, WIN[:, k, 0:1440], bWIN, w_in[k * 128:(k + 1) * 128, :], gmix[:, k:k + 1], bgmix)
        self.load_rows(stage, WIN[:, k, 1440:1472], bWIN, w_in[k * 128:(k + 1) * 128, 1408:1440],
                       gmix[:, k:k + 1], bgmix)
    gqa, bgqa = self.load_col(A, I["ev_q_a_norm"][e], 2)
    WQN = A.alloc([128, 2, 4, 128], BF16)
    WQR = A.alloc([128, 2, 4, 64], BF16)
    bWQ = Buf("wqb")
    for c in range(2):
        sv, sb = self.stage_load(stage, I["ev_w_q_b"][e][c * 128:(c + 1) * 128, :])
        svh = sv.rearrange("p (h e) -> p h e", e=96)
        self.ts(WQN[:, c].rearrange("p a b -> p (a b)").rearrange("p (h e) -> p h e", e=64), svh[:, :, 0:64],
                gqa[:, c:c + 1], None, ALU.mult, None, (sb, bgqa), (bWQ,))
        self.ts(WQR[:, c].rearrange("p a b -> p (a b)").rearrange("p (h e) -> p h e", e=32), svh[:, :, 64:96],
                gqa[:, c:c + 1], None, ALU.mult, None, (sb, bgqa), (bWQ,))
    gkva, bgkva = self.load_col(A, I["ev_kv_a_norm"][e], 1)
    WKN = A.alloc([128, 4, 128], BF16)
    WVV = A.alloc([128, 512], BF16)
    bWKV = Buf("wkvb")
    sv, sb = self.stage_load(stage, I["ev_w_kv_b"][e])
    svh = sv.rearrange("p (h e) -> p h e", e=128)
    self.ts(WKN.rearrange("p a b -> p (a b)").rearrange("p (h e) -> p h e", e=64), svh[:, :, 0:64],
            gkva[:, 0:1], None, ALU.mult, None, (sb, bgkva), (bWKV,))
    self.ts(WVV.rearrange("p (h e) -> p h e", e=64), svh[:, :, 64:128],
            gkva[:, 0:1], None, ALU.mult, None, (sb, bgkva), (bWKV,))
    ident, bid0 = self.load_const(A, self.c["c_ident"], [128, 128], BF16)
    id32 = A.alloc([128, 128], F32)
    bid32 = Buf("id32")
    self.cp("dve", id32, ident, (bid0,), (bid32,))
    CW = A.alloc([128, 4, 31], F32)
    bCW = Buf("cw")
    for cc in range(4):
        self.dma("sp", CW[:, cc, :], I["ev_conv_w"][e][:, cc * 128:(cc + 1) * 128].rearrange("j p -> p j"),
                 writes=(bCW,), stream="misc", ring=4, allow_slow_non_contiguous=True)
    DIAG = A.alloc([128, 4, 31, 128], BF16)
    bDG = Buf("diag")
    for cc in range(4):
        for j in range(31):
            if (cc * 31 + j) % 2 == 0:
                self.ts(DIAG[:, cc, j, :], id32, CW[:, cc, j:j + 1], None, ALU.mult, None, (bid32, bCW), (bDG,))
            else:
                self.act(DIAG[:, cc, j, :], id32, AF.Copy, (bid32, bCW), (bDG,), scale=CW[:, cc, j:j + 1])
    cb, bcb = self.load_col(A, I["ev_conv_b"][e], 4)
    lng, blng = self.load_col(A, I["ev_conv_ln_g"][e], 4)
    lnb, blnb = self.load_col(A, I["ev_conv_ln_b"][e], 4)
    gqn, bgqn = self.load_col_rep(A, I["ev_q_nope_norm"][e], 64, 2, SCALE)
    gkn, bgkn = self.load_col_rep(A, I["ev_k_nope_norm"][e], 64, 2)
    gqr, bgqr = self.load_col_rep(A, I["ev_q_rope_norm"][e], 32, 2, SCALE)
    gkr, bgkr = self.load_col_rep(A, I["ev_k_rope_norm"][e], 32, 2)
    ones, bones = self.load_const(A, self.c["c_ones"], [128, 128], BF16)
    bd64, bbd64 = self.load_const(A, self.c["c_bd64"], [128, 128], BF16)
    bd32, bbd32 = self.load_const(A, self.c["c_bd32"], [64, 64], BF16)
    rot, brot = self.load_const(A, self.c["c_rot"], [64, 64], F32)
    C = self.norm_ctx(A, use_ln=True)
    self.temp_pool(A, 8, 6)
    ABF = [(A.alloc([128, 30 + TT], BF16), Buf("abf")) for _ in range(4)]
    Y32 = [(A.alloc([128, TT], F32), Buf("y32")) for _ in range(4)]
    YBF = [(A.alloc([128, TT], BF16), Buf("ybf")) for _ in range(4)]
    YSQ = [(A.alloc([128, TT], BF16), Buf("ysq")) for _ in range(4)]
    M32 = (A.alloc([128, TT], F32), Buf("m32"))
    ATt = [(A.alloc([128, 4, TT], BF16), Buf("at")) for _ in range(2)]
    ZQN = [(A.alloc([128, TT], BF16), Buf("zqn")) for _ in range(2)]
    QNt = [(A.alloc([128, 4, TT], BF16), Buf("qnt")) for _ in range(2)]
    QRt = [(A.alloc([128, 4, TT], BF16), Buf("qrt")) for _ in range(2)]
    ZKVN = (A.alloc([128, TT], BF16), Buf("zkvn"))
    KNt = [(A.alloc([128, 4, TT], BF16), Buf("knt")) for _ in range(2)]
    VTt = [(A.alloc([128, 4, 512], BF16), Buf("vtt")) for _ in range(2)]
    KRt = [(A.alloc([128, TT], BF16), Buf("krt")) for _ in range(2)]
    CT = [(A.alloc([128, TT], F32), Buf("ct")) for _ in range(2)]
    ST = [(A.alloc([128, TT], F32), Buf("st")) for _ in range(2)]
    RN = (A.alloc([128, TT], F32), Buf("rn"))

    def src(t):
        return lambda j: xin[t * TT + j * 128:t * TT + (j + 1) * 128, :]

    def rope64(ps, bps, gcol, bgcol, ct, bct, st, bst, out, bout):
        X, bX = self.t32()
        Q, bQ = self.t16()
        self.act(X[0:64, :], ps, AF.Copy, (bps,), (bX,))
        self.act(Q[0:64, :], ps, AF.Square, (bps,), (bQ,))
        self.mm(PS[7][0:64, :], bd32, Q[0:64, :], True, True, (bbd32, bQ), (bPS[7],))
        Rr, bR = self.rstd_fm(PS[7][0:64, :], bPS[7], 64, 1.0 / 32)
        rn, brn = RN
        self.stt(rn[0:64, :], X[0:64, :], gcol[0:64, 0:1], Rr[0:64, :], ALU.mult, ALU.mult, (bX, bR, bgcol), (brn,))
        self.mm(PS[7][0:64, :], rot, rn[0:64, :], True, True, (brot, brn), (bPS[7],))
        T1, bT1 = self.t32()
        T2, bT2 = self.t32()
        self.tt("pool", T1[0:64, :], rn[0:64, :], ct[0:64, :], ALU.mult, (brn, bct), (bT1,))
        self.tt("dve", T2[0:64, :], PS[7][0:64, :], st[0:64, :], ALU.mult, (bPS[7], bst), (bT2,))
        self.tt("pool", out, T1[0:64, :], T2[0:64, :], ALU.add, (bT1, bT2), (bout,))

    def e1_tile(t, slot):
        s, ti = t // TPS, t % TPS
        c0 = ti * TT
        ht, bht = C["HT"][slot % 2]
        ct, bct = CT[t % 2]
        st, bst = ST[t % 2]
        self.dma("sp", ct[0:64, :], self.CM[s, :, c0:c0 + TT], writes=(bct,), stream="tab", ring=4)
        self.dma("sp", st[0:64, :], self.SM[s, :, c0:c0 + TT], writes=(bst,), stream="tab", ring=4)
        for cc in range(4):
            ab, bab = ABF[cc]
            if ti == 0:
                self.memset("pool", ab[:, 0:30], 0.0, (bab,))
            pv, pg = cc % 2, 2 + cc % 2
            for k in range(8):
                self.mm(PS[pv], WIN[:, k, cc * 128:(cc + 1) * 128], ht[:, k, :], k == 0, k == 7, (bWIN, bht), (bPS[pv],))
            for k in range(8):
                self.mm(PS[pg], WIN[:, k, 512 + cc * 128:512 + (cc + 1) * 128], ht[:, k, :], k == 0, k == 7,
                        (bWIN, bht), (bPS[pg],))
            T1, bT1 = self.t32()
            self.act(T1, PS[pg], AF.Exp, (bPS[pg],), (bT1,), scale=-1.0)
            self.ts(T1, T1, 1.0, None, ALU.add, None, (bT1,), (bT1,))
            self.recip(T1, T1, (bT1,), (bT1,))
            self.tt("dve", ab[:, 30:30 + TT], PS[pv], T1, ALU.mult, (bPS[pv], bT1), (bab,))
        for cc in range(4):
            ab, bab = ABF[cc]
            py = 4 + cc % 2
            for j in range(31):
                self.mm(PS[py], DIAG[:, cc, j, :], ab[:, j:j + TT], j == 0, j == 30, (bDG, bab), (bPS[py],))
            y32, by32 = Y32[cc]
            self.ts(y32, PS[py], cb[:, cc:cc + 1], None, ALU.add, None, (bPS[py], bcb), (by32,))
            ybf, bybf = YBF[cc]
            ysq, bysq = YSQ[cc]
            self.cp("pool", ybf, y32, (by32,), (bybf,))
            self.tt("pool", ysq, y32, y32, ALU.mult, (by32,), (bysq,))
            self.cp("pool", ab[:, 0:30], ab[:, TT:TT + 30], (bab,), (bab,))
        for cc in range(4):
            self.mm(PS[0], ones, YBF[cc][0], cc == 0, cc == 3, (bones, YBF[cc][1]), (bPS[0],))
        for cc in range(4):
            self.mm(PS[1], ones, YSQ[cc][0], cc == 0, cc == 3, (bones, YSQ[cc][1]), (bPS[1],))
        m32, bm32 = M32
        self.act(m32, PS[0], AF.Copy, (bPS[0],), (bm32,), scale=1.0 / 512)
        V, bV = self.t32()
        self.tt("pool", V, m32, m32, ALU.mult, (bm32,), (bV,))
        self.stt(V, PS[1], 1.0 / 512, V, ALU.mult, ALU.subtract, (bPS[1], bV), (bV,))
        self.act(V, V, AF.Ln, (bV,), (bV,), bias=EPS)
        self.act(V, V, AF.Exp, (bV,), (bV,), scale=-0.5)
        at, bat = ATt[t % 2]
        for cc in range(4):
            y32, by32 = Y32[cc]
            Z, bZ = self.t32()
            self.tt("pool", Z, y32, m32, ALU.subtract, (by32, bm32), (bZ,))
            self.tt("pool", Z, Z, V, ALU.mult, (bZ, bV), (bZ,))
            self.ts(Z, Z, lng[:, cc:cc + 1], lnb[:, cc:cc + 1], ALU.mult, ALU.add, (bZ, blng, blnb), (bZ,))
            E_, bE = self.t32()
            self.act(E_, Z, AF.Exp, (bZ,), (bE,), scale=-1.0)
            self.ts(E_, E_, 1.0, None, ALU.add, None, (bE,), (bE,))
            self.recip(E_, E_, (bE,), (bE,))
            self.tt("pool", at[:, cc, :], Z, E_, ALU.mult, (bZ, bE), (bat,))
        self.dma("sp", scr["AT"][t].rearrange("p (a b) -> p a b", a=4), at, reads=(bat,), stream="est", ring=4)
        srcs = []
        for c in range(2):
            for k in range(8):
                self.mm(PS[c], WIN[:, k, 1024 + c * 128:1024 + (c + 1) * 128], ht[:, k, :], k == 0, k == 7,
                        (bWIN, bht), (bPS[c],))
            srcs.append((PS[c], bPS[c]))
        self.fm_norm(srcs, 128, ones, bones, 2, 1.0 / 256, [(ZQN[0][0], ZQN[0][1]), (ZQN[1][0], ZQN[1][1])])
        qnt, bqnt = QNt[t % 2]
        for pr in range(4):
            b = pr % 2
            for c in range(2):
                self.mm(PS[b], WQN[:, c, pr, :], ZQN[c][0], c == 0, c == 1, (bWQ, ZQN[c][1]), (bPS[b],))
            self.fm_norm([(PS[b], bPS[b])], 128, bd64, bbd64, 2 + pr % 2, 1.0 / 64, [(qnt[:, pr, :], bqnt)],
                         gqn[:, 0:1], bgqn)
        qrt, bqrt = QRt[t % 2]
        for pr in range(4):
            b = 4 + pr % 2
            for c in range(2):
                self.mm(PS[b][0:64, :], WQR[:, c, pr, :], ZQN[c][0], c == 0, c == 1, (bWQ, ZQN[c][1]), (bPS[b],))
            rope64(PS[b][0:64, :], bPS[b], gqr, bgqr, ct, bct, st, bst, qrt[0:64, pr, :], bqrt)
        self.dma("sp", scr["QN"][t].rearrange("p (a b) -> p a b", a=4), qnt, reads=(bqnt,), stream="est", ring=4)
        self.dma("sp", scr["QR"][t].rearrange("p (a b) -> p a b", a=4), qrt[0:64], reads=(bqrt,), stream="est", ring=4)
        for k in range(8):
            self.mm(PS[0], WIN[:, k, 1280:1408], ht[:, k, :], k == 0, k == 7, (bWIN, bht), (bPS[0],))
        zk, bzk = ZKVN
        self.fm_norm([(PS[0], bPS[0])], 128, ones, bones, 2, 1.0 / 128, [(zk, bzk)])
        knt, bknt = KNt[t % 2]
        for pr in range(4):
            b = pr % 2
            self.mm(PS[b], WKN[:, pr, :], zk, True, True, (bWKV, bzk), (bPS[b],))
            self.fm_norm([(PS[b], bPS[b])], 128, bd64, bbd64, 2 + pr % 2, 1.0 / 64, [(knt[:, pr, :], bknt)],
                         gkn[:, 0:1], bgkn)
        self.dma("sp", scr["KN"][s, :, :, c0:c0 + TT], knt, reads=(bknt,), stream="est", ring=4)
        vtt, bvtt = VTt[t % 2]
        for j in range(4):
            b = 4 + j % 2
            self.mm(PS[b], zk[:, j * 128:(j + 1) * 128], WVV, True, True, (bWKV, bzk), (bPS[b],))
            self.cp("act" if j % 2 else "dve", vtt[:, j, :], PS[b], (bPS[b],), (bvtt,))
        self.dma("sp", scr["V"][s, :, ti * 4:ti * 4 + 4, :], vtt, reads=(bvtt,), stream="est", ring=4)
        for k in range(8):
            self.mm(PS[6][0:64, :], WIN[:, k, 1408:1472], ht[:, k, :], k == 0, k == 7, (bWIN, bht), (bPS[6],))
        krt, bkrt = KRt[t % 2]
        rope64(PS[6][0:64, :], bPS[6], gkr, bgkr, ct, bct, st, bst, krt[0:64, :], bkrt)
        self.dma("sp", scr["KR"][s, :, c0:c0 + TT], krt[0:64, :], reads=(bkrt,), stream="est", ring=4)

    self.norm1(C, 0, src(0))
    self.norm2(C, 0)
    for t in range(ntiles):
        if t + 1 < ntiles:
            self.norm1(C, t + 1, src(t + 1))
        e1_tile(t, t)
        if t + 1 < ntiles:
            self.norm2(C, t + 1)
    S.barrier()
    A.reset(m0)

    m0 = A.mark()
    stage = self.new_stage(A, 2)
    WOC = A.alloc([128, 4, D], BF16)
    WOM = A.alloc([128, 8, D], BF16)
    bWOC, bWOM = Buf("woc"), Buf("wom")
    w_out = I["ev_w_out"][e]
    for cc in range(4):
        self.load_rows(stage, WOC[:, cc, :], bWOC, w_out[cc * 128:(cc + 1) * 128, :])
    for h in range(8):
        self.load_rows(stage, WOM[0:64, h, :], bWOM, w_out[512 + h * 64:512 + (h + 1) * 64, :])
    mask, bmask = self.load_const(A, self.c["c_mask"], [128, 128], BF16)
    sel, bsel = self.load_const(A, self.c["c_sel"], [65, 64], F32)
    R = self.resid_ctx(A)
    KNs = A.alloc([128, 4, SQ], BF16)
    KRs = A.alloc([128, SQ], BF16)
    VA = A.alloc([128, NB, 8, 65], BF16)
    bKN, bKR, bVA = Buf("kns"), Buf("krs"), Buf("va")
    VST = [(A.alloc([128, 8, 512], BF16), Buf("vst")) for _ in range(2)]
    QNl = [(A.alloc([128, 4, TT], BF16), Buf("qnl")) for _ in range(2)]
    QRl = [(A.alloc([128, 4, TT], BF16), Buf("qrl")) for _ in range(2)]
    ATl = [(A.alloc([128, 4, TT], BF16), Buf("atl")) for _ in range(2)]
    PTl = [(A.alloc([128, TT], BF16), Buf("pt")) for _ in range(3)]
    OS = [(A.alloc([128, TT], F32), Buf("os")) for _ in range(2)]
    RD = [(A.alloc([128, TT], F32), Buf("rd")) for _ in range(2)]
    OT = [(A.alloc([128, 8, TT], BF16), Buf("ot")) for _ in range(2)]
    pcount = 0
    for s in range(ns):
        self.dma("sp", KNs, scr["KN"][s], writes=(bKN,), stream="kv", ring=4)
        self.dma("sp", KRs[0:64, :], scr["KR"][s], writes=(bKR,), stream="kv", ring=4)
        self.memset("pool", VA, 1.0, (bVA,))
        for g in range(NB // 8):
            vs, bvs = VST[g % 2]
            self.dma("sp", vs, scr["V"][s, :, g * 8:(g + 1) * 8, :], writes=(bvs,), stream="kv", ring=4)
            self.cp("pool", VA[:, g * 8:(g + 1) * 8, :, 0:64], vs.rearrange("p b (h e) -> p b h e", e=64),
                    (bvs,), (bVA,))
        for ti in range(TPS):
            t = s * TPS + ti
            qn, bqn = QNl[t % 2]
            qr, bqr = QRl[t % 2]
            at, bat = ATl[t % 2]
            ot, bot = OT[t % 2]
            self.dma("sp", qn, scr["QN"][t].rearrange("p (a b) -> p a b", a=4), writes=(bqn,), stream="ql", ring=4)
            self.dma("sp", qr[0:64], scr["QR"][t].rearrange("p (a b) -> p a b", a=4), writes=(bqr,), stream="ql", ring=4)
            self.dma("sp", at, scr["AT"][t].rearrange("p (a b) -> p a b", a=4), writes=(bat,), stream="ql", ring=4)
            nkb = 4 * ti + 4
            blocks = [(h, kb) for h in range(8) for kb in range(nkb)]
            state = {}

            def qk(i):
                nonlocal pcount
                h, kb = blocks[i]
                pr, hh = h // 2, h % 2
                n0 = max(0, kb - 4 * ti) * 128
                pb = pcount % 2
                p, bp = PTl[pcount % 3]
                pcount += 1
                self.mm(PS[pb][:, n0:TT], KNs[hh * 64:(hh + 1) * 64, pr, kb * 128:(kb + 1) * 128],
                        qn[hh * 64:(hh + 1) * 64, pr, n0:TT], True, False, (bKN, bqn), (bPS[pb],))
                self.mm(PS[pb][:, n0:TT], KRs[hh * 32:(hh + 1) * 32, kb * 128:(kb + 1) * 128],
                        qr[hh * 32:(hh + 1) * 32, pr, n0:TT], False, True, (bKR, bqr), (bPS[pb],))
                self.act(p[:, n0:TT], PS[pb][:, n0:TT], AF.Exp, (bPS[pb],), (bp,))
                if kb >= 4 * ti:
                    self.tt("pool", p[:, n0:n0 + 128], p[:, n0:n0 + 128], mask, ALU.mult, (bp, bmask), (bp,))
                state[i] = (p, bp, n0)

            def pv(i):
                h, kb = blocks[i]
                p, bp, n0 = state.pop(i)
                po = 2 + h % 2
                self.mm(PS[po][0:65, n0:TT], VA[:, kb, h, :], p[:, n0:TT], kb == 0, kb == nkb - 1,
                        (bVA, bp), (bPS[po],))

            def epi1(h):
                po = 2 + h % 2
                os_, bos = OS[h % 2]
                self.cp("act", os_[0:65, :], PS[po][0:65, :], (bPS[po],), (bos,))

            def epi2(h):
                os_, bos = OS[h % 2]
                rd, brd = RD[h % 2]
                pd = 4 + h % 2
                self.mm(PS[pd][0:64, :], sel, os_[0:65, :], True, True, (bsel, bos), (bPS[pd],))
                self.recip(rd[0:64, :], PS[pd][0:64, :], (bPS[pd],), (brd,))
                self.tt("pool", ot[0:64, h, :], os_[0:64, :], rd[0:64, :], ALU.mult, (bos, brd), (bot,))

            qk(0)
            pend = []
            for i in range(len(blocks)):
                if i + 1 < len(blocks):
                    qk(i + 1)
                pv(i)
                h, kb = blocks[i]
                for item in list(pend):
                    if item[1] <= i:
                        epi2(item[0])
                        pend.remove(item)
                if kb == nkb - 1:
                    epi1(h)
                    pend.append((h, i + 2))
            for item in pend:
                epi2(item[0])
            steps = [((lambda j, cc=cc, at=at: at[:, cc, j * 128:(j + 1) * 128]),
                      (lambda hf, cc=cc: WOC[:, cc, hf * 512:(hf + 1) * 512]), (bWOC, bat)) for cc in range(4)]
            steps += [((lambda j, h=h, ot=ot: ot[0:64, h, j * 128:(j + 1) * 128]),
                       (lambda hf, h=h: WOM[0:64, h, hf * 512:(hf + 1) * 512]), (bWOM, bot)) for h in range(8)]
            self.out_proj(R, t, (0, 1, 2, 3), steps, xin, xout, 1.0, banks=(6, 7))
    S.barrier()
    A.reset(m0)


Builder.even_phase = _even_phase


def _odd_phase(self, xin, xout, o, l):
    S, A = self.S_, self.arena
    PS, bPS = self.PS, self.bPS
    I = self.I
    ns, SQ, TPS = self.n_seq, self.S, self.TPS
    ntiles = self.NT // TT
    if not hasattr(self, "od_scr"):
        self.od_scr = dict(
            QT=self.scratch("s_rq", [ntiles, 128, 8 * TT], BF16),
            KT=self.scratch("s_rk", [ntiles, 128, 8 * TT], BF16),
            QX=self.scratch("s_rqx", [ntiles, 128, 8 * TT], BF16),
            V=self.scratch("s_rv", [ntiles, 128, 4 * 2048], BF16),
            SG=self.scratch("s_rsg", [ntiles, 128, 4 * 2048], BF16),
        )
    scr = self.od_scr
    w_in = I["od_w_in"][o]

    m0 = A.mark()
    stage = self.new_stage(A, 2)
    gmix, bgmix = self.load_col(A, I["mix_norm"][l], 8)
    gmk = A.alloc([128, 8], F32)
    bgmk = Buf("gmk")
    self.ts(gmk, gmix, 1.0 / 16.0, None, ALU.mult, None, (bgmix,), (bgmk,))
    WIN = A.alloc([128, 8, 6144], BF16)
    bWIN = Buf("win")
    for k in range(8):
        rows = w_in[k * 128:(k + 1) * 128, :]
        self.load_rows(stage, WIN[:, k, 0:1024], bWIN, rows[:, 0:1024], gmix[:, k:k + 1], bgmix)
        self.load_rows(stage, WIN[:, k, 1024:2048], bWIN, rows[:, 1024:2048], gmk[:, k:k + 1], bgmk)
        self.load_rows(stage, WIN[:, k, 2048:6144], bWIN, rows[:, 2048:6144], gmix[:, k:k + 1], bgmix)
    xi, bxi = self.load_const(A, self.c["c_xi"], [128, 4, 512], F32)
    C = self.norm_ctx(A, use_ln=False)
    self.temp_pool(A, 4, 1)
    CT = [(A.alloc([128, TT], F32), Buf("ct")) for _ in range(1)]
    ST = [(A.alloc([128, TT], F32), Buf("st")) for _ in range(1)]
    QTt = [(A.alloc([128, 8, TT], BF16), Buf("qt")) for _ in range(1)]
    KTt = [(A.alloc([128, 8, TT], BF16), Buf("kt")) for _ in range(1)]
    QXt = [(A.alloc([128, 8, TT], BF16), Buf("qx")) for _ in range(1)]
    Q32 = [(A.alloc([128, TT], F32), Buf("q32")) for _ in range(2)]
    VT = [(A.alloc([128, 2048], BF16), Buf("vt")) for _ in range(2)]
    SGT = [(A.alloc([128, 2048], BF16), Buf("sgt")) for _ in range(2)]

    def src(t):
        return lambda j: xin[t * TT + j * 128:t * TT + (j + 1) * 128, :]

    def r1_tile(t, slot):
        s, ti = t // TPS, t % TPS
        c0 = ti * TT
        ht, bht = C["HT"][slot % 2]
        ct, bct = CT[0]
        st, bst = ST[0]
        self.dma("sp", ct, self.CR[s, :, c0:c0 + TT], writes=(bct,), stream="tab", ring=4)
        self.dma("sp", st, self.SR[s, :, c0:c0 + TT], writes=(bst,), stream="tab", ring=4)
        qt, bqt = QTt[0]
        kt, bkt = KTt[0]
        qx, bqx = QXt[0]
        for which, dst, bdst in ((0, qt, bqt), (1, kt, bkt)):
            for h in range(4):
                col = which * 1024 + h * 256
                for dc in range(2):
                    for k in range(8):
                        self.mm(PS[dc], WIN[:, k, col + dc * 128:col + (dc + 1) * 128], ht[:, k, :], k == 0, k == 7,
                                (bWIN, bht), (bPS[dc],))
                T1, b1 = self.t32()
                T2, b2 = self.t32()
                T3, b3 = self.t32()
                T4, b4 = self.t32()
                self.tt("dve", T1, PS[0], ct, ALU.mult, (bPS[0], bct), (b1,))
                self.tt("dve", T2, PS[1], st, ALU.mult, (bPS[1], bst), (b2,))
                self.tt("dve", T3, PS[0], st, ALU.mult, (bPS[0], bst), (b3,))
                self.tt("dve", T4, PS[1], ct, ALU.mult, (bPS[1], bct), (b4,))
                if which == 0:
                    qa, bqa = Q32[0]
                    qb, bqb = Q32[1]
                    self.tt("pool", qa, T1, T2, ALU.subtract, (b1, b2), (bqa,))
                    self.tt("pool", qb, T3, T4, ALU.add, (b3, b4), (bqb,))
                    self.cp("act", dst[:, 2 * h, :], qa, (bqa,), (bdst,))
                    self.cp("act", dst[:, 2 * h + 1, :], qb, (bqb,), (bdst,))
                    self.tt("pool", qx[:, 2 * h, :], qa, xi[:, h, :], ALU.mult, (bqa, bxi), (bqx,))
                    self.tt("pool", qx[:, 2 * h + 1, :], qb, xi[:, h, :], ALU.mult, (bqb, bxi), (bqx,))
                else:
                    self.tt("pool", dst[:, 2 * h, :], T1, T2, ALU.subtract, (b1, b2), (bdst,))
                    self.tt("pool", dst[:, 2 * h + 1, :], T3, T4, ALU.add, (b3, b4), (bdst,))
        self.dma("sp", scr["QT"][t].rearrange("p (a b) -> p a b", a=8), qt, reads=(bqt,), stream="rst1", ring=4)
        self.dma("sp", scr["KT"][t].rearrange("p (a b) -> p a b", a=8), kt, reads=(bkt,), stream="rst1", ring=4)
        self.dma("sp", scr["QX"][t].rearrange("p (a b) -> p a b", a=8), qx, reads=(bqx,), stream="rst1", ring=4)
        for cj in range(4):
            vt, bvt = VT[cj % 2]
            sg, bsg = SGT[cj % 2]
            for h in range(4):
                b = 2 + h % 2
                for k in range(8):
                    self.mm(PS[b], ht[:, k, cj * 128:(cj + 1) * 128], WIN[:, k, 2048 + h * 512:2048 + (h + 1) * 512],
                            k == 0, k == 7, (bWIN, bht), (bPS[b],))
                self.cp("dve" if h % 2 else "act", vt[:, h * 512:(h + 1) * 512], PS[b], (bPS[b],), (bvt,))
            for h in range(4):
                b = 4 + h % 2
                for k in range(8):
                    self.mm(PS[b], ht[:, k, cj * 128:(cj + 1) * 128], WIN[:, k, 4096 + h * 512:4096 + (h + 1) * 512],
                            k == 0, k == 7, (bWIN, bht), (bPS[b],))
                self.act(sg[:, h * 512:(h + 1) * 512], PS[b], AF.Silu, (bPS[b],), (bsg,))
            self.dma("sp", scr["V"][t, :, cj * 2048:(cj + 1) * 2048], vt, reads=(bvt,), stream="rst2", ring=4)
            self.dma("sp", scr["SG"][t, :, cj * 2048:(cj + 1) * 2048], sg, reads=(bsg,), stream="rst2", ring=4)

    self.norm1(C, 0, src(0))
    self.norm2(C, 0)
    for t in range(ntiles):
        if t + 1 < ntiles:
            self.norm1(C, t + 1, src(t + 1))
        r1_tile(t, t)
        if t + 1 < ntiles:
            self.norm2(C, t + 1)
    S.barrier()
    A.reset(m0)

    m0 = A.mark()
    stage = self.new_stage(A, 2)
    WO = A.alloc([128, 16, D], BF16)
    bWO = Buf("wo")
    for c in range(16):
        self.load_rows(stage, WO[:, c, :], bWO, I["od_w_out"][o][c * 128:(c + 1) * 128, :])
    ident, bid = self.load_const(A, self.c["c_ident"], [128, 128], BF16)
    decay, bdec = self.load_const(A, self.c["c_decay"], [128, 4, 128], F32)
    zeta, bzeta = self.load_const(A, self.c["c_zeta"], [128, 4], F32)
    GNG = A.alloc([128, 2048], F32)
    GNB = A.alloc([128, 2048], F32)
    bGN = Buf("gn")
    self.dma("sp", GNG, I["od_gn_g"][o].rearrange("(o n) -> o n", o=1).partition_broadcast(128), writes=(bGN,),
             stream="misc", ring=4)
    self.dma("sp", GNB, I["od_gn_b"][o].rearrange("(o n) -> o n", o=1).partition_broadcast(128), writes=(bGN,),
             stream="misc", ring=4)
    R = self.resid_ctx(A)
    ST32 = A.alloc([128, 4, 2, 512], F32)
    STB = A.alloc([128, 4, 2, 512], BF16)
    bST = [Buf("st32_%d" % h) for h in range(4)]
    bSTB = [Buf("stb_%d" % h) for h in range(4)]
    QTl = [(A.alloc([128, 8, TT], BF16), Buf("qtl")) for _ in range(2)]
    KTl = [(A.alloc([128, 8, TT], BF16), Buf("ktl")) for _ in range(2)]
    QXl = [(A.alloc([128, 8, TT], BF16), Buf("qxl")) for _ in range(2)]
    Vl = [(A.alloc([128, 2048], BF16), Buf("vl")) for _ in range(2)]
    SGl = [(A.alloc([128, 2048], BF16), Buf("sgl")) for _ in range(2)]
    KZ = [(A.alloc([128, 256], BF16), Buf("kz")) for _ in range(2)]
    PTl = [(A.alloc([128, 128], BF16), Buf("ptl")) for _ in range(2)]
    O32 = [(A.alloc([128, 512], F32), Buf("o32")) for _ in range(3)]
    SQJ = (A.alloc([128, 512], BF16), Buf("sqj"))
    STAT = [(A.alloc([128, 8], F32), Buf("stat")) for _ in range(3)]
    OG = [(A.alloc([128, 2048], BF16), Buf("og")) for _ in range(2)]
    OGT = [(A.alloc([128, 16, TT], BF16), Buf("ogt")) for _ in range(2)]
    cnt = 0
    for s in range(ns):
        for h in range(4):
            self.memset("pool", ST32[:, h], 0.0, (bST[h],))
            self.memset("pool", STB[:, h], 0.0, (bSTB[h],))
        for ti in range(TPS):
            t = s * TPS + ti
            qt, bqt = QTl[t % 2]
            kt, bkt = KTl[t % 2]
            qx, bqx = QXl[t % 2]
            ogt, bogt = OGT[t % 2]
            self.dma("sp", qt, scr["QT"][t].rearrange("p (a b) -> p a b", a=8), writes=(bqt,), stream="rl", ring=4)
            self.dma("sp", kt, scr["KT"][t].rearrange("p (a b) -> p a b", a=8), writes=(bkt,), stream="rl", ring=4)
            self.dma("sp", qx, scr["QX"][t].rearrange("p (a b) -> p a b", a=8), writes=(bqx,), stream="rl", ring=4)
            items = [(cj, h) for cj in range(4) for h in range(4)]
            info = {}

            def stage_a(i):
                nonlocal cnt
                cj, h = items[i]
                cs = slice(cj * 128, (cj + 1) * 128)
                vl, bvl = Vl[cj % 2]
                sg, bsg = SGl[cj % 2]
                if h == 0:
                    self.dma("sp", vl, scr["V"][t, :, cj * 2048:(cj + 1) * 2048], writes=(bvl,), stream="rl2", ring=4)
                    self.dma("sp", sg, scr["SG"][t, :, cj * 2048:(cj + 1) * 2048], writes=(bsg,), stream="rl2", ring=4)
                cnt += 1
                k_ = cnt
                pz = 6 + k_ % 2
                ptz = PS[pz].bitcast(BF16)
                for dc in range(2):
                    self.S_.op("pe", lambda eng, ptz=ptz, kt=kt, h=h, dc=dc, cs=cs: eng.transpose(
                        out=ptz[:, dc * 128:(dc + 1) * 128], in_=kt[:, 2 * h + dc, cs], identity=ident),
                        reads=(bkt, bid), writes=(bPS[pz],))
                kz, bkz = KZ[k_ % 2]
                self.act(kz, ptz[:, 0:256], AF.Copy, (bPS[pz], bzeta), (bkz,), scale=zeta[:, h:h + 1])
                pb = 0 + k_ % 2
                for dc in range(2):
                    self.mm(PS[pb][:, 0:128], kt[:, 2 * h + dc, cs], qt[:, 2 * h + dc, cs], dc == 0, dc == 1,
                            (bkt, bqt), (bPS[pb],))
                p, bp = PTl[k_ % 2]
                self.tt("dve", p, PS[pb][:, 0:128], decay[:, h, :], ALU.mult, (bPS[pb], bdec), (bp,))
                info[i] = (k_, kz, bkz, p, bp)

            def stage_b(i):
                cj, h = items[i]
                cs = slice(cj * 128, (cj + 1) * 128)
                vl, bvl = Vl[cj % 2]
                sg, bsg = SGl[cj % 2]
                og, bog = OG[cj % 2]
                k_, kz, bkz, p, bp = info.pop(i)
                po = 2 + k_ % 2
                self.mm(PS[po], p, vl[:, h * 512:(h + 1) * 512], True, False, (bp, bvl), (bPS[po],))
                for dc in range(2):
                    self.mm(PS[po], qx[:, 2 * h + dc, cs], STB[:, h, dc, :], False, dc == 1, (bqx, bSTB[h]),
                            (bPS[po],))
                for dc in range(2):
                    pu = 4 + dc
                    self.mm(PS[pu], kz[:, dc * 128:(dc + 1) * 128], vl[:, h * 512:(h + 1) * 512], True, True,
                            (bkz, bvl), (bPS[pu],))
                    self.stt(ST32[:, h, dc, :], ST32[:, h, dc, :], float(self.gch[h]), PS[pu], ALU.mult, ALU.add,
                             (bPS[pu], bST[h]), (bST[h],))
                self.cp("pool", STB[:, h], ST32[:, h], (bST[h],), (bSTB[h],))
                o32, bo32 = O32[k_ % 3]
                stt_, bstat = STAT[k_ % 3]
                sq, bsq = SQJ
                self.act(o32, PS[po], AF.Copy, (bPS[po],), (bo32, bstat), accum_out=stt_[:, 0:1])
                self.act(sq, PS[po], AF.Square, (bPS[po],), (bsq, bstat), accum_out=stt_[:, 1:2])
                self.ts(stt_[:, 2:3], stt_[:, 0:1], 1.0 / 512, None, ALU.mult, None, (bstat,), (bstat,))
                self.tt("dve", stt_[:, 3:4], stt_[:, 2:3], stt_[:, 2:3], ALU.mult, (bstat,), (bstat,))
                self.stt(stt_[:, 4:5], stt_[:, 1:2], 1.0 / 512, stt_[:, 3:4], ALU.mult, ALU.subtract,
                         (bstat,), (bstat,))
                self.ts(stt_[:, 4:5], stt_[:, 4:5], EPS, None, ALU.add, None, (bstat,), (bstat,))
                self.act(stt_[:, 5:6], stt_[:, 4:5], AF.Sqrt, (bstat,), (bstat,))
                self.recip(stt_[:, 6:7], stt_[:, 5:6], (bstat,), (bstat,))
                self.ts(o32, o32, stt_[:, 2:3], stt_[:, 6:7], ALU.subtract, ALU.mult, (bo32, bstat), (bo32,))
                self.tt("pool", o32, o32, GNG[:, h * 512:(h + 1) * 512], ALU.mult, (bo32, bGN), (bo32,))
                self.tt("pool", o32, o32, GNB[:, h * 512:(h + 1) * 512], ALU.add, (bo32, bGN), (bo32,))
                self.tt("pool", og[:, h * 512:(h + 1) * 512], o32, sg[:, h * 512:(h + 1) * 512], ALU.mult,
                        (bo32, bsg), (bog,))

            def chunk_end(cj):
                cs = slice(cj * 128, (cj + 1) * 128)
                og, bog = OG[cj % 2]
                for g in range(2):
                    pz = 6 + g
                    ptz = PS[pz].bitcast(BF16).rearrange("p (c t) -> p c t", c=8)
                    for c in range(8):
                        self.S_.op("pe", lambda eng, ptz=ptz, og=og, c=c, g=g: eng.transpose(
                            out=ptz[:, c, :], in_=og[:, (g * 8 + c) * 128:(g * 8 + c + 1) * 128], identity=ident),
                            reads=(bog, bid), writes=(bPS[pz],))
                    self.cp("dve" if g else "act", ogt[:, g * 8:(g + 1) * 8, cs], ptz, (bPS[pz],), (bogt,))

            stage_a(0)
            pend = []
            for i in range(len(items)):
                if i + 1 < len(items):
                    stage_a(i + 1)
                stage_b(i)
                for it in list(pend):
                    if it[1] <= i:
                        chunk_end(it[0])
                        pend.remove(it)
                if items[i][1] == 3:
                    pend.append((items[i][0], i + 2))
            for it in pend:
                chunk_end(it[0])
            steps = [((lambda j, c=c, ogt=ogt: ogt[:, c, j * 128:(j + 1) * 128]),
                      (lambda hf, c=c: WO[:, c, hf * 512:(hf + 1) * 512]), (bWO, bogt)) for c in range(16)]
            self.out_proj(R, t, (0, 1, 2, 3), steps, xin, xout, 1.0, banks=(4, 5))
    S.barrier()
    A.reset(m0)


Builder.odd_phase = _odd_phase
```

```python
import numpy as np
import concourse.bass as bass
import concourse.mybir as mybir
from concourse.bass_utils import run_bass_kernel_spmd

F32 = mybir.dt.float32
BF16 = mybir.dt.bfloat16
I32 = mybir.dt.int32
U8 = mybir.dt.uint8
ALU = mybir.AluOpType
AF = mybir.ActivationFunctionType
AX = mybir.AxisListType

ENGS = ("pe", "act", "dve", "pool", "sp")
EPOCH = 20000
DT_SIZE = {F32: 4, BF16: 2, I32: 4, U8: 1}


class Buf:
    __slots__ = ("name", "w", "r")

    def __init__(self, name=""):
        self.name = name
        self.w = None
        self.r = {}


class Sched:
    def __init__(self, nc):
        self.nc = nc
        self.eobj = {"pe": nc.tensor, "act": nc.scalar, "dve": nc.vector,
                     "pool": nc.gpsimd, "sp": nc.sync}
        self.ops = {e: [] for e in ENGS}
        self.streams = {}
        self.sem_handles = {}
        self.nsem = 0

    def _sem(self, key):
        h = self.sem_handles.get(key)
        if h is None:
            h = self.nc.alloc_semaphore("s%d" % self.nsem)
            self.nsem += 1
            self.sem_handles[key] = h
        return h

    @staticmethod
    def _add(deps, key, val):
        if deps.get(key, -1) < val:
            deps[key] = val

    def _deps(self, e, reads, writes):
        deps = {}
        for b in reads:
            if b.w is not None:
                self._add(deps, *b.w)
        for b in writes:
            if b.w is not None:
                self._add(deps, *b.w)
            for k, v in b.r.items():
                self._add(deps, k, v)
        if e == "pe":
            deps.pop(("e", "pe"), None)
        return deps

    def _mark(self, me, reads, writes):
        for b in reads:
            if b.r.get(me[0], -1) < me[1]:
                b.r[me[0]] = me[1]
        for b in writes:
            b.w = me
            b.r = {}

    def op(self, e, fn, reads=(), writes=()):
        idx = len(self.ops[e])
        deps = self._deps(e, reads, writes)
        self.ops[e].append([fn, deps, None])
        self._mark((("e", e), idx), reads, writes)

    def dma(self, q, out_ap, in_ap, reads=(), writes=(), stream="ld", ring=6, **kw):
        st = self.streams.setdefault(stream, [0, ring])
        i = st[0]
        st[0] += 1
        slot = i % st[1]
        gen = i // st[1]
        key = ("d", stream, slot)
        deps = self._deps(q, reads, writes)
        if gen > 0:
            self._add(deps, key, 16 * gen)

        def fn(eng, out_ap=out_ap, in_ap=in_ap, kw=kw):
            return eng.dma_start(out=out_ap, in_=in_ap, **kw)
        self.ops[q].append([fn, deps, key])
        self._mark((key, 16 * (gen + 1)), reads, writes)

    def barrier(self):
        last = {}
        for e in ENGS:
            for i in range(len(self.ops[e]) - 1, -1, -1):
                if self.ops[e][i][2] is None:
                    last[("e", e)] = i
                    break
        for name, (cnt, ring) in self.streams.items():
            for slot in range(min(cnt, ring)):
                n = (cnt - 1 - slot) // ring + 1
                last[("d", name, slot)] = 16 * n
        for e in ENGS:
            d = dict(last)
            if e == "pe":
                d.pop(("e", "pe"), None)
            self.ops[e].append([lambda eng: eng.nop(), d, None])

    def finalize(self):
        nc = self.nc
        need = {e: set() for e in ENGS}
        for e in ENGS:
            for fn, deps, dk in self.ops[e]:
                for k, v in deps.items():
                    if k[0] == "e":
                        need[k[1]].add(v)
        rank = {}
        for e in ENGS:
            r = 0
            for i in sorted(need[e]):
                rank[(e, i)] = r
                r += 1

        def resolve(k, v):
            if k[0] == "e":
                r = rank[(k[1], v)]
                return self._sem(("e", k[1], r // EPOCH)), r % EPOCH + 1
            return self._sem(k), v

        for e in ENGS:
            for fn, deps, dk in self.ops[e]:
                for k, v in deps.items():
                    resolve(k, v)
                if dk is not None:
                    self._sem(dk)

        def emit(e, eng):
            seen = {}
            for i, (fn, deps, dk) in enumerate(self.ops[e]):
                for k, v in deps.items():
                    sem, val = resolve(k, v)
                    sk = id(sem)
                    if seen.get(sk, -1) >= val:
                        continue
                    seen[sk] = val
                    eng.wait_ge(sem, val)
                ins = fn(eng)
                if dk is not None:
                    ins.then_inc(self._sem(dk), 16)
                elif (e, i) in rank:
                    r = rank[(e, i)]
                    ins.then_inc(self._sem(("e", e, r // EPOCH)), 1)

        with nc.Block() as block:
            @block.tensor
            def _(eng):
                emit("pe", eng)

            @block.scalar
            def _(eng):
                emit("act", eng)

            @block.vector
            def _(eng):
                emit("dve", eng)

            @block.gpsimd
            def _(eng):
                emit("pool", eng)

            @block.sync
            def _(eng):
                emit("sp", eng)
        for h in self.sem_handles.values():
            nc.gpsimd.sem_clear(h)
        nc.all_engine_barrier()


class Arena:
    def __init__(self, nc, nbytes):
        self.t = nc.alloc_sbuf_tensor("arena", [128, nbytes], U8)
        self.n = nbytes
        self.off = 0
        self.marks = []

    def alloc(self, shape, dt):
        n = int(np.prod(shape[1:])) * DT_SIZE[dt]
        n_al = (n + 63) // 64 * 64
        assert self.off + n_al <= self.n, ("SBUF arena overflow", self.off, n_al, self.n)
        v = self.t[0:shape[0], self.off:self.off + n].bitcast(dt)
        self.off += n_al
        if len(shape) == 3:
            v = v.rearrange("p (a b) -> p a b", a=shape[1])
        elif len(shape) == 4:
            v = v.rearrange("p (a b c) -> p a b c", a=shape[1], b=shape[2])
        return v

    def mark(self):
        return self.off

    def reset(self, m):
        self.off = m


D = 1024
DFF = 2816
NFC = DFF // 128
EPS = 1e-6
TT = 512
MEM = 256
NWARM = 1
TWO_PI = 6.283184


def host_consts():
    import ml_dtypes
    bf = ml_dtypes.bfloat16
    c = {}
    c["c_ident"] = np.eye(128, dtype=np.float32).astype(bf)
    c["c_ones"] = np.ones((128, 128), np.float32).astype(bf)
    bd64 = np.zeros((128, 128), np.float32)
    bd64[:64, :64] = 1
    bd64[64:, 64:] = 1
    c["c_bd64"] = bd64.astype(bf)
    bd32 = np.zeros((64, 64), np.float32)
    bd32[:32, :32] = 1
    bd32[32:, 32:] = 1
    c["c_bd32"] = bd32.astype(bf)
    rot = np.zeros((64, 64), np.float32)
    for m in range(64):
        if m % 32 < 16:
            rot[m + 16, m] = -1.0
        else:
            rot[m - 16, m] = 1.0
    c["c_rot"] = rot
    sel = np.zeros((65, 64), np.float32)
    sel[64, :] = 1.0
    c["c_sel"] = sel
    mask = np.zeros((128, 128), np.float32)
    for j in range(128):
        mask[j, j:] = 1.0
    c["c_mask"] = mask.astype(bf)
    inv_m = (10000.0 ** (-np.arange(0, 32, 2, dtype=np.float32) / 32)).astype(np.float32)
    inv_r = (10000.0 ** (-np.arange(0, 256, 2, dtype=np.float32) / 256)).astype(np.float32)
    tab = np.zeros((128, 2), np.float32)
    tab[:, 0] = inv_m[np.arange(128) % 16] / (2 * np.pi)
    tab[:, 1] = inv_r / (2 * np.pi)
    c["c_inv"] = tab
    H = 4
    log_g = np.log1p(-np.exp2(-5.0 - np.arange(H, dtype=np.float64)))
    idx = np.arange(128, dtype=np.float64)
    diff = idx[None, :] - idx[:, None]
    dec = np.where(diff >= 0, np.exp(log_g[:, None, None] * np.maximum(diff, 0.0)), 0.0)
    c["c_decay"] = np.ascontiguousarray(dec.transpose(1, 0, 2)).astype(np.float32)
    xi = np.exp(log_g[:, None] * (idx + 1.0))
    c["c_xi"] = np.tile(xi[None, :, None, :], (128, 1, 4, 1)).reshape(128, 4, 512).astype(np.float32)
    zeta = np.exp(log_g[:, None] * (128 - 1.0 - idx))
    c["c_zeta"] = np.ascontiguousarray(zeta.T).astype(np.float32)
    c["_gchunk"] = [float(np.exp(log_g[h] * 128)) for h in range(H)]
    return c


class Builder:
    def __init__(self, n_seq, S, depth=4, phases=None):
        self.n_seq, self.S, self.depth = n_seq, S, depth
        self.NT = n_seq * S
        self.TPS = S // TT
        self.phases = phases
        nc = bass.Bass("TRN2", target_bir_lowering=False)
        self.nc = nc
        self.S_ = Sched(nc)
        self.arena = Arena(nc, 212000)
        self.PS = [nc.alloc_psum_tensor("ps%d" % i, [128, 512], F32)[:, :] for i in range(8)]
        self.bPS = [Buf("ps%d" % i) for i in range(8)]
        self.cast_rr = 0
        self.stage_i = 0
        self.gch = host_consts()["_gchunk"]

    def inp(self, name, shape, dt=F32):
        return self.nc.dram_tensor(name, list(shape), dt, kind="ExternalInput").ap()

    def scratch(self, name, shape, dt):
        return self.nc.dram_tensor(name, list(shape), dt).ap()

    def mm(self, out, lhsT, rhs, start, stop, reads, writes):
        self.S_.op("pe", lambda eng: eng.matmul(out=out, lhsT=lhsT, rhs=rhs, start=start, stop=stop),
                   reads, writes)

    def act(self, out, in_, func, reads, writes, **kw):
        self.S_.op("act", lambda eng: eng.activation(out=out, in_=in_, func=func, **kw), reads, writes)

    def tt(self, e, out, in0, in1, op, reads, writes):
        self.S_.op(e, lambda eng: eng.tensor_tensor(out=out, in0=in0, in1=in1, op=op), reads, writes)

    def ts(self, out, in0, s1, s2, op0, op1, reads, writes, e="dve"):
        if s2 is None:
            self.S_.op(e, lambda eng: eng.tensor_scalar(out=out, in0=in0, scalar1=s1, scalar2=None, op0=op0),
                       reads, writes)
        else:
            self.S_.op(e, lambda eng: eng.tensor_scalar(out=out, in0=in0, scalar1=s1, scalar2=s2,
                                                        op0=op0, op1=op1), reads, writes)

    def stt(self, out, in0, scalar, in1, op0, op1, reads, writes, e="dve"):
        self.S_.op(e, lambda eng: eng.scalar_tensor_tensor(out=out, in0=in0, scalar=scalar, in1=in1,
                                                           op0=op0, op1=op1), reads, writes)

    def cp(self, e, out, in_, reads, writes):
        if e == "act":
            self.S_.op(e, lambda eng: eng.copy(out=out, in_=in_), reads, writes)
        else:
            self.S_.op(e, lambda eng: eng.tensor_copy(out=out, in_=in_), reads, writes)

    def recip(self, out, in_, reads, writes):
        self.S_.op("dve", lambda eng: eng.reciprocal(out=out, in_=in_), reads, writes)

    def memset(self, e, out, val, writes):
        self.S_.op(e, lambda eng: eng.memset(out, val), (), writes)

    def dma(self, q, out, in_, reads=(), writes=(), stream="ld", ring=4, **kw):
        self.S_.dma(q, out, in_, reads=reads, writes=writes, stream=stream, ring=ring, **kw)

    def cast_op(self, out, in_, reads, writes, scale_ap=None):
        S = self.S_
        if scale_ap is None:
            e = ("dve", "pool", "act")[self.cast_rr % 3]
        else:
            e = ("dve", "act")[self.cast_rr % 2]
        self.cast_rr += 1
        if scale_ap is None:
            self.cp(e, out, in_, reads, writes)
        elif e == "act":
            self.act(out, in_, AF.Copy, reads, writes, scale=scale_ap)
        else:
            self.ts(out, in_, scale_ap, None, ALU.mult, None, reads, writes)

    def new_stage(self, A, n=2, cols=1024):
        return [(A.alloc([128, cols], F32), Buf("stg")) for _ in range(n)]

    def stage_load(self, stage, src):
        k = self.stage_i
        self.stage_i += 1
        sb, sbuf = stage[k % len(stage)]
        p, n = src.shape
        q = ("sp", "pool")[k % 2]
        self.dma(q, sb[0:p, 0:n], src, writes=(sbuf,), stream="wld", ring=4)
        return sb[0:p, 0:n], sbuf

    def load_rows(self, stage, dst, dbuf, src, gain=None, gbuf=None):
        p, n = src.shape
        cols = stage[0][0].shape[1]
        for c0 in range(0, n, cols):
            w = min(cols, n - c0)
            sv, sbuf = self.stage_load(stage, src[:, c0:c0 + w])
            rd = (sbuf,) if gain is None else (sbuf, gbuf)
            self.cast_op(dst[:, c0:c0 + w], sv, rd, (dbuf,), gain)

    def load_const(self, A, dram, shape, dt):
        t = A.alloc(list(shape), dt)
        b = Buf("const")
        self.dma("sp", t, dram, writes=(b,), stream="misc", ring=4)
        return t, b

    def load_col(self, A, vec, nchunk, npart=128):
        t = A.alloc([128, nchunk], F32)
        b = Buf("col")
        self.dma("sp", t[0:npart, :], vec.rearrange("(c p) -> p c", p=npart), writes=(b,), stream="misc",
                 ring=4, allow_slow_non_contiguous=True)
        return t, b

    def load_col_rep(self, A, vec, n, reps, scale=None):
        t = A.alloc([128, 1], F32)
        b = Buf("colr")
        for r in range(reps):
            self.dma("sp", t[r * n:(r + 1) * n, :], vec.rearrange("(p o) -> p o", o=1), writes=(b,),
                     stream="misc", ring=4, allow_slow_non_contiguous=True)
        if scale is not None:
            self.ts(t[0:n * reps, :], t[0:n * reps, :], float(scale), None, ALU.mult, None, (b,), (b,))
        return t, b

    def norm_ctx(self, A, use_ln):
        C = {"use_ln": use_ln}
        C["XN"] = [(A.alloc([128, D], F32), Buf("xn")) for _ in range(2)]
        C["HN"] = [(A.alloc([128, D], BF16), Buf("hn")) for _ in range(4)]
        C["HT"] = [(A.alloc([128, 8, TT], BF16), Buf("ht")) for _ in range(2)]
        C["SS"] = [(A.alloc([128, 16], F32), Buf("ss")) for _ in range(2)]
        C["ident"], C["bid"] = self.load_const(A, self.c["c_ident"], [128, 128], BF16)
        C["x"] = 0
        C["pt"] = 0
        return C

    def norm1(self, C, t, src, nsub=4):
        ss, bss = C["SS"][t % 2]
        for j in range(nsub):
            xn, bxn = C["XN"][C["x"] % 2]
            C["x"] += 1
            hn, bhn = C["HN"][j]
            self.dma("sp", xn, src(j), writes=(bxn,), stream="xn", ring=2)
            self.act(hn, xn, AF.Square, (bxn,), (bhn, bss), accum_out=ss[:, j:j + 1])
        if C["use_ln"]:
            self.act(ss[:, 8:8 + nsub], ss[:, 0:nsub], AF.Ln, (bss,), (bss,), scale=1.0 / D, bias=EPS)
            self.act(ss[:, 12:12 + nsub], ss[:, 8:8 + nsub], AF.Exp, (bss,), (bss,), scale=-0.5)
        else:
            self.ts(ss[:, 4:4 + nsub], ss[:, 0:nsub], 1.0 / D, EPS, ALU.mult, ALU.add, (bss,), (bss,))
            self.act(ss[:, 8:8 + nsub], ss[:, 4:4 + nsub], AF.Sqrt, (bss,), (bss,))
            self.recip(ss[:, 12:12 + nsub], ss[:, 8:8 + nsub], (bss,), (bss,))
        for j in range(nsub):
            xn, bxn = C["XN"][C["x"] % 2]
            C["x"] += 1
            hn, bhn = C["HN"][j]
            self.dma("sp", xn, src(j), writes=(bxn,), stream="xn", ring=2)
            self.act(hn, xn, AF.Copy, (bxn, bss), (bhn,), scale=ss[:, 12 + j:13 + j])

    def norm2(self, C, t, nsub=4):
        ht, bht = C["HT"][t % 2]
        PS, bPS = self.PS, self.bPS
        ident, bid = C["ident"], C["bid"]
        for j in range(nsub):
            hn, bhn = C["HN"][j]
            pi = 6 + C["pt"] % 2
            C["pt"] += 1
            pt = PS[pi].bitcast(BF16).rearrange("p (c t) -> p c t", c=8)
            for c in range(8):
                self.S_.op("pe", lambda eng, pt=pt, hn=hn, c=c: eng.transpose(
                    out=pt[:, c, :], in_=hn[:, c * 128:(c + 1) * 128], identity=ident),
                    reads=(bhn, bid), writes=(bPS[pi],))
            self.cp("dve", ht[:, :, j * 128:(j + 1) * 128], pt, (bPS[pi],), (bht,))
        return ht, bht

    def resid_ctx(self, A):
        return {"XR": [(A.alloc([128, D], F32), Buf("xr")) for _ in range(2)]}

    def out_proj(self, R, t, js, steps, xin, xout, scale, banks=(4, 5)):
        PS, bPS = self.PS, self.bPS
        n = len(steps)
        for j in js:
            r0 = t * TT + j * 128
            xr, bxr = R["XR"][j % 2]
            self.dma("pool", xr, xin[r0:r0 + 128, :], writes=(bxr,), stream="xr", ring=2)
            for h in range(2):
                po = banks[h]
                for i, (lf, rf, rd) in enumerate(steps):
                    self.mm(PS[po], lf(j), rf(h), i == 0, i == n - 1, rd, (bPS[po],))
                self.stt(xr[:, h * 512:(h + 1) * 512], PS[po], float(scale), xr[:, h * 512:(h + 1) * 512],
                         ALU.mult, ALU.add, (bPS[po], bxr), (bxr,))
            self.dma("sp", xout[r0:r0 + 128, :], xr, reads=(bxr,), stream="xst", ring=2)

    def temp_pool(self, A, n32, n16):
        self.T32 = [(A.alloc([128, TT], F32), Buf("t32")) for _ in range(n32)]
        self.T16 = [(A.alloc([128, TT], BF16), Buf("t16")) for _ in range(n16)]
        self.t32_i = 0
        self.t16_i = 0

    def t32(self):
        r = self.T32[self.t32_i % len(self.T32)]
        self.t32_i += 1
        return r

    def t16(self):
        r = self.T16[self.t16_i % len(self.T16)]
        self.t16_i += 1
        return r

    def rstd_fm(self, ps_sum, bps, P, inv_n):
        L, bL = self.t32()
        self.act(L[0:P, :], ps_sum, AF.Ln, (bps,), (bL,), scale=float(inv_n), bias=EPS)
        self.act(L[0:P, :], L[0:P, :], AF.Exp, (bL,), (bL,), scale=-0.5)
        return L, bL

    def fm_norm(self, srcs, P, G, bG, nbank, inv_n, outs, gcol=None, bgcol=None):
        PS, bPS = self.PS, self.bPS
        xs = []
        for i, (ps, bps) in enumerate(srcs):
            X, bX = self.t32()
            Q, bQ = self.t16()
            self.act(X[0:P, :], ps, AF.Copy, (bps,), (bX,))
            self.act(Q[0:P, :], ps, AF.Square, (bps,), (bQ,))
            xs.append((X, bX, Q, bQ))
        pn = PS[nbank][0:P, :]
        for i, (X, bX, Q, bQ) in enumerate(xs):
            self.mm(pn, G, Q[0:P, :], i == 0, i == len(xs) - 1, (bG, bQ), (bPS[nbank],))
        Rr, bR = self.rstd_fm(pn, bPS[nbank], P, inv_n)
        for (X, bX, Q, bQ), (o, bo) in zip(xs, outs):
            if gcol is None:
                self.tt("dve", o, X[0:P, :], Rr[0:P, :], ALU.mult, (bX, bR), (bo,))
            else:
                self.stt(o, X[0:P, :], gcol, Rr[0:P, :], ALU.mult, ALU.mult, (bX, bR, bgcol), (bo,))

    def ffn_phase(self, xin, xout, norm_g, wg, wu, wd):
        S, A = self.S_, self.arena
        m0 = A.mark()
        WG = A.alloc([128, 8, DFF], BF16)
        WU = A.alloc([128, 8, DFF], BF16)
        WD = A.alloc([128, NFC, D], BF16)
        bWG, bWU, bWD = Buf("WG"), Buf("WU"), Buf("WD")
        stage = self.new_stage(A, 2)
        gcol, bg = self.load_col(A, norm_g, 8)
        C = self.norm_ctx(A, use_ln=False)
        R = self.resid_ctx(A)
        ACTT = A.alloc([128, NFC, TT], BF16)
        bACT = [Buf("act%d" % c) for c in range(NFC)]
        SG = [(A.alloc([128, TT], BF16), Buf("sg")) for _ in range(2)]
        for c in range(8):
            self.load_rows(stage, WG[:, c, :], bWG, wg[c * 128:(c + 1) * 128, :], gcol[:, c:c + 1], bg)
        for c in range(8):
            self.load_rows(stage, WU[:, c, :], bWU, wu[c * 128:(c + 1) * 128, :], gcol[:, c:c + 1], bg)
        for c in range(NFC):
            self.load_rows(stage, WD[:, c, :], bWD, wd[c * 128:(c + 1) * 128, :])
        PS, bPS = self.PS, self.bPS
        ntiles = self.NT // TT

        def src(t):
            return lambda j: xin[t * TT + j * 128:t * TT + (j + 1) * 128, :]

        def gateup(t):
            ht, bht = C["HT"][t % 2]
            for c in range(NFC):
                pg, pu = c % 2, 2 + c % 2
                for k in range(8):
                    self.mm(PS[pg], WG[:, k, c * 128:(c + 1) * 128], ht[:, k, :], k == 0, k == 7,
                            (bWG, bht), (bPS[pg],))
                for k in range(8):
                    self.mm(PS[pu], WU[:, k, c * 128:(c + 1) * 128], ht[:, k, :], k == 0, k == 7,
                            (bWU, bht), (bPS[pu],))
                sg, bsg = SG[c % 2]
                self.act(sg, PS[pg], AF.Silu, (bPS[pg],), (bsg,))
                self.tt("dve", ACTT[:, c, :], PS[pu], sg, ALU.mult, (bPS[pu], bsg), (bACT[c],))

        def down(t, js):
            steps = [((lambda j, c=c: ACTT[:, c, j * 128:(j + 1) * 128]),
                      (lambda h, c=c: WD[:, c, h * 512:(h + 1) * 512]),
                      (bWD, bACT[c])) for c in range(NFC)]
            self.out_proj(R, t, js, steps, xin, xout, 0.5)

        self.norm1(C, 0, src(0))
        self.norm2(C, 0)
        for t in range(ntiles):
            gateup(t)
            if t + 1 < ntiles:
                self.norm1(C, t + 1, src(t + 1))
            down(t, (0, 1))
            if t + 1 < ntiles:
                self.norm2(C, t + 1)
            down(t, (2, 3))
        S.barrier()
        A.reset(m0)

    def rope_phase(self, positions):
        S, A = self.S_, self.arena
        m0 = A.mark()
        ns, SQ = self.n_seq, self.S
        self.CM = self.scratch("s_cm", [ns, 64, SQ], F32)
        self.SM = self.scratch("s_sm", [ns, 64, SQ], F32)
        self.CR = self.scratch("s_cr", [ns, 128, SQ], F32)
        self.SR = self.scratch("s_sr", [ns, 128, SQ], F32)
        inv, binv = self.load_const(A, self.c["c_inv"], [128, 2], F32)
        POS = [(A.alloc([128, TT], I32), Buf("pos")) for _ in range(2)]
        PF = [(A.alloc([128, TT], F32), Buf("pf")) for _ in range(2)]
        U = [(A.alloc([128, TT], F32), Buf("u")) for _ in range(2)]
        KI = [(A.alloc([128, TT], I32), Buf("ki")) for _ in range(2)]
        KF = [(A.alloc([128, TT], F32), Buf("kf")) for _ in range(2)]
        OUT = [(A.alloc([128, TT], F32), Buf("ro")) for _ in range(4)]
        n = 0
        for s in range(ns):
            for ti in range(self.TPS):
                c0 = ti * TT
                pos, bpos = POS[(s * self.TPS + ti) % 2]
                pf, bpf = PF[(s * self.TPS + ti) % 2]
                self.dma("sp", pos, positions[s:s + 1, c0:c0 + TT].partition_broadcast(128), writes=(bpos,),
                         stream="misc", ring=4)
                self.cp("dve", pf, pos, (bpos,), (bpf,))
                for typ, P, cdst, sdst in ((0, 64, self.CM, self.SM), (1, 128, self.CR, self.SR)):
                    for off, dst in ((0.25, cdst), (0.0, sdst)):
                        u, bu = U[n % 2]
                        ki, bki = KI[n % 2]
                        kf, bkf = KF[n % 2]
                        o, bo = OUT[n % 4]
                        n += 1
                        self.ts(u[0:P, :], pf[0:P, :], inv[0:P, typ:typ + 1], off, ALU.mult, ALU.add,
                                (bpf, binv), (bu,))
                        self.cp("dve", ki[0:P, :], u[0:P, :], (bu,), (bki,))
                        self.cp("dve", kf[0:P, :], ki[0:P, :], (bki,), (bkf,))
                        self.tt("dve", u[0:P, :], u[0:P, :], kf[0:P, :], ALU.subtract, (bu, bkf), (bu,))
                        self.ts(kf[0:P, :], u[0:P, :], 0.5, None, ALU.is_gt, None, (bu,), (bkf,))
                        self.tt("dve", u[0:P, :], u[0:P, :], kf[0:P, :], ALU.subtract, (bu, bkf), (bu,))
                        self.act(o[0:P, :], u[0:P, :], AF.Sin, (bu,), (bo,), scale=TWO_PI)
                        self.dma("sp", dst[s, :, c0:c0 + TT], o[0:P, :], reads=(bo,), stream="rst", ring=4)
        S.barrier()
        A.reset(m0)

    def xattn_phase(self, xin, xout, mem, xnorm, mnorm, wq, wk, wv, wo, qn, kn):
        S, A = self.S_, self.arena
        PS, bPS = self.PS, self.bPS
        m0 = A.mark()
        stage = self.new_stage(A, 2)
        WQ = A.alloc([128, 8, D], BF16)
        WO = A.alloc([128, 8, D], BF16)
        WK = A.alloc([128, 8, D], BF16)
        WV = A.alloc([128, 8, D], BF16)
        bWQ, bWO, bWK, bWV = Buf("wq"), Buf("wo"), Buf("wk"), Buf("wv")
        gx, bgx = self.load_col(A, xnorm, 8)
        gm, bgm = self.load_col(A, mnorm, 8)
        gq, bgq = self.load_col(A, qn, 2)
        gk, bgk = self.load_col(A, kn, 2)
        self.ts(gk[:, 0:2], gk[:, 0:2], 1.0 / 16.0, None, ALU.mult, None, (bgk,), (bgk,))
        ones, bones = self.load_const(A, self.c["c_ones"], [128, 128], BF16)
        C = self.norm_ctx(A, use_ln=True)
        R = self.resid_ctx(A)
        self.temp_pool(A, 12, 8)
        for c in range(8):
            self.load_rows(stage, WK[:, c, :], bWK, wk[c * 128:(c + 1) * 128, :], gm[:, c:c + 1], bgm)
            self.load_rows(stage, WV[:, c, :], bWV, wv[c * 128:(c + 1) * 128, :], gm[:, c:c + 1], bgm)
        for c in range(8):
            self.load_rows(stage, WQ[:, c, :], bWQ, wq[c * 128:(c + 1) * 128, :], gx[:, c:c + 1], bgx)
            self.load_rows(stage, WO[:, c, :], bWO, wo[c * 128:(c + 1) * 128, :])
        KN = [(A.alloc([128, 8, MEM], BF16), Buf("kn")) for _ in range(self.n_seq)]
        VM = [(A.alloc([128, 2, D], BF16), Buf("vm")) for _ in range(self.n_seq)]
        QN = (A.alloc([128, 8, TT], BF16), Buf("qn"))
        PT = [(A.alloc([128, TT], BF16), Buf("p")) for _ in range(8)]
        OT = A.alloc([128, 8, TT], BF16)
        bOT = [Buf("ot%d" % c) for c in range(8)]
        RD = [(A.alloc([128, TT], F32), Buf("rd")) for _ in range(2)]
        for s in range(self.n_seq):
            self.norm1(C, s, lambda j, s=s: mem[s, j * 128:(j + 1) * 128, :], nsub=2)
            mt, bmt = self.norm2(C, s, nsub=2)
            kn_t, bkn = KN[s]
            vm_t, bvm = VM[s]
            for hh in range(4):
                srcs = []
                for dc in range(2):
                    c = 2 * hh + dc
                    b = dc
                    for k in range(8):
                        self.mm(PS[b][:, 0:MEM], WK[:, k, c * 128:(c + 1) * 128], mt[:, k, 0:MEM], k == 0, k == 7,
                                (bWK, bmt), (bPS[b],))
                    srcs.append((PS[b][:, 0:MEM], bPS[b]))
                xs = []
                for (ps, bps) in srcs:
                    X, bX = self.t32()
                    Q, bQ = self.t16()
                    self.act(X[:, 0:MEM], ps, AF.Copy, (bps,), (bX,))
                    self.act(Q[:, 0:MEM], ps, AF.Square, (bps,), (bQ,))
                    xs.append((X, bX, Q, bQ))
                for i, (X, bX, Q, bQ) in enumerate(xs):
                    self.mm(PS[2][:, 0:MEM], ones, Q[:, 0:MEM], i == 0, i == 1, (bones, bQ), (bPS[2],))
                L, bL = self.t32()
                self.act(L[:, 0:MEM], PS[2][:, 0:MEM], AF.Ln, (bPS[2],), (bL,), scale=1.0 / 256, bias=EPS)
                self.act(L[:, 0:MEM], L[:, 0:MEM], AF.Exp, (bL,), (bL,), scale=-0.5)
                for dc, (X, bX, Q, bQ) in enumerate(xs):
                    self.stt(kn_t[:, 2 * hh + dc, :], X[:, 0:MEM], gk[:, dc:dc + 1], L[:, 0:MEM], ALU.mult, ALU.mult,
                             (bX, bL, bgk), (bkn,))
            for mc in range(2):
                for h in range(2):
                    b = 4 + h
                    for k in range(8):
                        self.mm(PS[b], mt[:, k, mc * 128:(mc + 1) * 128], WV[:, k, h * 512:(h + 1) * 512],
                                k == 0, k == 7, (bWV, bmt), (bPS[b],))
                    self.cp("dve", vm_t[:, mc, h * 512:(h + 1) * 512], PS[b], (bPS[b],), (bvm,))
        ntiles = self.NT // TT

        def src(t):
            return lambda j: xin[t * TT + j * 128:t * TT + (j + 1) * 128, :]

        def attend(t, slot):
            s = t // self.TPS
            ht, bht = C["HT"][slot % 2]
            kn_t, bkn = KN[s]
            vm_t, bvm = VM[s]
            qn_t, bqn = QN
            xs = []
            for c in range(8):
                b = c % 2
                for k in range(8):
                    self.mm(PS[b], WQ[:, k, c * 128:(c + 1) * 128], ht[:, k, :], k == 0, k == 7,
                            (bWQ, bht), (bPS[b],))
                X, bX = self.t32()
                Q, bQ = self.t16()
                self.act(X, PS[b], AF.Copy, (bPS[b],), (bX,))
                self.act(Q, PS[b], AF.Square, (bPS[b],), (bQ,))
                xs.append((X, bX, Q, bQ))
            for hh in range(4):
                nb = 2 + hh % 2
                for dc in range(2):
                    X, bX, Q, bQ = xs[2 * hh + dc]
                    self.mm(PS[nb], ones, Q, dc == 0, dc == 1, (bones, bQ), (bPS[nb],))
                L, bL = self.rstd_fm(PS[nb], bPS[nb], 128, 1.0 / 256)
                for dc in range(2):
                    X, bX, Q, bQ = xs[2 * hh + dc]
                    self.stt(qn_t[:, 2 * hh + dc, :], X, gq[:, dc:dc + 1], L, ALU.mult, ALU.mult,
                             (bX, bL, bgq), (bqn,))
            ps_ = []
            for hh in range(4):
                for mc in range(2):
                    b = 4 + 2 * (hh % 2) + mc
                    for dc in range(2):
                        self.mm(PS[b], kn_t[:, 2 * hh + dc, mc * 128:(mc + 1) * 128], qn_t[:, 2 * hh + dc, :],
                                dc == 0, dc == 1, (bkn, bqn), (bPS[b],))
                    p, bp = PT[2 * hh + mc]
                    self.act(p, PS[b], AF.Exp, (bPS[b],), (bp,))
                    ps_.append((p, bp))
            for hh in range(4):
                nb = 2 + hh % 2
                for mc in range(2):
                    p, bp = ps_[2 * hh + mc]
                    self.mm(PS[nb], ones, p, mc == 0, mc == 1, (bones, bp), (bPS[nb],))
                rd, brd = RD[hh % 2]
                self.recip(rd, PS[nb], (bPS[nb],), (brd,))
                for dc in range(2):
                    c = 2 * hh + dc
                    b = dc
                    for mc in range(2):
                        p, bp = ps_[2 * hh + mc]
                        self.mm(PS[b], vm_t[:, mc, c * 128:(c + 1) * 128], p, mc == 0, mc == 1,
                                (bvm, bp), (bPS[b],))
                    self.tt("dve", OT[:, c, :], PS[b], rd, ALU.mult, (bPS[b], brd), (bOT[c],))

        def outp(t, js):
            steps = [((lambda j, c=c: OT[:, c, j * 128:(j + 1) * 128]),
                      (lambda h, c=c: WO[:, c, h * 512:(h + 1) * 512]),
                      (bWO, bOT[c])) for c in range(8)]
            self.out_proj(R, t, js, steps, xin, xout, 1.0, banks=(3, 4))

        base = self.n_seq
        self.norm1(C, base + 0, src(0))
        self.norm2(C, base + 0)
        for t in range(ntiles):
            attend(t, base + t)
            if t + 1 < ntiles:
                self.norm1(C, base + t + 1, src(t + 1))
            outp(t, (0, 1))
            if t + 1 < ntiles:
                self.norm2(C, base + t + 1)
            outp(t, (2, 3))
        S.barrier()
        A.reset(m0)

    def build(self):
        nc = self.nc
        NT, L, ns = self.NT, self.depth, self.n_seq
        E, O = (L + 1) // 2, L // 2
        hc = host_consts()
        self.c = {}
        for k, v in hc.items():
            if k.startswith("c_"):
                dt = BF16 if v.dtype != np.float32 else F32
                self.c[k] = self.inp(k, v.shape, dt)
        I = {}
        I["x"] = self.inp("x", [NT, D])
        I["mem"] = self.inp("mem", [ns, MEM, D])
        I["positions"] = self.inp("positions", [ns, self.S], I32)
        for nm, shp in PARAM_SHAPES(L, E, O):
            I[nm] = self.inp(nm, shp)
        y = nc.dram_tensor("y", [NT, D], F32, kind="ExternalOutput").ap()
        self.I = I
        phases = self.phases
        if phases is None:
            phases = [("rope",)]
            for l in range(L):
                phases.append(("ffn1", l))
                phases.append(("even", l) if l % 2 == 0 else ("odd", l))
                phases.append(("xattn", l))
                phases.append(("ffn2", l))
        cur = I["x"]
        for ph in phases:
            kind = ph[0]
            if kind == "rope":
                self.rope_phase(I["positions"])
                continue
            l = ph[1]
            if kind in ("ffn1", "ffn2"):
                self.ffn_phase(cur, y, I[kind + "_norm"][l], I[kind + "_w_gate"][l], I[kind + "_w_up"][l],
                               I[kind + "_w_down"][l])
            elif kind == "xattn":
                self.xattn_phase(cur, y, I["mem"], I["xattn_norm"][l], I["mem_norm"][l], I["xattn_wq"][l],
                                 I["xattn_wk"][l], I["xattn_wv"][l], I["xattn_wo"][l], I["xattn_q_norm"][l],
                                 I["xattn_k_norm"][l])
            elif kind == "even":
                self.even_phase(cur, y, l // 2, l)
            elif kind == "odd":
                self.odd_phase(cur, y, l // 2, l)
            cur = y
        self.S_.finalize()
        return nc


def PARAM_SHAPES(L, E, O):
    return [
        ("ffn1_norm", [L, D]), ("ffn1_w_gate", [L, D, DFF]), ("ffn1_w_up", [L, D, DFF]), ("ffn1_w_down", [L, DFF, D]),
        ("ffn2_norm", [L, D]), ("ffn2_w_gate", [L, D, DFF]), ("ffn2_w_up", [L, D, DFF]), ("ffn2_w_down", [L, DFF, D]),
        ("mix_norm", [L, D]), ("xattn_norm", [L, D]), ("mem_norm", [L, D]),
        ("xattn_wq", [L, D, D]), ("xattn_wk", [L, D, D]), ("xattn_wv", [L, D, D]), ("xattn_wo", [L, D, D]),
        ("xattn_q_norm", [L, 256]), ("xattn_k_norm", [L, 256]),
        ("ev_w_in", [E, D, 1440]), ("ev_conv_w", [E, 31, 512]), ("ev_conv_b", [E, 512]),
        ("ev_conv_ln_g", [E, 512]), ("ev_conv_ln_b", [E, 512]), ("ev_q_a_norm", [E, 256]),
        ("ev_w_q_b", [E, 256, 768]), ("ev_kv_a_norm", [E, 128]), ("ev_w_kv_b", [E, 128, 1024]),
        ("ev_q_nope_norm", [E, 64]), ("ev_k_nope_norm", [E, 64]), ("ev_q_rope_norm", [E, 32]),
        ("ev_k_rope_norm", [E, 32]), ("ev_w_out", [E, D, D]),
        ("od_w_in", [O, D, 6144]), ("od_gn_g", [O, 2048]), ("od_gn_b", [O, 2048]), ("od_w_out", [O, 2048, D]),
    ]


N_CORES = 8


def kernel(**inputs):
    x = np.ascontiguousarray(inputs["x"], dtype=np.float32)
    B, S, _ = x.shape
    ns = B // N_CORES
    L = inputs["ffn1_norm"].shape[0]
    b = Builder(ns, S, depth=L)
    nc = b.build()
    hc = {k: v for k, v in host_consts().items() if k.startswith("c_")}
    shared = {}
    for nm, shp in PARAM_SHAPES(L, (L + 1) // 2, L // 2):
        shared[nm] = np.ascontiguousarray(inputs[nm], dtype=np.float32)
    mem = np.ascontiguousarray(inputs["mem"], dtype=np.float32)
    pos = np.ascontiguousarray(inputs["positions"], dtype=np.int32)
    in_maps = []
    for c in range(N_CORES):
        m = dict(shared)
        m.update(hc)
        m["x"] = x[c * ns:(c + 1) * ns].reshape(ns * S, D)
        m["mem"] = mem[c * ns:(c + 1) * ns]
        m["positions"] = pos[c * ns:(c + 1) * ns]
        in_maps.append(m)
    res = run_bass_kernel_spmd(nc, in_maps, core_ids=list(range(N_CORES)))
    out = np.concatenate([r["y"].reshape(ns, S, D) for r in res.results], axis=0)
    return out.astype(np.float32)


def _even_phase(self, xin, xout, e, l):
    S, A = self.S_, self.arena
    PS, bPS = self.PS, self.bPS
    I = self.I
    ns, SQ, TPS = self.n_seq, self.S, self.TPS
    ntiles = self.NT // TT
    NB = SQ // 128
    SCALE = 96.0 ** -0.5
    if not hasattr(self, "ev_scr"):
        self.ev_scr = dict(
            AT=self.scratch("s_at", [ntiles, 128, 4 * TT], BF16),
            QN=self.scratch("s_qn", [ntiles, 128, 4 * TT], BF16),
            QR=self.scratch("s_qr", [ntiles, 64, 4 * TT], BF16),
            KN=self.scratch("s_kn", [ns, 128, 4, SQ], BF16),
            KR=self.scratch("s_kr", [ns, 64, SQ], BF16),
            V=self.scratch("s_v", [ns, 128, NB, 512], BF16),
        )
    scr = self.ev_scr

    m0 = A.mark()
    stage = self.new_stage(A, 2)
    gmix, bgmix = self.load_col(A, I["mix_norm"][l], 8)
    WIN = A.alloc([128, 8, 1472], BF16)
    bWIN = Buf("win")
    w_in = I["ev_w_in"][e]
    for k in range(8):
        self.load_rows(stage, WIN[:, k, 0:1440], bWIN, w_in[k * 128:(k + 1) * 128, :], gmix[:, k:k + 1], bgmix)
        self.load_rows(stage, WIN[:, k, 1440:1472], bWIN, w_in[k * 128:(k + 1) * 128, 1408:1440],
                       gmix[:, k:k + 1], bgmix)
    gqa, bgqa = self.load_col(A, I["ev_q_a_norm"][e], 2)
    WQN = A.alloc([128, 2, 4, 128], BF16)
    WQR = A.alloc([128, 2, 4, 64], BF16)
    bWQ = Buf("wqb")
    for c in range(2):
        sv, sb = self.stage_load(stage, I["ev_w_q_b"][e][c * 128:(c + 1) * 128, :])
        svh = sv.rearrange("p (h e) -> p h e", e=96)
        self.ts(WQN[:, c].rearrange("p a b -> p (a b)").rearrange("p (h e) -> p h e", e=64), svh[:, :, 0:64],
                gqa[:, c:c + 1], None, ALU.mult, None, (sb, bgqa), (bWQ,))
        self.ts(WQR[:, c].rearrange("p a b -> p (a b)").rearrange("p (h e) -> p h e", e=32), svh[:, :, 64:96],
                gqa[:, c:c + 1], None, ALU.mult, None, (sb, bgqa), (bWQ,))
    gkva, bgkva = self.load_col(A, I["ev_kv_a_norm"][e], 1)
    WKN = A.alloc([128, 4, 128], BF16)
    WVV = A.alloc([128, 512], BF16)
    bWKV = Buf("wkvb")
    sv, sb = self.stage_load(stage, I["ev_w_kv_b"][e])
    svh = sv.rearrange("p (h e) -> p h e", e=128)
    self.ts(WKN.rearrange("p a b -> p (a b)").rearrange("p (h e) -> p h e", e=64), svh[:, :, 0:64],
            gkva[:, 0:1], None, ALU.mult, None, (sb, bgkva), (bWKV,))
    self.ts(WVV.rearrange("p (h e) -> p h e", e=64), svh[:, :, 64:128],
            gkva[:, 0:1], None, ALU.mult, None, (sb, bgkva), (bWKV,))
    ident, bid0 = self.load_const(A, self.c["c_ident"], [128, 128], BF16)
    id32 = A.alloc([128, 128], F32)
    bid32 = Buf("id32")
    self.cp("dve", id32, ident, (bid0,), (bid32,))
    CW = A.alloc([128, 4, 31], F32)
    bCW = Buf("cw")
    for cc in range(4):
        self.dma("sp", CW[:, cc, :], I["ev_conv_w"][e][:, cc * 128:(cc + 1) * 128].rearrange("j p -> p j"),
                 writes=(bCW,), stream="misc", ring=4, allow_slow_non_contiguous=True)
    DIAG = A.alloc([128, 4, 31, 128], BF16)
    bDG = Buf("diag")
    for cc in range(4):
        for j in range(31):
            if (cc * 31 + j) % 2 == 0:
                self.ts(DIAG[:, cc, j, :], id32, CW[:, cc, j:j + 1], None, ALU.mult, None, (bid32, bCW), (bDG,))
            else:
                self.act(DIAG[:, cc, j, :], id32, AF.Copy, (bid32, bCW), (bDG,), scale=CW[:, cc, j:j + 1])
    cb, bcb = self.load_col(A, I["ev_conv_b"][e], 4)
    lng, blng = self.load_col(A, I["ev_conv_ln_g"][e], 4)
    lnb, blnb = self.load_col(A, I["ev_conv_ln_b"][e], 4)
    gqn, bgqn = self.load_col_rep(A, I["ev_q_nope_norm"][e], 64, 2, SCALE)
    gkn, bgkn = self.load_col_rep(A, I["ev_k_nope_norm"][e], 64, 2)
    gqr, bgqr = self.load_col_rep(A, I["ev_q_rope_norm"][e], 32, 2, SCALE)
    gkr, bgkr = self.load_col_rep(A, I["ev_k_rope_norm"][e], 32, 2)
    ones, bones = self.load_const(A, self.c["c_ones"], [128, 128], BF16)
    bd64, bbd64 = self.load_const(A, self.c["c_bd64"], [128, 128], BF16)
    bd32, bbd32 = self.load_const(A, self.c["c_bd32"], [64, 64], BF16)
    rot, brot = self.load_const(A, self.c["c_rot"], [64, 64], F32)
    C = self.norm_ctx(A, use_ln=True)
    self.temp_pool(A, 8, 6)
    ABF = [(A.alloc([128, 30 + TT], BF16), Buf("abf")) for _ in range(4)]
    Y32 = [(A.alloc([128, TT], F32), Buf("y32")) for _ in range(4)]
    YBF = [(A.alloc([128, TT], BF16), Buf("ybf")) for _ in range(4)]
    YSQ = [(A.alloc([128, TT], BF16), Buf("ysq")) for _ in range(4)]
    M32 = (A.alloc([128, TT], F32), Buf("m32"))
    ATt = [(A.alloc([128, 4, TT], BF16), Buf("at")) for _ in range(2)]
    ZQN = [(A.alloc([128, TT], BF16), Buf("zqn")) for _ in range(2)]
    QNt = [(A.alloc([128, 4, TT], BF16), Buf("qnt")) for _ in range(2)]
    QRt = [(A.alloc([128, 4, TT], BF16), Buf("qrt")) for _ in range(2)]
    ZKVN = (A.alloc([128, TT], BF16), Buf("zkvn"))
    KNt = [(A.alloc([128, 4, TT], BF16), Buf("knt")) for _ in range(2)]
    VTt = [(A.alloc([128, 4, 512], BF16), Buf("vtt")) for _ in range(2)]
    KRt = [(A.alloc([128, TT], BF16), Buf("krt")) for _ in range(2)]
    CT = [(A.alloc([128, TT], F32), Buf("ct")) for _ in range(2)]
    ST = [(A.alloc([128, TT], F32), Buf("st")) for _ in range(2)]
    RN = (A.alloc([128, TT], F32), Buf("rn"))

    def src(t):
        return lambda j: xin[t * TT + j * 128:t * TT + (j + 1) * 128, :]

    def rope64(ps, bps, gcol, bgcol, ct, bct, st, bst, out, bout):
        X, bX = self.t32()
        Q, bQ = self.t16()
        self.act(X[0:64, :], ps, AF.Copy, (bps,), (bX,))
        self.act(Q[0:64, :], ps, AF.Square, (bps,), (bQ,))
        self.mm(PS[7][0:64, :], bd32, Q[0:64, :], True, True, (bbd32, bQ), (bPS[7],))
        Rr, bR = self.rstd_fm(PS[7][0:64, :], bPS[7], 64, 1.0 / 32)
        rn, brn = RN
        self.stt(rn[0:64, :], X[0:64, :], gcol[0:64, 0:1], Rr[0:64, :], ALU.mult, ALU.mult, (bX, bR, bgcol), (brn,))
        self.mm(PS[7][0:64, :], rot, rn[0:64, :], True, True, (brot, brn), (bPS[7],))
        T1, bT1 = self.t32()
        T2, bT2 = self.t32()
        self.tt("pool", T1[0:64, :], rn[0:64, :], ct[0:64, :], ALU.mult, (brn, bct), (bT1,))
        self.tt("dve", T2[0:64, :], PS[7][0:64, :], st[0:64, :], ALU.mult, (bPS[7], bst), (bT2,))
        self.tt("pool", out, T1[0:64, :], T2[0:64, :], ALU.add, (bT1, bT2), (bout,))

    def e1_tile(t, slot):
        s, ti = t // TPS, t % TPS
        c0 = ti * TT
        ht, bht = C["HT"][slot % 2]
        ct, bct = CT[t % 2]
        st, bst = ST[t % 2]
        self.dma("sp", ct[0:64, :], self.CM[s, :, c0:c0 + TT], writes=(bct,), stream="tab", ring=4)
        self.dma("sp", st[0:64, :], self.SM[s, :, c0:c0 + TT], writes=(bst,), stream="tab", ring=4)
        for cc in range(4):
            ab, bab = ABF[cc]
            if ti == 0:
                self.memset("pool", ab[:, 0:30], 0.0, (bab,))
            pv, pg = cc % 2, 2 + cc % 2
            for k in range(8):
                self.mm(PS[pv], WIN[:, k, cc * 128:(cc + 1) * 128], ht[:, k, :], k == 0, k == 7, (bWIN, bht), (bPS[pv],))
            for k in range(8):
                self.mm(PS[pg], WIN[:, k, 512 + cc * 128:512 + (cc + 1) * 128], ht[:, k, :], k == 0, k == 7,
                        (bWIN, bht), (bPS[pg],))
            T1, bT1 = self.t32()
            self.act(T1, PS[pg], AF.Exp, (bPS[pg],), (bT1,), scale=-1.0)
            self.ts(T1, T1, 1.0, None, ALU.add, None, (bT1,), (bT1,))
            self.recip(T1, T1, (bT1,), (bT1,))
            self.tt("dve", ab[:, 30:30 + TT], PS[pv], T1, ALU.mult, (bPS[pv], bT1), (bab,))
        for cc in range(4):
            ab, bab = ABF[cc]
            py = 4 + cc % 2
            for j in range(31):
                self.mm(PS[py], DIAG[:, cc, j, :], ab[:, j:j + TT], j == 0, j == 30, (bDG, bab), (bPS[py],))
            y32, by32 = Y32[cc]
            self.ts(y32, PS[py], cb[:, cc:cc + 1], None, ALU.add, None, (bPS[py], bcb), (by32,))
            ybf, bybf = YBF[cc]
            ysq, bysq = YSQ[cc]
            self.cp("pool", ybf, y32, (by32,), (bybf,))
            self.tt("pool", ysq, y32, y32, ALU.mult, (by32,), (bysq,))
            self.cp("pool", ab[:, 0:30], ab[:, TT:TT + 30], (bab,), (bab,))
        for cc in range(4):
            self.mm(PS[0], ones, YBF[cc][0], cc == 0, cc == 3, (bones, YBF[cc][1]), (bPS[0],))
        for cc in range(4):
            self.mm(PS[1], ones, YSQ[cc][0], cc == 0, cc == 3, (bones, YSQ[cc][1]), (bPS[1],))
        m32, bm32 = M32
        self.act(m32, PS[0], AF.Copy, (bPS[0],), (bm32,), scale=1.0 / 512)
        V, bV = self.t32()
        self.tt("pool", V, m32, m32, ALU.mult, (bm32,), (bV,))
        self.stt(V, PS[1], 1.0 / 512, V, ALU.mult, ALU.subtract, (bPS[1], bV), (bV,))
        self.act(V, V, AF.Ln, (bV,), (bV,), bias=EPS)
        self.act(V, V, AF.Exp, (bV,), (bV,), scale=-0.5)
        at, bat = ATt[t % 2]
        for cc in range(4):
            y32, by32 = Y32[cc]
            Z, bZ = self.t32()
            self.tt("pool", Z, y32, m32, ALU.subtract, (by32, bm32), (bZ,))
            self.tt("pool", Z, Z, V, ALU.mult, (bZ, bV), (bZ,))
            self.ts(Z, Z, lng[:, cc:cc + 1], lnb[:, cc:cc + 1], ALU.mult, ALU.add, (bZ, blng, blnb), (bZ,))
            E_, bE = self.t32()
            self.act(E_, Z, AF.Exp, (bZ,), (bE,), scale=-1.0)
            self.ts(E_, E_, 1.0, None, ALU.add, None, (bE,), (bE,))
            self.recip(E_, E_, (bE,), (bE,))
            self.tt("pool", at[:, cc, :], Z, E_, ALU.mult, (bZ, bE), (bat,))
        self.dma("sp", scr["AT"][t].rearrange("p (a b) -> p a b", a=4), at, reads=(bat,), stream="est", ring=4)
        srcs = []
        for c in range(2):
            for k in range(8):
                self.mm(PS[c], WIN[:, k, 1024 + c * 128:1024 + (c + 1) * 128], ht[:, k, :], k == 0, k == 7,
                        (bWIN, bht), (bPS[c],))
            srcs.append((PS[c], bPS[c]))
        self.fm_norm(srcs, 128, ones, bones, 2, 1.0 / 256, [(ZQN[0][0], ZQN[0][1]), (ZQN[1][0], ZQN[1][1])])
        qnt, bqnt = QNt[t % 2]
        for pr in range(4):
            b = pr % 2
            for c in range(2):
                self.mm(PS[b], WQN[:, c, pr, :], ZQN[c][0], c == 0, c == 1, (bWQ, ZQN[c][1]), (bPS[b],))
            self.fm_norm([(PS[b], bPS[b])], 128, bd64, bbd64, 2 + pr % 2, 1.0 / 64, [(qnt[:, pr, :], bqnt)],
                         gqn[:, 0:1], bgqn)
        qrt, bqrt = QRt[t % 2]
        for pr in range(4):
            b = 4 + pr % 2
            for c in range(2):
                self.mm(PS[b][0:64, :], WQR[:, c, pr, :], ZQN[c][0], c == 0, c == 1, (bWQ, ZQN[c][1]), (bPS[b],))
            rope64(PS[b][0:64, :], bPS[b], gqr, bgqr, ct, bct, st, bst, qrt[0:64, pr, :], bqrt)
        self.dma("sp", scr["QN"][t].rearrange("p (a b) -> p a b", a=4), qnt, reads=(bqnt,), stream="est", ring=4)
        self.dma("sp", scr["QR"][t].rearrange("p (a b) -> p a b", a=4), qrt[0:64], reads=(bqrt,), stream="est", ring=4)
        for k in range(8):
            self.mm(PS[0], WIN[:, k, 1280:1408], ht[:, k, :], k == 0, k == 7, (bWIN, bht), (bPS[0],))
        zk, bzk = ZKVN
        self.fm_norm([(PS[0], bPS[0])], 128, ones, bones, 2, 1.0 / 128, [(zk, bzk)])
        knt, bknt = KNt[t % 2]
        for pr in range(4):
            b = pr % 2
            self.mm(PS[b], WKN[:, pr, :], zk, True, True, (bWKV, bzk), (bPS[b],))
            self.fm_norm([(PS[b], bPS[b])], 128, bd64, bbd64, 2 + pr % 2, 1.0 / 64, [(knt[:, pr, :], bknt)],
                         gkn[:, 0:1], bgkn)
        self.dma("sp", scr["KN"][s, :, :, c0:c0 + TT], knt, reads=(bknt,), stream="est", ring=4)
        vtt, bvtt = VTt[t % 2]
        for j in range(4):
            b = 4 + j % 2
            self.mm(PS[b], zk[:, j * 128:(j + 1) * 128], WVV, True, True, (bWKV, bzk), (bPS[b],))
            self.cp("act" if j % 2 else "dve", vtt[:, j, :], PS[b], (bPS[b],), (bvtt,))
        self.dma("sp", scr["V"][s, :, ti * 4:ti * 4 + 4, :], vtt, reads=(bvtt,), stream="est", ring=4)
        for k in range(8):
            self.mm(PS[6][0:64, :], WIN[:, k, 1408:1472], ht[:, k, :], k == 0, k == 7, (bWIN, bht), (bPS[6],))
        krt, bkrt = KRt[t % 2]
        rope64(PS[6][0:64, :], bPS[6], gkr, bgkr, ct, bct, st, bst, krt[0:64, :], bkrt)
        self.dma("sp", scr["KR"][s, :, c0:c0 + TT], krt[0:64, :], reads=(bkrt,), stream="est", ring=4)

    self.norm1(C, 0, src(0))
    self.norm2(C, 0)
    for t in range(ntiles):
        if t + 1 < ntiles:
            self.norm1(C, t + 1, src(t + 1))
        e1_tile(t, t)
        if t + 1 < ntiles:
            self.norm2(C, t + 1)
    S.barrier()
    A.reset(m0)

    m0 = A.mark()
    stage = self.new_stage(A, 2, cols=512)
    WOC = A.alloc([128, 4, D], BF16)
    WOM = A.alloc([128, 8, D], BF16)
    bWOC, bWOM = Buf("woc"), Buf("wom")
    w_out = I["ev_w_out"][e]
    for cc in range(4):
        self.load_rows(stage, WOC[:, cc, :], bWOC, w_out[cc * 128:(cc + 1) * 128, :])
    for h in range(8):
        self.load_rows(stage, WOM[0:64, h, :], bWOM, w_out[512 + h * 64:512 + (h + 1) * 64, :])
    mask, bmask = self.load_const(A, self.c["c_mask"], [128, 128], BF16)
    sel, bsel = self.load_const(A, self.c["c_sel"], [65, 64], F32)
    R = self.resid_ctx(A)
    KH = A.alloc([128, 8, SQ], BF16)
    VA = A.alloc([128, NB, 8, 65], BF16)
    bKH, bVA = Buf("kh"), Buf("va")
    VST = [(A.alloc([128, 8, 512], BF16), Buf("vst")) for _ in range(1)]
    QHl = [(A.alloc([128, 8, TT], BF16), Buf("qhl")) for _ in range(2)]
    ATl = [(A.alloc([128, 4, TT], BF16), Buf("atl")) for _ in range(2)]
    PTl = [(A.alloc([128, TT], BF16), Buf("pt")) for _ in range(3)]
    OS = [(A.alloc([128, TT], F32), Buf("os")) for _ in range(2)]
    RD = [(A.alloc([128, TT], F32), Buf("rd")) for _ in range(2)]
    OT = [(A.alloc([128, 8, TT], BF16), Buf("ot")) for _ in range(2)]
    pcount = 0
    for s in range(ns):
        for h in range(8):
            self.dma("sp", KH[0:64, h, :], scr["KN"][s, (h % 2) * 64:(h % 2 + 1) * 64, h // 2, :], writes=(bKH,),
                     stream="kv", ring=4)
            self.dma("sp", KH[64:96, h, :], scr["KR"][s, 0:32, :], writes=(bKH,), stream="kv", ring=4)
        self.memset("pool", VA, 1.0, (bVA,))
        for g in range(NB // 8):
            vs, bvs = VST[0]
            self.dma("sp", vs, scr["V"][s, :, g * 8:(g + 1) * 8, :], writes=(bvs,), stream="kv", ring=4)
            self.cp("pool", VA[:, g * 8:(g + 1) * 8, :, 0:64], vs.rearrange("p b (h e) -> p b h e", e=64),
                    (bvs,), (bVA,))
        for ti in range(TPS):
            t = s * TPS + ti
            qh, bqh = QHl[t % 2]
            at, bat = ATl[t % 2]
            ot, bot = OT[t % 2]
            qn_s = scr["QN"][t].rearrange("p (a b) -> p a b", a=4)
            qr_s = scr["QR"][t].rearrange("p (a b) -> p a b", a=4)
            for h in range(8):
                self.dma("sp", qh[0:64, h, :], qn_s[(h % 2) * 64:(h % 2 + 1) * 64, h // 2, :], writes=(bqh,),
                         stream="ql", ring=6)
                self.dma("sp", qh[64:96, h, :], qr_s[(h % 2) * 32:(h % 2 + 1) * 32, h // 2, :], writes=(bqh,),
                         stream="ql", ring=6)
            self.dma("sp", at, scr["AT"][t].rearrange("p (a b) -> p a b", a=4), writes=(bat,), stream="ql", ring=6)
            nkb = 4 * ti + 4
            blocks = [(h, kb) for h in range(8) for kb in range(nkb)]
            state = {}

            def qk(i):
                nonlocal pcount
                h, kb = blocks[i]
                pr, hh = h // 2, h % 2
                n0 = max(0, kb - 4 * ti) * 128
                pb = pcount % 2
                p, bp = PTl[pcount % 3]
                pcount += 1
                self.mm(PS[pb][:, n0:TT], KH[0:96, h, kb * 128:(kb + 1) * 128], qh[0:96, h, n0:TT], True, True,
                        (bKH, bqh), (bPS[pb],))
                self.act(p[:, n0:TT], PS[pb][:, n0:TT], AF.Exp, (bPS[pb],), (bp,))
                if kb >= 4 * ti:
                    self.tt("pool", p[:, n0:n0 + 128], p[:, n0:n0 + 128], mask, ALU.mult, (bp, bmask), (bp,))
                state[i] = (p, bp, n0)

            def pv(i):
                h, kb = blocks[i]
                p, bp, n0 = state.pop(i)
                po = 2 + h % 2
                self.mm(PS[po][0:65, n0:TT], VA[:, kb, h, :], p[:, n0:TT], kb == 0, kb == nkb - 1,
                        (bVA, bp), (bPS[po],))

            def epi1(h):
                po = 2 + h % 2
                os_, bos = OS[h % 2]
                self.cp("act", os_[0:65, :], PS[po][0:65, :], (bPS[po],), (bos,))

            def epi2(h):
                os_, bos = OS[h % 2]
                rd, brd = RD[h % 2]
                pd = 4
                self.mm(PS[pd][0:64, :], sel, os_[0:65, :], True, True, (bsel, bos), (bPS[pd],))
                self.recip(rd[0:64, :], PS[pd][0:64, :], (bPS[pd],), (brd,))
                self.tt("pool", ot[0:64, h, :], os_[0:64, :], rd[0:64, :], ALU.mult, (bos, brd), (bot,))

            qk(0)
            pend = []
            for i in range(len(blocks)):
                if i + 1 < len(blocks):
                    qk(i + 1)
                pv(i)
                h, kb = blocks[i]
                for item in list(pend):
                    if item[1] <= i:
                        epi2(item[0])
                        pend.remove(item)
                if kb == nkb - 1:
                    epi1(h)
                    pend.append((h, i + 2))
            for item in pend:
                epi2(item[0])
            steps = [((lambda j, cc=cc, at=at: at[:, cc, j * 128:(j + 1) * 128]),
                      (lambda hf, cc=cc: WOC[:, cc, hf * 512:(hf + 1) * 512]), (bWOC, bat)) for cc in range(4)]
            steps += [((lambda j, h=h, ot=ot: ot[0:64, h, j * 128:(j + 1) * 128]),
                       (lambda hf, h=h: WOM[0:64, h, hf * 512:(hf + 1) * 512]), (bWOM, bot)) for h in range(8)]
            self.out_proj(R, t, (0, 1, 2, 3), steps, xin, xout, 1.0, banks=(6, 7))
    S.barrier()
    A.reset(m0)


Builder.even_phase = _even_phase


def _odd_phase(self, xin, xout, o, l):
    S, A = self.S_, self.arena
    PS, bPS = self.PS, self.bPS
    I = self.I
    ns, SQ, TPS = self.n_seq, self.S, self.TPS
    ntiles = self.NT // TT
    if not hasattr(self, "od_scr"):
        self.od_scr = dict(
            QT=self.scratch("s_rq", [ntiles, 128, 8 * TT], BF16),
            KT=self.scratch("s_rk", [ntiles, 128, 8 * TT], BF16),
            QX=self.scratch("s_rqx", [ntiles, 128, 8 * TT], BF16),
            V=self.scratch("s_rv", [ntiles, 128, 4 * 2048], BF16),
            SG=self.scratch("s_rsg", [ntiles, 128, 4 * 2048], BF16),
        )
    scr = self.od_scr
    w_in = I["od_w_in"][o]

    m0 = A.mark()
    stage = self.new_stage(A, 2)
    gmix, bgmix = self.load_col(A, I["mix_norm"][l], 8)
    gmk = A.alloc([128, 8], F32)
    bgmk = Buf("gmk")
    self.ts(gmk, gmix, 1.0 / 16.0, None, ALU.mult, None, (bgmix,), (bgmk,))
    WIN = A.alloc([128, 8, 6144], BF16)
    bWIN = Buf("win")
    for k in range(8):
        rows = w_in[k * 128:(k + 1) * 128, :]
        self.load_rows(stage, WIN[:, k, 0:1024], bWIN, rows[:, 0:1024], gmix[:, k:k + 1], bgmix)
        self.load_rows(stage, WIN[:, k, 1024:2048], bWIN, rows[:, 1024:2048], gmk[:, k:k + 1], bgmk)
        self.load_rows(stage, WIN[:, k, 2048:6144], bWIN, rows[:, 2048:6144], gmix[:, k:k + 1], bgmix)
    xi, bxi = self.load_const(A, self.c["c_xi"], [128, 4, 512], F32)
    C = self.norm_ctx(A, use_ln=False)
    self.temp_pool(A, 4, 1)
    CT = [(A.alloc([128, TT], F32), Buf("ct")) for _ in range(1)]
    ST = [(A.alloc([128, TT], F32), Buf("st")) for _ in range(1)]
    QTt = [(A.alloc([128, 8, TT], BF16), Buf("qt")) for _ in range(1)]
    KTt = [(A.alloc([128, 8, TT], BF16), Buf("kt")) for _ in range(1)]
    QXt = [(A.alloc([128, 8, TT], BF16), Buf("qx")) for _ in range(1)]
    Q32 = [(A.alloc([128, TT], F32), Buf("q32")) for _ in range(2)]
    VT = [(A.alloc([128, 2048], BF16), Buf("vt")) for _ in range(2)]
    SGT = [(A.alloc([128, 2048], BF16), Buf("sgt")) for _ in range(2)]

    def src(t):
        return lambda j: xin[t * TT + j * 128:t * TT + (j + 1) * 128, :]

    def r1_tile(t, slot):
        s, ti = t // TPS, t % TPS
        c0 = ti * TT
        ht, bht = C["HT"][slot % 2]
        ct, bct = CT[0]
        st, bst = ST[0]
        self.dma("sp", ct, self.CR[s, :, c0:c0 + TT], writes=(bct,), stream="tab", ring=4)
        self.dma("sp", st, self.SR[s, :, c0:c0 + TT], writes=(bst,), stream="tab", ring=4)
        qt, bqt = QTt[0]
        kt, bkt = KTt[0]
        qx, bqx = QXt[0]
        for which, dst, bdst in ((0, qt, bqt), (1, kt, bkt)):
            for h in range(4):
                col = which * 1024 + h * 256
                for dc in range(2):
                    for k in range(8):
                        self.mm(PS[dc], WIN[:, k, col + dc * 128:col + (dc + 1) * 128], ht[:, k, :], k == 0, k == 7,
                                (bWIN, bht), (bPS[dc],))
                T1, b1 = self.t32()
                T2, b2 = self.t32()
                T3, b3 = self.t32()
                T4, b4 = self.t32()
                self.tt("dve", T1, PS[0], ct, ALU.mult, (bPS[0], bct), (b1,))
                self.tt("dve", T2, PS[1], st, ALU.mult, (bPS[1], bst), (b2,))
                self.tt("dve", T3, PS[0], st, ALU.mult, (bPS[0], bst), (b3,))
                self.tt("dve", T4, PS[1], ct, ALU.mult, (bPS[1], bct), (b4,))
                if which == 0:
                    qa, bqa = Q32[0]
                    qb, bqb = Q32[1]
                    self.tt("pool", qa, T1, T2, ALU.subtract, (b1, b2), (bqa,))
                    self.tt("pool", qb, T3, T4, ALU.add, (b3, b4), (bqb,))
                    self.cp("act", dst[:, 2 * h, :], qa, (bqa,), (bdst,))
                    self.cp("act", dst[:, 2 * h + 1, :], qb, (bqb,), (bdst,))
                    self.tt("pool", qx[:, 2 * h, :], qa, xi[:, h, :], ALU.mult, (bqa, bxi), (bqx,))
                    self.tt("pool", qx[:, 2 * h + 1, :], qb, xi[:, h, :], ALU.mult, (bqb, bxi), (bqx,))
                else:
                    self.tt("pool", dst[:, 2 * h, :], T1, T2, ALU.subtract, (b1, b2), (bdst,))
                    self.tt("pool", dst[:, 2 * h + 1, :], T3, T4, ALU.add, (b3, b4), (bdst,))
        self.dma("sp", scr["QT"][t].rearrange("p (a b) -> p a b", a=8), qt, reads=(bqt,), stream="rst1", ring=4)
        self.dma("sp", scr["KT"][t].rearrange("p (a b) -> p a b", a=8), kt, reads=(bkt,), stream="rst1", ring=4)
        self.dma("sp", scr["QX"][t].rearrange("p (a b) -> p a b", a=8), qx, reads=(bqx,), stream="rst1", ring=4)
        for cj in range(4):
            vt, bvt = VT[cj % 2]
            sg, bsg = SGT[cj % 2]
            for h in range(4):
                b = 2 + h % 2
                for k in range(8):
                    self.mm(PS[b], ht[:, k, cj * 128:(cj + 1) * 128], WIN[:, k, 2048 + h * 512:2048 + (h + 1) * 512],
                            k == 0, k == 7, (bWIN, bht), (bPS[b],))
                self.cp("dve" if h % 2 else "act", vt[:, h * 512:(h + 1) * 512], PS[b], (bPS[b],), (bvt,))
            for h in range(4):
                b = 4 + h % 2
                for k in range(8):
                    self.mm(PS[b], ht[:, k, cj * 128:(cj + 1) * 128], WIN[:, k, 4096 + h * 512:4096 + (h + 1) * 512],
                            k == 0, k == 7, (bWIN, bht), (bPS[b],))
                self.act(sg[:, h * 512:(h + 1) * 512], PS[b], AF.Silu, (bPS[b],), (bsg,))
            self.dma("sp", scr["V"][t, :, cj * 2048:(cj + 1) * 2048], vt, reads=(bvt,), stream="rst2", ring=4)
            self.dma("sp", scr["SG"][t, :, cj * 2048:(cj + 1) * 2048], sg, reads=(bsg,), stream="rst2", ring=4)

    self.norm1(C, 0, src(0))
    self.norm2(C, 0)
    for t in range(ntiles):
        if t + 1 < ntiles:
            self.norm1(C, t + 1, src(t + 1))
        r1_tile(t, t)
        if t + 1 < ntiles:
            self.norm2(C, t + 1)
    S.barrier()
    A.reset(m0)

    m0 = A.mark()
    stage = self.new_stage(A, 2)
    WO = A.alloc([128, 16, D], BF16)
    bWO = Buf("wo")
    for c in range(16):
        self.load_rows(stage, WO[:, c, :], bWO, I["od_w_out"][o][c * 128:(c + 1) * 128, :])
    ident, bid = self.load_const(A, self.c["c_ident"], [128, 128], BF16)
    decay, bdec = self.load_const(A, self.c["c_decay"], [128, 4, 128], F32)
    zeta, bzeta = self.load_const(A, self.c["c_zeta"], [128, 4], F32)
    GNG = A.alloc([128, 2048], F32)
    GNB = A.alloc([128, 2048], F32)
    bGN = Buf("gn")
    self.dma("sp", GNG, I["od_gn_g"][o].rearrange("(o n) -> o n", o=1).partition_broadcast(128), writes=(bGN,),
             stream="misc", ring=4)
    self.dma("sp", GNB, I["od_gn_b"][o].rearrange("(o n) -> o n", o=1).partition_broadcast(128), writes=(bGN,),
             stream="misc", ring=4)
    R = self.resid_ctx(A)
    ST32 = A.alloc([128, 4, 2, 512], F32)
    STB = A.alloc([128, 4, 2, 512], BF16)
    bST = [Buf("st32_%d" % h) for h in range(4)]
    bSTB = [Buf("stb_%d" % h) for h in range(4)]
    QTl = [(A.alloc([128, 8, TT], BF16), Buf("qtl")) for _ in range(2)]
    KTl = [(A.alloc([128, 8, TT], BF16), Buf("ktl")) for _ in range(2)]
    QXl = [(A.alloc([128, 8, TT], BF16), Buf("qxl")) for _ in range(2)]
    Vl = [(A.alloc([128, 2048], BF16), Buf("vl")) for _ in range(2)]
    SGl = [(A.alloc([128, 2048], BF16), Buf("sgl")) for _ in range(2)]
    KZ = [(A.alloc([128, 256], BF16), Buf("kz")) for _ in range(2)]
    PTl = [(A.alloc([128, 128], BF16), Buf("ptl")) for _ in range(2)]
    O32 = [(A.alloc([128, 512], F32), Buf("o32")) for _ in range(3)]
    SQJ = (A.alloc([128, 512], BF16), Buf("sqj"))
    STAT = [(A.alloc([128, 8], F32), Buf("stat")) for _ in range(3)]
    OG = [(A.alloc([128, 2048], BF16), Buf("og")) for _ in range(2)]
    OGT = [(A.alloc([128, 16, TT], BF16), Buf("ogt")) for _ in range(2)]
    cnt = 0
    for s in range(ns):
        for h in range(4):
            self.memset("pool", ST32[:, h], 0.0, (bST[h],))
            self.memset("pool", STB[:, h], 0.0, (bSTB[h],))
        for ti in range(TPS):
            t = s * TPS + ti
            qt, bqt = QTl[t % 2]
            kt, bkt = KTl[t % 2]
            qx, bqx = QXl[t % 2]
            ogt, bogt = OGT[t % 2]
            self.dma("sp", qt, scr["QT"][t].rearrange("p (a b) -> p a b", a=8), writes=(bqt,), stream="rl", ring=4)
            self.dma("sp", kt, scr["KT"][t].rearrange("p (a b) -> p a b", a=8), writes=(bkt,), stream="rl", ring=4)
            self.dma("sp", qx, scr["QX"][t].rearrange("p (a b) -> p a b", a=8), writes=(bqx,), stream="rl", ring=4)
            items = [(cj, h) for cj in range(4) for h in range(4)]
            info = {}

            def stage_a(i):
                nonlocal cnt
                cj, h = items[i]
                cs = slice(cj * 128, (cj + 1) * 128)
                vl, bvl = Vl[cj % 2]
                sg, bsg = SGl[cj % 2]
                if h == 0:
                    self.dma("sp", vl, scr["V"][t, :, cj * 2048:(cj + 1) * 2048], writes=(bvl,), stream="rl2", ring=4)
                    self.dma("sp", sg, scr["SG"][t, :, cj * 2048:(cj + 1) * 2048], writes=(bsg,), stream="rl2", ring=4)
                cnt += 1
                k_ = cnt
                pz = 6 + k_ % 2
                ptz = PS[pz].bitcast(BF16)
                for dc in range(2):
                    self.S_.op("pe", lambda eng, ptz=ptz, kt=kt, h=h, dc=dc, cs=cs: eng.transpose(
                        out=ptz[:, dc * 128:(dc + 1) * 128], in_=kt[:, 2 * h + dc, cs], identity=ident),
                        reads=(bkt, bid), writes=(bPS[pz],))
                kz, bkz = KZ[k_ % 2]
                self.act(kz, ptz[:, 0:256], AF.Copy, (bPS[pz], bzeta), (bkz,), scale=zeta[:, h:h + 1])
                pb = 0 + k_ % 2
                for dc in range(2):
                    self.mm(PS[pb][:, 0:128], kt[:, 2 * h + dc, cs], qt[:, 2 * h + dc, cs], dc == 0, dc == 1,
                            (bkt, bqt), (bPS[pb],))
                p, bp = PTl[k_ % 2]
                self.tt("dve", p, PS[pb][:, 0:128], decay[:, h, :], ALU.mult, (bPS[pb], bdec), (bp,))
                info[i] = (k_, kz, bkz, p, bp)

            def stage_b(i):
                cj, h = items[i]
                cs = slice(cj * 128, (cj + 1) * 128)
                vl, bvl = Vl[cj % 2]
                sg, bsg = SGl[cj % 2]
                og, bog = OG[cj % 2]
                k_, kz, bkz, p, bp = info.pop(i)
                po = 2 + k_ % 2
                self.mm(PS[po], p, vl[:, h * 512:(h + 1) * 512], True, False, (bp, bvl), (bPS[po],))
                for dc in range(2):
                    self.mm(PS[po], qx[:, 2 * h + dc, cs], STB[:, h, dc, :], False, dc == 1, (bqx, bSTB[h]),
                            (bPS[po],))
                for dc in range(2):
                    pu = 4 + dc
                    self.mm(PS[pu], kz[:, dc * 128:(dc + 1) * 128], vl[:, h * 512:(h + 1) * 512], True, True,
                            (bkz, bvl), (bPS[pu],))
                    self.stt(ST32[:, h, dc, :], ST32[:, h, dc, :], float(self.gch[h]), PS[pu], ALU.mult, ALU.add,
                             (bPS[pu], bST[h]), (bST[h],))
                self.cp("pool", STB[:, h], ST32[:, h], (bST[h],), (bSTB[h],))
                o32, bo32 = O32[k_ % 3]
                stt_, bstat = STAT[k_ % 3]
                sq, bsq = SQJ
                self.act(o32, PS[po], AF.Copy, (bPS[po],), (bo32, bstat), accum_out=stt_[:, 0:1])
                self.act(sq, PS[po], AF.Square, (bPS[po],), (bsq, bstat), accum_out=stt_[:, 1:2])
                self.ts(stt_[:, 2:3], stt_[:, 0:1], 1.0 / 512, None, ALU.mult, None, (bstat,), (bstat,))
                self.tt("dve", stt_[:, 3:4], stt_[:, 2:3], stt_[:, 2:3], ALU.mult, (bstat,), (bstat,))
                self.stt(stt_[:, 4:5], stt_[:, 1:2], 1.0 / 512, stt_[:, 3:4], ALU.mult, ALU.subtract,
                         (bstat,), (bstat,))
                self.ts(stt_[:, 4:5], stt_[:, 4:5], EPS, None, ALU.add, None, (bstat,), (bstat,))
                self.act(stt_[:, 5:6], stt_[:, 4:5], AF.Sqrt, (bstat,), (bstat,))
                self.recip(stt_[:, 6:7], stt_[:, 5:6], (bstat,), (bstat,))
                self.ts(o32, o32, stt_[:, 2:3], stt_[:, 6:7], ALU.subtract, ALU.mult, (bo32, bstat), (bo32,))
                self.tt("pool", o32, o32, GNG[:, h * 512:(h + 1) * 512], ALU.mult, (bo32, bGN), (bo32,))
                self.tt("pool", o32, o32, GNB[:, h * 512:(h + 1) * 512], ALU.add, (bo32, bGN), (bo32,))
                self.tt("pool", og[:, h * 512:(h + 1) * 512], o32, sg[:, h * 512:(h + 1) * 512], ALU.mult,
                        (bo32, bsg), (bog,))

            def chunk_end(cj):
                cs = slice(cj * 128, (cj + 1) * 128)
                og, bog = OG[cj % 2]
                for g in range(2):
                    pz = 6 + g
                    ptz = PS[pz].bitcast(BF16).rearrange("p (c t) -> p c t", c=8)
                    for c in range(8):
                        self.S_.op("pe", lambda eng, ptz=ptz, og=og, c=c, g=g: eng.transpose(
                            out=ptz[:, c, :], in_=og[:, (g * 8 + c) * 128:(g * 8 + c + 1) * 128], identity=ident),
                            reads=(bog, bid), writes=(bPS[pz],))
                    self.cp("dve" if g else "act", ogt[:, g * 8:(g + 1) * 8, cs], ptz, (bPS[pz],), (bogt,))

            stage_a(0)
            pend = []
            for i in range(len(items)):
                if i + 1 < len(items):
                    stage_a(i + 1)
                stage_b(i)
                for it in list(pend):
                    if it[1] <= i:
                        chunk_end(it[0])
                        pend.remove(it)
                if items[i][1] == 3:
                    pend.append((items[i][0], i + 2))
            for it in pend:
                chunk_end(it[0])
            steps = [((lambda j, c=c, ogt=ogt: ogt[:, c, j * 128:(j + 1) * 128]),
                      (lambda hf, c=c: WO[:, c, hf * 512:(hf + 1) * 512]), (bWO, bogt)) for c in range(16)]
            self.out_proj(R, t, (0, 1, 2, 3), steps, xin, xout, 1.0, banks=(4, 5))
    S.barrier()
    A.reset(m0)


Builder.odd_phase = _odd_phase
```

```python
import numpy as np
import concourse.bass as bass
import concourse.mybir as mybir
from concourse.bass_utils import run_bass_kernel_spmd

F32 = mybir.dt.float32
BF16 = mybir.dt.bfloat16
I32 = mybir.dt.int32
U8 = mybir.dt.uint8
ALU = mybir.AluOpType
AF = mybir.ActivationFunctionType
AX = mybir.AxisListType

ENGS = ("pe", "act", "dve", "pool", "sp")
EPOCH = 20000
DT_SIZE = {F32: 4, BF16: 2, I32: 4, U8: 1}


class Buf:
    __slots__ = ("name", "w", "r")

    def __init__(self, name=""):
        self.name = name
        self.w = None
        self.r = {}


class Sched:
    def __init__(self, nc):
        self.nc = nc
        self.eobj = {"pe": nc.tensor, "act": nc.scalar, "dve": nc.vector,
                     "pool": nc.gpsimd, "sp": nc.sync}
        self.ops = {e: [] for e in ENGS}
        self.streams = {}
        self.sem_handles = {}
        self.nsem = 0

    def _sem(self, key):
        h = self.sem_handles.get(key)
        if h is None:
            h = self.nc.alloc_semaphore("s%d" % self.nsem)
            self.nsem += 1
            self.sem_handles[key] = h
        return h

    @staticmethod
    def _add(deps, key, val):
        if deps.get(key, -1) < val:
            deps[key] = val

    def _deps(self, e, reads, writes):
        deps = {}
        for b in reads:
            if b.w is not None:
                self._add(deps, *b.w)
        for b in writes:
            if b.w is not None:
                self._add(deps, *b.w)
            for k, v in b.r.items():
                self._add(deps, k, v)
        if e == "pe":
            deps.pop(("e", "pe"), None)
        return deps

    def _mark(self, me, reads, writes):
        for b in reads:
            if b.r.get(me[0], -1) < me[1]:
                b.r[me[0]] = me[1]
        for b in writes:
            b.w = me
            b.r = {}

    def op(self, e, fn, reads=(), writes=()):
        idx = len(self.ops[e])
        deps = self._deps(e, reads, writes)
        self.ops[e].append([fn, deps, None])
        self._mark((("e", e), idx), reads, writes)

    def dma(self, q, out_ap, in_ap, reads=(), writes=(), stream="ld", ring=6, **kw):
        st = self.streams.setdefault(stream, [0, ring])
        i = st[0]
        st[0] += 1
        slot = i % st[1]
        gen = i // st[1]
        key = ("d", stream, slot)
        deps = self._deps(q, reads, writes)
        if gen > 0:
            self._add(deps, key, 16 * gen)

        def fn(eng, out_ap=out_ap, in_ap=in_ap, kw=kw):
            return eng.dma_start(out=out_ap, in_=in_ap, **kw)
        self.ops[q].append([fn, deps, key])
        self._mark((key, 16 * (gen + 1)), reads, writes)

    def barrier(self):
        last = {}
        for e in ENGS:
            for i in range(len(self.ops[e]) - 1, -1, -1):
                if self.ops[e][i][2] is None:
                    last[("e", e)] = i
                    break
        for name, (cnt, ring) in self.streams.items():
            for slot in range(min(cnt, ring)):
                n = (cnt - 1 - slot) // ring + 1
                last[("d", name, slot)] = 16 * n
        for e in ENGS:
            d = dict(last)
            if e == "pe":
                d.pop(("e", "pe"), None)
            self.ops[e].append([lambda eng: eng.nop(), d, None])

    def finalize(self):
        nc = self.nc
        need = {e: set() for e in ENGS}
        for e in ENGS:
            for fn, deps, dk in self.ops[e]:
                for k, v in deps.items():
                    if k[0] == "e":
                        need[k[1]].add(v)
        rank = {}
        for e in ENGS:
            r = 0
            for i in sorted(need[e]):
                rank[(e, i)] = r
                r += 1

        def resolve(k, v):
            if k[0] == "e":
                r = rank[(k[1], v)]
                return self._sem(("e", k[1], r // EPOCH)), r % EPOCH + 1
            return self._sem(k), v

        for e in ENGS:
            for fn, deps, dk in self.ops[e]:
                for k, v in deps.items():
                    resolve(k, v)
                if dk is not None:
                    self._sem(dk)

        def emit(e, eng):
            seen = {}
            for i, (fn, deps, dk) in enumerate(self.ops[e]):
                for k, v in deps.items():
                    sem, val = resolve(k, v)
                    sk = id(sem)
                    if seen.get(sk, -1) >= val:
                        continue
                    seen[sk] = val
                    eng.wait_ge(sem, val)
                ins = fn(eng)
                if dk is not None:
                    ins.then_inc(self._sem(dk), 16)
                elif (e, i) in rank:
                    r = rank[(e, i)]
                    ins.then_inc(self._sem(("e", e, r // EPOCH)), 1)

        with nc.Block() as block:
            @block.tensor
            def _(eng):
                emit("pe", eng)

            @block.scalar
            def _(eng):
                emit("act", eng)

            @block.vector
            def _(eng):
                emit("dve", eng)

            @block.gpsimd
            def _(eng):
                emit("pool", eng)

            @block.sync
            def _(eng):
                emit("sp", eng)
        for h in self.sem_handles.values():
            nc.gpsimd.sem_clear(h)
        nc.all_engine_barrier()


class Arena:
    def __init__(self, nc, nbytes):
        self.t = nc.alloc_sbuf_tensor("arena", [128, nbytes], U8)
        self.n = nbytes
        self.off = 0
        self.marks = []

    def alloc(self, shape, dt):
        n = int(np.prod(shape[1:])) * DT_SIZE[dt]
        n_al = (n + 63) // 64 * 64
        assert self.off + n_al <= self.n, ("SBUF arena overflow", self.off, n_al, self.n)
        v = self.t[0:shape[0], self.off:self.off + n].bitcast(dt)
        self.off += n_al
        if len(shape) == 3:
            v = v.rearrange("p (a b) -> p a b", a=shape[1])
        elif len(shape) == 4:
            v = v.rearrange("p (a b c) -> p a b c", a=shape[1], b=shape[2])
        return v

    def mark(self):
        return self.off

    def reset(self, m):
        self.off = m


D = 1024
DFF = 2816
NFC = DFF // 128
EPS = 1e-6
TT = 512
MEM = 256
NWARM = 1
TWO_PI = 6.283184


def host_consts():
    import ml_dtypes
    bf = ml_dtypes.bfloat16
    c = {}
    c["c_ident"] = np.eye(128, dtype=np.float32).astype(bf)
    c["c_ones"] = np.ones((128, 128), np.float32).astype(bf)
    bd64 = np.zeros((128, 128), np.float32)
    bd64[:64, :64] = 1
    bd64[64:, 64:] = 1
    c["c_bd64"] = bd64.astype(bf)
    bd32 = np.zeros((64, 64), np.float32)
    bd32[:32, :32] = 1
    bd32[32:, 32:] = 1
    c["c_bd32"] = bd32.astype(bf)
    rot = np.zeros((64, 64), np.float32)
    for m in range(64):
        if m % 32 < 16:
            rot[m + 16, m] = -1.0
        else:
            rot[m - 16, m] = 1.0
    c["c_rot"] = rot
    sel = np.zeros((65, 64), np.float32)
    sel[64, :] = 1.0
    c["c_sel"] = sel
    mask = np.zeros((128, 128), np.float32)
    for j in range(128):
        mask[j, j:] = 1.0
    c["c_mask"] = mask.astype(bf)
    inv_m = (10000.0 ** (-np.arange(0, 32, 2, dtype=np.float32) / 32)).astype(np.float32)
    inv_r = (10000.0 ** (-np.arange(0, 256, 2, dtype=np.float32) / 256)).astype(np.float32)
    tab = np.zeros((128, 2), np.float32)
    tab[:, 0] = inv_m[np.arange(128) % 16] / (2 * np.pi)
    tab[:, 1] = inv_r / (2 * np.pi)
    c["c_inv"] = tab
    H = 4
    log_g = np.log1p(-np.exp2(-5.0 - np.arange(H, dtype=np.float64)))
    idx = np.arange(128, dtype=np.float64)
    diff = idx[None, :] - idx[:, None]
    dec = np.where(diff >= 0, np.exp(log_g[:, None, None] * np.maximum(diff, 0.0)), 0.0)
    c["c_decay"] = np.ascontiguousarray(dec.transpose(1, 0, 2)).astype(np.float32)
    xi = np.exp(log_g[:, None] * (idx + 1.0))
    c["c_xi"] = np.tile(xi[None, :, None, :], (128, 1, 4, 1)).reshape(128, 4, 512).astype(np.float32)
    zeta = np.exp(log_g[:, None] * (128 - 1.0 - idx))
    c["c_zeta"] = np.ascontiguousarray(zeta.T).astype(np.float32)
    c["_gchunk"] = [float(np.exp(log_g[h] * 128)) for h in range(H)]
    return c


class Builder:
    def __init__(self, n_seq, S, depth=4, phases=None):
        self.n_seq, self.S, self.depth = n_seq, S, depth
        self.NT = n_seq * S
        self.TPS = S // TT
        self.phases = phases
        nc = bass.Bass("TRN2", target_bir_lowering=False)
        self.nc = nc
        self.S_ = Sched(nc)
        self.arena = Arena(nc, 212000)
        self.PS = [nc.alloc_psum_tensor("ps%d" % i, [128, 512], F32)[:, :] for i in range(8)]
        self.bPS = [Buf("ps%d" % i) for i in range(8)]
        self.cast_rr = 0
        self.stage_i = 0
        self.gch = host_consts()["_gchunk"]

    def inp(self, name, shape, dt=F32):
        return self.nc.dram_tensor(name, list(shape), dt, kind="ExternalInput").ap()

    def scratch(self, name, shape, dt):
        return self.nc.dram_tensor(name, list(shape), dt).ap()

    def mm(self, out, lhsT, rhs, start, stop, reads, writes):
        self.S_.op("pe", lambda eng: eng.matmul(out=out, lhsT=lhsT, rhs=rhs, start=start, stop=stop),
                   reads, writes)

    def act(self, out, in_, func, reads, writes, **kw):
        self.S_.op("act", lambda eng: eng.activation(out=out, in_=in_, func=func, **kw), reads, writes)

    def tt(self, e, out, in0, in1, op, reads, writes):
        self.S_.op(e, lambda eng: eng.tensor_tensor(out=out, in0=in0, in1=in1, op=op), reads, writes)

    def ts(self, out, in0, s1, s2, op0, op1, reads, writes, e="dve"):
        if s2 is None:
            self.S_.op(e, lambda eng: eng.tensor_scalar(out=out, in0=in0, scalar1=s1, scalar2=None, op0=op0),
                       reads, writes)
        else:
            self.S_.op(e, lambda eng: eng.tensor_scalar(out=out, in0=in0, scalar1=s1, scalar2=s2,
                                                        op0=op0, op1=op1), reads, writes)

    def stt(self, out, in0, scalar, in1, op0, op1, reads, writes, e="dve"):
        self.S_.op(e, lambda eng: eng.scalar_tensor_tensor(out=out, in0=in0, scalar=scalar, in1=in1,
                                                           op0=op0, op1=op1), reads, writes)

    def cp(self, e, out, in_, reads, writes):
        if e == "act":
            self.S_.op(e, lambda eng: eng.copy(out=out, in_=in_), reads, writes)
        else:
            self.S_.op(e, lambda eng: eng.tensor_copy(out=out, in_=in_), reads, writes)

    def recip(self, out, in_, reads, writes):
        self.S_.op("dve", lambda eng: eng.reciprocal(out=out, in_=in_), reads, writes)

    def memset(self, e, out, val, writes):
        self.S_.op(e, lambda eng: eng.memset(out, val), (), writes)

    def dma(self, q, out, in_, reads=(), writes=(), stream="ld", ring=4, **kw):
        self.S_.dma(q, out, in_, reads=reads, writes=writes, stream=stream, ring=ring, **kw)

    def cast_op(self, out, in_, reads, writes, scale_ap=None):
        S = self.S_
        if scale_ap is None:
            e = ("dve", "pool", "act")[self.cast_rr % 3]
        else:
            e = ("dve", "act")[self.cast_rr % 2]
        self.cast_rr += 1
        if scale_ap is None:
            self.cp(e, out, in_, reads, writes)
        elif e == "act":
            self.act(out, in_, AF.Copy, reads, writes, scale=scale_ap)
        else:
            self.ts(out, in_, scale_ap, None, ALU.mult, None, reads, writes)

    def new_stage(self, A, n=2, cols=1024):
        return [(A.alloc([128, cols], F32), Buf("stg")) for _ in range(n)]

    def stage_load(self, stage, src):
        k = self.stage_i
        self.stage_i += 1
        sb, sbuf = stage[k % len(stage)]
        p, n = src.shape
        q = ("sp", "pool")[k % 2]
        self.dma(q, sb[0:p, 0:n], src, writes=(sbuf,), stream="wld", ring=4)
        return sb[0:p, 0:n], sbuf

    def load_rows(self, stage, dst, dbuf, src, gain=None, gbuf=None):
        p, n = src.shape
        cols = stage[0][0].shape[1]
        for c0 in range(0, n, cols):
            w = min(cols, n - c0)
            sv, sbuf = self.stage_load(stage, src[:, c0:c0 + w])
            rd = (sbuf,) if gain is None else (sbuf, gbuf)
            self.cast_op(dst[:, c0:c0 + w], sv, rd, (dbuf,), gain)

    def load_const(self, A, dram, shape, dt):
        t = A.alloc(list(shape), dt)
        b = Buf("const")
        self.dma("sp", t, dram, writes=(b,), stream="misc", ring=4)
        return t, b

    def load_col(self, A, vec, nchunk, npart=128):
        t = A.alloc([128, nchunk], F32)
        b = Buf("col")
        self.dma("sp", t[0:npart, :], vec.rearrange("(c p) -> p c", p=npart), writes=(b,), stream="misc",
                 ring=4, allow_slow_non_contiguous=True)
        return t, b

    def load_col_rep(self, A, vec, n, reps, scale=None):
        t = A.alloc([128, 1], F32)
        b = Buf("colr")
        for r in range(reps):
            self.dma("sp", t[r * n:(r + 1) * n, :], vec.rearrange("(p o) -> p o", o=1), writes=(b,),
                     stream="misc", ring=4, allow_slow_non_contiguous=True)
        if scale is not None:
            self.ts(t[0:n * reps, :], t[0:n * reps, :], float(scale), None, ALU.mult, None, (b,), (b,))
        return t, b

    def norm_ctx(self, A, use_ln):
        C = {"use_ln": use_ln}
        C["XN"] = [(A.alloc([128, D], F32), Buf("xn")) for _ in range(2)]
        C["HN"] = [(A.alloc([128, D], BF16), Buf("hn")) for _ in range(4)]
        C["HT"] = [(A.alloc([128, 8, TT], BF16), Buf("ht")) for _ in range(2)]
        C["SS"] = [(A.alloc([128, 16], F32), Buf("ss")) for _ in range(2)]
        C["ident"], C["bid"] = self.load_const(A, self.c["c_ident"], [128, 128], BF16)
        C["x"] = 0
        C["pt"] = 0
        return C

    def norm1(self, C, t, src, nsub=4):
        ss, bss = C["SS"][t % 2]
        for j in range(nsub):
            xn, bxn = C["XN"][C["x"] % 2]
            C["x"] += 1
            hn, bhn = C["HN"][j]
            self.dma("sp", xn, src(j), writes=(bxn,), stream="xn", ring=2)
            self.act(hn, xn, AF.Square, (bxn,), (bhn, bss), accum_out=ss[:, j:j + 1])
        if C["use_ln"]:
            self.act(ss[:, 8:8 + nsub], ss[:, 0:nsub], AF.Ln, (bss,), (bss,), scale=1.0 / D, bias=EPS)
            self.act(ss[:, 12:12 + nsub], ss[:, 8:8 + nsub], AF.Exp, (bss,), (bss,), scale=-0.5)
        else:
            self.ts(ss[:, 4:4 + nsub], ss[:, 0:nsub], 1.0 / D, EPS, ALU.mult, ALU.add, (bss,), (bss,))
            self.act(ss[:, 8:8 + nsub], ss[:, 4:4 + nsub], AF.Sqrt, (bss,), (bss,))
            self.recip(ss[:, 12:12 + nsub], ss[:, 8:8 + nsub], (bss,), (bss,))
        for j in range(nsub):
            xn, bxn = C["XN"][C["x"] % 2]
            C["x"] += 1
            hn, bhn = C["HN"][j]
            self.dma("sp", xn, src(j), writes=(bxn,), stream="xn", ring=2)
            self.act(hn, xn, AF.Copy, (bxn, bss), (bhn,), scale=ss[:, 12 + j:13 + j])

    def norm2(self, C, t, nsub=4):
        ht, bht = C["HT"][t % 2]
        PS, bPS = self.PS, self.bPS
        ident, bid = C["ident"], C["bid"]
        for j in range(nsub):
            hn, bhn = C["HN"][j]
            pi = 6 + C["pt"] % 2
            C["pt"] += 1
            pt = PS[pi].bitcast(BF16).rearrange("p (c t) -> p c t", c=8)
            for c in range(8):
                self.S_.op("pe", lambda eng, pt=pt, hn=hn, c=c: eng.transpose(
                    out=pt[:, c, :], in_=hn[:, c * 128:(c + 1) * 128], identity=ident),
                    reads=(bhn, bid), writes=(bPS[pi],))
            self.cp("dve", ht[:, :, j * 128:(j + 1) * 128], pt, (bPS[pi],), (bht,))
        return ht, bht

    def resid_ctx(self, A):
        return {"XR": [(A.alloc([128, D], F32), Buf("xr")) for _ in range(2)]}

    def out_proj(self, R, t, js, steps, xin, xout, scale, banks=(4, 5)):
        PS, bPS = self.PS, self.bPS
        n = len(steps)
        for j in js:
            r0 = t * TT + j * 128
            xr, bxr = R["XR"][j % 2]
            self.dma("pool", xr, xin[r0:r0 + 128, :], writes=(bxr,), stream="xr", ring=2)
            for h in range(2):
                po = banks[h]
                for i, (lf, rf, rd) in enumerate(steps):
                    self.mm(PS[po], lf(j), rf(h), i == 0, i == n - 1, rd, (bPS[po],))
                self.stt(xr[:, h * 512:(h + 1) * 512], PS[po], float(scale), xr[:, h * 512:(h + 1) * 512],
                         ALU.mult, ALU.add, (bPS[po], bxr), (bxr,))
            self.dma("sp", xout[r0:r0 + 128, :], xr, reads=(bxr,), stream="xst", ring=2)

    def temp_pool(self, A, n32, n16):
        self.T32 = [(A.alloc([128, TT], F32), Buf("t32")) for _ in range(n32)]
        self.T16 = [(A.alloc([128, TT], BF16), Buf("t16")) for _ in range(n16)]
        self.t32_i = 0
        self.t16_i = 0

    def t32(self):
        r = self.T32[self.t32_i % len(self.T32)]
        self.t32_i += 1
        return r

    def t16(self):
        r = self.T16[self.t16_i % len(self.T16)]
        self.t16_i += 1
        return r

    def rstd_fm(self, ps_sum, bps, P, inv_n):
        L, bL = self.t32()
        self.act(L[0:P, :], ps_sum, AF.Ln, (bps,), (bL,), scale=float(inv_n), bias=EPS)
        self.act(L[0:P, :], L[0:P, :], AF.Exp, (bL,), (bL,), scale=-0.5)
        return L, bL

    def fm_norm(self, srcs, P, G, bG, nbank, inv_n, outs, gcol=None, bgcol=None):
        PS, bPS = self.PS, self.bPS
        xs = []
        for i, (ps, bps) in enumerate(srcs):
            X, bX = self.t32()
            Q, bQ = self.t16()
            self.act(X[0:P, :], ps, AF.Copy, (bps,), (bX,))
            self.act(Q[0:P, :], ps, AF.Square, (bps,), (bQ,))
            xs.append((X, bX, Q, bQ))
        pn = PS[nbank][0:P, :]
        for i, (X, bX, Q, bQ) in enumerate(xs):
            self.mm(pn, G, Q[0:P, :], i == 0, i == len(xs) - 1, (bG, bQ), (bPS[nbank],))
        Rr, bR = self.rstd_fm(pn, bPS[nbank], P, inv_n)
        for (X, bX, Q, bQ), (o, bo) in zip(xs, outs):
            if gcol is None:
                self.tt("dve", o, X[0:P, :], Rr[0:P, :], ALU.mult, (bX, bR), (bo,))
            else:
                self.stt(o, X[0:P, :], gcol, Rr[0:P, :], ALU.mult, ALU.mult, (bX, bR, bgcol), (bo,))

    def ffn_phase(self, xin, xout, norm_g, wg, wu, wd):
        S, A = self.S_, self.arena
        m0 = A.mark()
        WG = A.alloc([128, 8, DFF], BF16)
        WU = A.alloc([128, 8, DFF], BF16)
        WD = A.alloc([128, NFC, D], BF16)
        bWG, bWU, bWD = Buf("WG"), Buf("WU"), Buf("WD")
        stage = self.new_stage(A, 2)
        gcol, bg = self.load_col(A, norm_g, 8)
        C = self.norm_ctx(A, use_ln=False)
        R = self.resid_ctx(A)
        ACTT = A.alloc([128, NFC, TT], BF16)
        bACT = [Buf("act%d" % c) for c in range(NFC)]
        SG = [(A.alloc([128, TT], BF16), Buf("sg")) for _ in range(2)]
        for c in range(8):
            self.load_rows(stage, WG[:, c, :], bWG, wg[c * 128:(c + 1) * 128, :], gcol[:, c:c + 1], bg)
        for c in range(8):
            self.load_rows(stage, WU[:, c, :], bWU, wu[c * 128:(c + 1) * 128, :], gcol[:, c:c + 1], bg)
        for c in range(NFC):
            self.load_rows(stage, WD[:, c, :], bWD, wd[c * 128:(c + 1) * 128, :])
        PS, bPS = self.PS, self.bPS
        ntiles = self.NT // TT

        def src(t):
            return lambda j: xin[t * TT + j * 128:t * TT + (j + 1) * 128, :]

        def gateup(t):
            ht, bht = C["HT"][t % 2]
            for c in range(NFC):
                pg, pu = c % 2, 2 + c % 2
                for k in range(8):
                    self.mm(PS[pg], WG[:, k, c * 128:(c + 1) * 128], ht[:, k, :], k == 0, k == 7,
                            (bWG, bht), (bPS[pg],))
                for k in range(8):
                    self.mm(PS[pu], WU[:, k, c * 128:(c + 1) * 128], ht[:, k, :], k == 0, k == 7,
                            (bWU, bht), (bPS[pu],))
                sg, bsg = SG[c % 2]
                self.act(sg, PS[pg], AF.Silu, (bPS[pg],), (bsg,))
                self.tt("dve", ACTT[:, c, :], PS[pu], sg, ALU.mult, (bPS[pu], bsg), (bACT[c],))

        def down(t, js):
            steps = [((lambda j, c=c: ACTT[:, c, j * 128:(j + 1) * 128]),
                      (lambda h, c=c: WD[:, c, h * 512:(h + 1) * 512]),
                      (bWD, bACT[c])) for c in range(NFC)]
            self.out_proj(R, t, js, steps, xin, xout, 0.5)

        self.norm1(C, 0, src(0))
        self.norm2(C, 0)
        for t in range(ntiles):
            gateup(t)
            if t + 1 < ntiles:
                self.norm1(C, t + 1, src(t + 1))
            down(t, (0, 1))
            if t + 1 < ntiles:
                self.norm2(C, t + 1)
            down(t, (2, 3))
        S.barrier()
        A.reset(m0)

    def rope_phase(self, positions):
        S, A = self.S_, self.arena
        m0 = A.mark()
        ns, SQ = self.n_seq, self.S
        self.CM = self.scratch("s_cm", [ns, 64, SQ], F32)
        self.SM = self.scratch("s_sm", [ns, 64, SQ], F32)
        self.CR = self.scratch("s_cr", [ns, 128, SQ], F32)
        self.SR = self.scratch("s_sr", [ns, 128, SQ], F32)
        inv, binv = self.load_const(A, self.c["c_inv"], [128, 2], F32)
        POS = [(A.alloc([128, TT], I32), Buf("pos")) for _ in range(2)]
        PF = [(A.alloc([128, TT], F32), Buf("pf")) for _ in range(2)]
        U = [(A.alloc([128, TT], F32), Buf("u")) for _ in range(2)]
        KI = [(A.alloc([128, TT], I32), Buf("ki")) for _ in range(2)]
        KF = [(A.alloc([128, TT], F32), Buf("kf")) for _ in range(2)]
        OUT = [(A.alloc([128, TT], F32), Buf("ro")) for _ in range(4)]
        n = 0
        for s in range(ns):
            for ti in range(self.TPS):
                c0 = ti * TT
                pos, bpos = POS[(s * self.TPS + ti) % 2]
                pf, bpf = PF[(s * self.TPS + ti) % 2]
                self.dma("sp", pos, positions[s:s + 1, c0:c0 + TT].partition_broadcast(128), writes=(bpos,),
                         stream="misc", ring=4)
                self.cp("dve", pf, pos, (bpos,), (bpf,))
                for typ, P, cdst, sdst in ((0, 64, self.CM, self.SM), (1, 128, self.CR, self.SR)):
                    for off, dst in ((0.25, cdst), (0.0, sdst)):
                        u, bu = U[n % 2]
                        ki, bki = KI[n % 2]
                        kf, bkf = KF[n % 2]
                        o, bo = OUT[n % 4]
                        n += 1
                        self.ts(u[0:P, :], pf[0:P, :], inv[0:P, typ:typ + 1], off, ALU.mult, ALU.add,
                                (bpf, binv), (bu,))
                        self.cp("dve", ki[0:P, :], u[0:P, :], (bu,), (bki,))
                        self.cp("dve", kf[0:P, :], ki[0:P, :], (bki,), (bkf,))
                        self.tt("dve", u[0:P, :], u[0:P, :], kf[0:P, :], ALU.subtract, (bu, bkf), (bu,))
                        self.ts(kf[0:P, :], u[0:P, :], 0.5, None, ALU.is_gt, None, (bu,), (bkf,))
                        self.tt("dve", u[0:P, :], u[0:P, :], kf[0:P, :], ALU.subtract, (bu, bkf), (bu,))
                        self.act(o[0:P, :], u[0:P, :], AF.Sin, (bu,), (bo,), scale=TWO_PI)
                        self.dma("sp", dst[s, :, c0:c0 + TT], o[0:P, :], reads=(bo,), stream="rst", ring=4)
        S.barrier()
        A.reset(m0)

    def xattn_phase(self, xin, xout, mem, xnorm, mnorm, wq, wk, wv, wo, qn, kn):
        S, A = self.S_, self.arena
        PS, bPS = self.PS, self.bPS
        m0 = A.mark()
        stage = self.new_stage(A, 2)
        WQ = A.alloc([128, 8, D], BF16)
        WO = A.alloc([128, 8, D], BF16)
        WK = A.alloc([128, 8, D], BF16)
        WV = A.alloc([128, 8, D], BF16)
        bWQ, bWO, bWK, bWV = Buf("wq"), Buf("wo"), Buf("wk"), Buf("wv")
        gx, bgx = self.load_col(A, xnorm, 8)
        gm, bgm = self.load_col(A, mnorm, 8)
        gq, bgq = self.load_col(A, qn, 2)
        gk, bgk = self.load_col(A, kn, 2)
        self.ts(gk[:, 0:2], gk[:, 0:2], 1.0 / 16.0, None, ALU.mult, None, (bgk,), (bgk,))
        ones, bones = self.load_const(A, self.c["c_ones"], [128, 128], BF16)
        C = self.norm_ctx(A, use_ln=True)
        R = self.resid_ctx(A)
        self.temp_pool(A, 12, 8)
        for c in range(8):
            self.load_rows(stage, WK[:, c, :], bWK, wk[c * 128:(c + 1) * 128, :], gm[:, c:c + 1], bgm)
            self.load_rows(stage, WV[:, c, :], bWV, wv[c * 128:(c + 1) * 128, :], gm[:, c:c + 1], bgm)
        for c in range(8):
            self.load_rows(stage, WQ[:, c, :], bWQ, wq[c * 128:(c + 1) * 128, :], gx[:, c:c + 1], bgx)
            self.load_rows(stage, WO[:, c, :], bWO, wo[c * 128:(c + 1) * 128, :])
        KN = [(A.alloc([128, 8, MEM], BF16), Buf("kn")) for _ in range(self.n_seq)]
        VM = [(A.alloc([128, 2, D], BF16), Buf("vm")) for _ in range(self.n_seq)]
        QN = (A.alloc([128, 8, TT], BF16), Buf("qn"))
        PT = [(A.alloc([128, TT], BF16), Buf("p")) for _ in range(8)]
        OT = A.alloc([128, 8, TT], BF16)
        bOT = [Buf("ot%d" % c) for c in range(8)]
        RD = [(A.alloc([128, TT], F32), Buf("rd")) for _ in range(2)]
        for s in range(self.n_seq):
            self.norm1(C, s, lambda j, s=s: mem[s, j * 128:(j + 1) * 128, :], nsub=2)
            mt, bmt = self.norm2(C, s, nsub=2)
            kn_t, bkn = KN[s]
            vm_t, bvm = VM[s]
            for hh in range(4):
                srcs = []
                for dc in range(2):
                    c = 2 * hh + dc
                    b = dc
                    for k in range(8):
                        self.mm(PS[b][:, 0:MEM], WK[:, k, c * 128:(c + 1) * 128], mt[:, k, 0:MEM], k == 0, k == 7,
                                (bWK, bmt), (bPS[b],))
                    srcs.append((PS[b][:, 0:MEM], bPS[b]))
                xs = []
                for (ps, bps) in srcs:
                    X, bX = self.t32()
                    Q, bQ = self.t16()
                    self.act(X[:, 0:MEM], ps, AF.Copy, (bps,), (bX,))
                    self.act(Q[:, 0:MEM], ps, AF.Square, (bps,), (bQ,))
                    xs.append((X, bX, Q, bQ))
                for i, (X, bX, Q, bQ) in enumerate(xs):
                    self.mm(PS[2][:, 0:MEM], ones, Q[:, 0:MEM], i == 0, i == 1, (bones, bQ), (bPS[2],))
                L, bL = self.t32()
                self.act(L[:, 0:MEM], PS[2][:, 0:MEM], AF.Ln, (bPS[2],), (bL,), scale=1.0 / 256, bias=EPS)
                self.act(L[:, 0:MEM], L[:, 0:MEM], AF.Exp, (bL,), (bL,), scale=-0.5)
                for dc, (X, bX, Q, bQ) in enumerate(xs):
                    self.stt(kn_t[:, 2 * hh + dc, :], X[:, 0:MEM], gk[:, dc:dc + 1], L[:, 0:MEM], ALU.mult, ALU.mult,
                             (bX, bL, bgk), (bkn,))
            for mc in range(2):
                for h in range(2):
                    b = 4 + h
                    for k in range(8):
                        self.mm(PS[b], mt[:, k, mc * 128:(mc + 1) * 128], WV[:, k, h * 512:(h + 1) * 512],
                                k == 0, k == 7, (bWV, bmt), (bPS[b],))
                    self.cp("dve", vm_t[:, mc, h * 512:(h + 1) * 512], PS[b], (bPS[b],), (bvm,))
        ntiles = self.NT // TT

        def src(t):
            return lambda j: xin[t * TT + j * 128:t * TT + (j + 1) * 128, :]

        def attend(t, slot):
            s = t // self.TPS
            ht, bht = C["HT"][slot % 2]
            kn_t, bkn = KN[s]
            vm_t, bvm = VM[s]
            qn_t, bqn = QN
            xs = []
            for c in range(8):
                b = c % 2
                for k in range(8):
                    self.mm(PS[b], WQ[:, k, c * 128:(c + 1) * 128], ht[:, k, :], k == 0, k == 7,
                            (bWQ, bht), (bPS[b],))
                X, bX = self.t32()
                Q, bQ = self.t16()
                self.act(X, PS[b], AF.Copy, (bPS[b],), (bX,))
                self.act(Q, PS[b], AF.Square, (bPS[b],), (bQ,))
                xs.append((X, bX, Q, bQ))
            for hh in range(4):
                nb = 2 + hh % 2
                for dc in range(2):
                    X, bX, Q, bQ = xs[2 * hh + dc]
                    self.mm(PS[nb], ones, Q, dc == 0, dc == 1, (bones, bQ), (bPS[nb],))
                L, bL = self.rstd_fm(PS[nb], bPS[nb], 128, 1.0 / 256)
                for dc in range(2):
                    X, bX, Q, bQ = xs[2 * hh + dc]
                    self.stt(qn_t[:, 2 * hh + dc, :], X, gq[:, dc:dc + 1], L, ALU.mult, ALU.mult,
                             (bX, bL, bgq), (bqn,))
            ps_ = []
            for hh in range(4):
                for mc in range(2):
                    b = 4 + 2 * (hh % 2) + mc
                    for dc in range(2):
                        self.mm(PS[b], kn_t[:, 2 * hh + dc, mc * 128:(mc + 1) * 128], qn_t[:, 2 * hh + dc, :],
                                dc == 0, dc == 1, (bkn, bqn), (bPS[b],))
                    p, bp = PT[2 * hh + mc]
                    self.act(p, PS[b], AF.Exp, (bPS[b],), (bp,))
                    ps_.append((p, bp))
            for hh in range(4):
                nb = 2 + hh % 2
                for mc in range(2):
                    p, bp = ps_[2 * hh + mc]
                    self.mm(PS[nb], ones, p, mc == 0, mc == 1, (bones, bp), (bPS[nb],))
                rd, brd = RD[hh % 2]
                self.recip(rd, PS[nb], (bPS[nb],), (brd,))
                for dc in range(2):
                    c = 2 * hh + dc
                    b = dc
                    for mc in range(2):
                        p, bp = ps_[2 * hh + mc]
                        self.mm(PS[b], vm_t[:, mc, c * 128:(c + 1) * 128], p, mc == 0, mc == 1,
                                (bvm, bp), (bPS[b],))
                    self.tt("dve", OT[:, c, :], PS[b], rd, ALU.mult, (bPS[b], brd), (bOT[c],))

        def outp(t, js):
            steps = [((lambda j, c=c: OT[:, c, j * 128:(j + 1) * 128]),
                      (lambda h, c=c: WO[:, c, h * 512:(h + 1) * 512]),
                      (bWO, bOT[c])) for c in range(8)]
            self.out_proj(R, t, js, steps, xin, xout, 1.0, banks=(3, 4))

        base = self.n_seq
        self.norm1(C, base + 0, src(0))
        self.norm2(C, base + 0)
        for t in range(ntiles):
            attend(t, base + t)
            if t + 1 < ntiles:
                self.norm1(C, base + t + 1, src(t + 1))
            outp(t, (0, 1))
            if t + 1 < ntiles:
                self.norm2(C, base + t + 1)
            outp(t, (2, 3))
        S.barrier()
        A.reset(m0)

    def build(self):
        nc = self.nc
        NT, L, ns = self.NT, self.depth, self.n_seq
        E, O = (L + 1) // 2, L // 2
        hc = host_consts()
        self.c = {}
        for k, v in hc.items():
            if k.startswith("c_"):
                dt = BF16 if v.dtype != np.float32 else F32
                self.c[k] = self.inp(k, v.shape, dt)
        I = {}
        I["x"] = self.inp("x", [NT, D])
        I["mem"] = self.inp("mem", [ns, MEM, D])
        I["positions"] = self.inp("positions", [ns, self.S], I32)
        for nm, shp in PARAM_SHAPES(L, E, O):
            I[nm] = self.inp(nm, shp)
        y = nc.dram_tensor("y", [NT, D], F32, kind="ExternalOutput").ap()
        self.I = I
        phases = self.phases
        if phases is None:
            phases = [("rope",)]
            for l in range(L):
                phases.append(("ffn1", l))
                phases.append(("even", l) if l % 2 == 0 else ("odd", l))
                phases.append(("xattn", l))
                phases.append(("ffn2", l))
        cur = I["x"]
        for ph in phases:
            kind = ph[0]
            if kind == "rope":
                self.rope_phase(I["positions"])
                continue
            l = ph[1]
            if kind in ("ffn1", "ffn2"):
                self.ffn_phase(cur, y, I[kind + "_norm"][l], I[kind + "_w_gate"][l], I[kind + "_w_up"][l],
                               I[kind + "_w_down"][l])
            elif kind == "xattn":
                self.xattn_phase(cur, y, I["mem"], I["xattn_norm"][l], I["mem_norm"][l], I["xattn_wq"][l],
                                 I["xattn_wk"][l], I["xattn_wv"][l], I["xattn_wo"][l], I["xattn_q_norm"][l],
                                 I["xattn_k_norm"][l])
            elif kind == "even":
                self.even_phase(cur, y, l // 2, l)
            elif kind == "odd":
                self.odd_phase(cur, y, l // 2, l)
            cur = y
        self.S_.finalize()
        return nc


def PARAM_SHAPES(L, E, O):
    return [
        ("ffn1_norm", [L, D]), ("ffn1_w_gate", [L, D, DFF]), ("ffn1_w_up", [L, D, DFF]), ("ffn1_w_down", [L, DFF, D]),
        ("ffn2_norm", [L, D]), ("ffn2_w_gate", [L, D, DFF]), ("ffn2_w_up", [L, D, DFF]), ("ffn2_w_down", [L, DFF, D]),
        ("mix_norm", [L, D]), ("xattn_norm", [L, D]), ("mem_norm", [L, D]),
        ("xattn_wq", [L, D, D]), ("xattn_wk", [L, D, D]), ("xattn_wv", [L, D, D]), ("xattn_wo", [L, D, D]),
        ("xattn_q_norm", [L, 256]), ("xattn_k_norm", [L, 256]),
        ("ev_w_in", [E, D, 1440]), ("ev_conv_w", [E, 31, 512]), ("ev_conv_b", [E, 512]),
        ("ev_conv_ln_g", [E, 512]), ("ev_conv_ln_b", [E, 512]), ("ev_q_a_norm", [E, 256]),
        ("ev_w_q_b", [E, 256, 768]), ("ev_kv_a_norm", [E, 128]), ("ev_w_kv_b", [E, 128, 1024]),
        ("ev_q_nope_norm", [E, 64]), ("ev_k_nope_norm", [E, 64]), ("ev_q_rope_norm", [E, 32]),
        ("ev_k_rope_norm", [E, 32]), ("ev_w_out", [E, D, D]),
        ("od_w_in", [O, D, 6144]), ("od_gn_g", [O, 2048]), ("od_gn_b", [O, 2048]), ("od_w_out", [O, 2048, D]),
    ]


N_CORES = 8


def kernel(**inputs):
    x = np.ascontiguousarray(inputs["x"], dtype=np.float32)
    B, S, _ = x.shape
    ns = B // N_CORES
    L = inputs["ffn1_norm"].shape[0]
    b = Builder(ns, S, depth=L)
    nc = b.build()
    hc = {k: v for k, v in host_consts().items() if k.startswith("c_")}
    shared = {}
    for nm, shp in PARAM_SHAPES(L, (L + 1) // 2, L // 2):
        shared[nm] = np.ascontiguousarray(inputs[nm], dtype=np.float32)
    mem = np.ascontiguousarray(inputs["mem"], dtype=np.float32)
    pos = np.ascontiguousarray(inputs["positions"], dtype=np.int32)
    in_maps = []
    for c in range(N_CORES):
        m = dict(shared)
        m.update(hc)
        m["x"] = x[c * ns:(c + 1) * ns].reshape(ns * S, D)
        m["mem"] = mem[c * ns:(c + 1) * ns]
        m["positions"] = pos[c * ns:(c + 1) * ns]
        in_maps.append(m)
    res = run_bass_kernel_spmd(nc, in_maps, core_ids=list(range(N_CORES)))
    out = np.concatenate([r["y"].reshape(ns, S, D) for r in res.results], axis=0)
    return out.astype(np.float32)


def _even_phase(self, xin, xout, e, l):
    S, A = self.S_, self.arena
    PS, bPS = self.PS, self.bPS
    I = self.I
    ns, SQ, TPS = self.n_seq, self.S, self.TPS
    ntiles = self.NT // TT
    NB = SQ // 128
    SCALE = 96.0 ** -0.5
    if not hasattr(self, "ev_scr"):
        self.ev_scr = dict(
            AT=self.scratch("s_at", [ntiles, 128, 4 * TT], BF16),
            QN=self.scratch("s_qn", [ntiles, 128, 4 * TT], BF16),
            QR=self.scratch("s_qr", [ntiles, 64, 4 * TT], BF16),
            KN=self.scratch("s_kn", [ns, 128, 4, SQ], BF16),
            KR=self.scratch("s_kr", [ns, 64, SQ], BF16),
            V=self.scratch("s_v", [ns, 128, NB, 512], BF16),
        )
    scr = self.ev_scr

    m0 = A.mark()
    stage = self.new_stage(A, 2)
    gmix, bgmix = self.load_col(A, I["mix_norm"][l], 8)
    WIN = A.alloc([128, 8, 1472], BF16)
    bWIN = Buf("win")
    w_in = I["ev_w_in"][e]
    for k in range(8):
        self.load_rows(stage, WIN[:, k, 0:1440], bWIN, w_in[k * 128:(k + 1) * 128, :], gmix[:, k:k + 1], bgmix)
        self.load_rows(stage, WIN[:, k, 1440:1472], bWIN, w_in[k * 128:(k + 1) * 128, 1408:1440],
                       gmix[:, k:k + 1], bgmix)
    gqa, bgqa = self.load_col(A, I["ev_q_a_norm"][e], 2)
    WQN = A.alloc([128, 2, 4, 128], BF16)
    WQR = A.alloc([128, 2, 4, 64], BF16)
    bWQ = Buf("wqb")
    for c in range(2):
        sv, sb = self.stage_load(stage, I["ev_w_q_b"][e][c * 128:(c + 1) * 128, :])
        svh = sv.rearrange("p (h e) -> p h e", e=96)
        self.ts(WQN[:, c].rearrange("p a b -> p (a b)").rearrange("p (h e) -> p h e", e=64), svh[:, :, 0:64],
                gqa[:, c:c + 1], None, ALU.mult, None, (sb, bgqa), (bWQ,))
        self.ts(WQR[:, c].rearrange("p a b -> p (a b)").rearrange("p (h e) -> p h e", e=32), svh[:, :, 64:96],
                gqa[:, c:c + 1], None, ALU.mult, None, (sb, bgqa), (bWQ,))
    gkva, bgkva = self.load_col(A, I["ev_kv_a_norm"][e], 1)
    WKN = A.alloc([128, 4, 128], BF16)
    WVV = A.alloc([128, 512], BF16)
    bWKV = Buf("wkvb")
    sv, sb = self.stage_load(stage, I["ev_w_kv_b"][e])
    svh = sv.rearrange("p (h e) -> p h e", e=128)
    self.ts(WKN.rearrange("p a b -> p (a b)").rearrange("p (h e) -> p h e", e=64), svh[:, :, 0:64],
            gkva[:, 0:1], None, ALU.mult, None, (sb, bgkva), (bWKV,))
    self.ts(WVV.rearrange("p (h e) -> p h e", e=64), svh[:, :, 64:128],
            gkva[:, 0:1], None, ALU.mult, None, (sb, bgkva), (bWKV,))
    ident, bid0 = self.load_const(A, self.c["c_ident"], [128, 128], BF16)
    id32 = A.alloc([128, 128], F32)
    bid32 = Buf("id32")
    self.cp("dve", id32, ident, (bid0,), (bid32,))
    CW = A.alloc([128, 4, 31], F32)
    bCW = Buf("cw")
    for cc in range(4):
        self.dma("sp", CW[:, cc, :], I["ev_conv_w"][e][:, cc * 128:(cc + 1) * 128].rearrange("j p -> p j"),
                 writes=(bCW,), stream="misc", ring=4, allow_slow_non_contiguous=True)
    DIAG = A.alloc([128, 4, 31, 128], BF16)
    bDG = Buf("diag")
    for cc in range(4):
        for j in range(31):
            if (cc * 31 + j) % 2 == 0:
                self.ts(DIAG[:, cc, j, :], id32, CW[:, cc, j:j + 1], None, ALU.mult, None, (bid32, bCW), (bDG,))
            else:
                self.act(DIAG[:, cc, j, :], id32, AF.Copy, (bid32, bCW), (bDG,), scale=CW[:, cc, j:j + 1])
    cb, bcb = self.load_col(A, I["ev_conv_b"][e], 4)
    lng, blng = self.load_col(A, I["ev_conv_ln_g"][e], 4)
    lnb, blnb = self.load_col(A, I["ev_conv_ln_b"][e], 4)
    gqn, bgqn = self.load_col_rep(A, I["ev_q_nope_norm"][e], 64, 2, SCALE)
    gkn, bgkn = self.load_col_rep(A, I["ev_k_nope_norm"][e], 64, 2)
    gqr, bgqr = self.load_col_rep(A, I["ev_q_rope_norm"][e], 32, 2, SCALE)
    gkr, bgkr = self.load_col_rep(A, I["ev_k_rope_norm"][e], 32, 2)
    ones, bones = self.load_const(A, self.c["c_ones"], [128, 128], BF16)
    bd64, bbd64 = self.load_const(A, self.c["c_bd64"], [128, 128], BF16)
    bd32, bbd32 = self.load_const(A, self.c["c_bd32"], [64, 64], BF16)
    rot, brot = self.load_const(A, self.c["c_rot"], [64, 64], F32)
    C = self.norm_ctx(A, use_ln=True)
    self.temp_pool(A, 8, 6)
    ABF = [(A.alloc([128, 30 + TT], BF16), Buf("abf")) for _ in range(4)]
    Y32 = [(A.alloc([128, TT], F32), Buf("y32")) for _ in range(4)]
    YBF = [(A.alloc([128, TT], BF16), Buf("ybf")) for _ in range(4)]
    YSQ = [(A.alloc([128, TT], BF16), Buf("ysq")) for _ in range(4)]
    M32 = (A.alloc([128, TT], F32), Buf("m32"))
    ATt = [(A.alloc([128, 4, TT], BF16), Buf("at")) for _ in range(2)]
    ZQN = [(A.alloc([128, TT], BF16), Buf("zqn")) for _ in range(2)]
    QNt = [(A.alloc([128, 4, TT], BF16), Buf("qnt")) for _ in range(2)]
    QRt = [(A.alloc([128, 4, TT], BF16), Buf("qrt")) for _ in range(2)]
    ZKVN = (A.alloc([128, TT], BF16), Buf("zkvn"))
    KNt = [(A.alloc([128, 4, TT], BF16), Buf("knt")) for _ in range(2)]
    VTt = [(A.alloc([128, 4, 512], BF16), Buf("vtt")) for _ in range(2)]
    KRt = [(A.alloc([128, TT], BF16), Buf("krt")) for _ in range(2)]
    CT = [(A.alloc([128, TT], F32), Buf("ct")) for _ in range(2)]
    ST = [(A.alloc([128, TT], F32), Buf("st")) for _ in range(2)]
    RN = (A.alloc([128, TT], F32), Buf("rn"))

    def src(t):
        return lambda j: xin[t * TT + j * 128:t * TT + (j + 1) * 128, :]

    def rope64(ps, bps, gcol, bgcol, ct, bct, st, bst, out, bout):
        X, bX = self.t32()
        Q, bQ = self.t16()
        self.act(X[0:64, :], ps, AF.Copy, (bps,), (bX,))
        self.act(Q[0:64, :], ps, AF.Square, (bps,), (bQ,))
        self.mm(PS[7][0:64, :], bd32, Q[0:64, :], True, True, (bbd32, bQ), (bPS[7],))
        Rr, bR = self.rstd_fm(PS[7][0:64, :], bPS[7], 64, 1.0 / 32)
        rn, brn = RN
        self.stt(rn[0:64, :], X[0:64, :], gcol[0:64, 0:1], Rr[0:64, :], ALU.mult, ALU.mult, (bX, bR, bgcol), (brn,))
        self.mm(PS[7][0:64, :], rot, rn[0:64, :], True, True, (brot, brn), (bPS[7],))
        T1, bT1 = self.t32()
        T2, bT2 = self.t32()
        self.tt("pool", T1[0:64, :], rn[0:64, :], ct[0:64, :], ALU.mult, (brn, bct), (bT1,))
        self.tt("dve", T2[0:64, :], PS[7][0:64, :], st[0:64, :], ALU.mult, (bPS[7], bst), (bT2,))
        self.tt("pool", out, T1[0:64, :], T2[0:64, :], ALU.add, (bT1, bT2), (bout,))

    def e1_tile(t, slot):
        s, ti = t // TPS, t % TPS
        c0 = ti * TT
        ht, bht = C["HT"][slot % 2]
        ct, bct = CT[t % 2]
        st, bst = ST[t % 2]
        self.dma("sp", ct[0:64, :], self.CM[s, :, c0:c0 + TT], writes=(bct,), stream="tab", ring=4)
        self.dma("sp", st[0:64, :], self.SM[s, :, c0:c0 + TT], writes=(bst,), stream="tab", ring=4)
        for cc in range(4):
            ab, bab = ABF[cc]
            if ti == 0:
                self.memset("pool", ab[:, 0:30], 0.0, (bab,))
            pv, pg = cc % 2, 2 + cc % 2
            for k in range(8):
                self.mm(PS[pv], WIN[:, k, cc * 128:(cc + 1) * 128], ht[:, k, :], k == 0, k == 7, (bWIN, bht), (bPS[pv],))
            for k in range(8):
                self.mm(PS[pg], WIN[:, k, 512 + cc * 128:512 + (cc + 1) * 128], ht[:, k, :], k == 0, k == 7,
                        (bWIN, bht), (bPS[pg],))
            T1, bT1 = self.t32()
            self.act(T1, PS[pg], AF.Exp, (bPS[pg],), (bT1,), scale=-1.0)
            self.ts(T1, T1, 1.0, None, ALU.add, None, (bT1,), (bT1,))
            self.recip(T1, T1, (bT1,), (bT1,))
            self.tt("dve", ab[:, 30:30 + TT], PS[pv], T1, ALU.mult, (bPS[pv], bT1), (bab,))
        for cc in range(4):
            ab, bab = ABF[cc]
            py = 4 + cc % 2
            for j in range(31):
                self.mm(PS[py], DIAG[:, cc, j, :], ab[:, j:j + TT], j == 0, j == 30, (bDG, bab), (bPS[py],))
            y32, by32 = Y32[cc]
            self.ts(y32, PS[py], cb[:, cc:cc + 1], None, ALU.add, None, (bPS[py], bcb), (by32,))
            ybf, bybf = YBF[cc]
            ysq, bysq = YSQ[cc]
            self.cp("pool", ybf, y32, (by32,), (bybf,))
            self.tt("pool", ysq, y32, y32, ALU.mult, (by32,), (bysq,))
            self.cp("pool", ab[:, 0:30], ab[:, TT:TT + 30], (bab,), (bab,))
        for cc in range(4):
            self.mm(PS[0], ones, YBF[cc][0], cc == 0, cc == 3, (bones, YBF[cc][1]), (bPS[0],))
        for cc in range(4):
            self.mm(PS[1], ones, YSQ[cc][0], cc == 0, cc == 3, (bones, YSQ[cc][1]), (bPS[1],))
        m32, bm32 = M32
        self.act(m32, PS[0], AF.Copy, (bPS[0],), (bm32,), scale=1.0 / 512)
        V, bV = self.t32()
        self.tt("pool", V, m32, m32, ALU.mult, (bm32,), (bV,))
        self.stt(V, PS[1], 1.0 / 512, V, ALU.mult, ALU.subtract, (bPS[1], bV), (bV,))
        self.act(V, V, AF.Ln, (bV,), (bV,), bias=EPS)
        self.act(V, V, AF.Exp, (bV,), (bV,), scale=-0.5)
        at, bat = ATt[t % 2]
        for cc in range(4):
            y32, by32 = Y32[cc]
            Z, bZ = self.t32()
            self.tt("pool", Z, y32, m32, ALU.subtract, (by32, bm32), (bZ,))
            self.tt("pool", Z, Z, V, ALU.mult, (bZ, bV), (bZ,))
            self.ts(Z, Z, lng[:, cc:cc + 1], lnb[:, cc:cc + 1], ALU.mult, ALU.add, (bZ, blng, blnb), (bZ,))
            E_, bE = self.t32()
            self.act(E_, Z, AF.Exp, (bZ,), (bE,), scale=-1.0)
            self.ts(E_, E_, 1.0, None, ALU.add, None, (bE,), (bE,))
            self.recip(E_, E_, (bE,), (bE,))
            self.tt("pool", at[:, cc, :], Z, E_, ALU.mult, (bZ, bE), (bat,))
        self.dma("sp", scr["AT"][t].rearrange("p (a b) -> p a b", a=4), at, reads=(bat,), stream="est", ring=4)
        srcs = []
        for c in range(2):
            for k in range(8):
                self.mm(PS[c], WIN[:, k, 1024 + c * 128:1024 + (c + 1) * 128], ht[:, k, :], k == 0, k == 7,
                        (bWIN, bht), (bPS[c],))
            srcs.append((PS[c], bPS[c]))
        self.fm_norm(srcs, 128, ones, bones, 2, 1.0 / 256, [(ZQN[0][0], ZQN[0][1]), (ZQN[1][0], ZQN[1][1])])
        qnt, bqnt = QNt[t % 2]
        for pr in range(4):
            b = pr % 2
            for c in range(2):
                self.mm(PS[b], WQN[:, c, pr, :], ZQN[c][0], c == 0, c == 1, (bWQ, ZQN[c][1]), (bPS[b],))
            self.fm_norm([(PS[b], bPS[b])], 128, bd64, bbd64, 2 + pr % 2, 1.0 / 64, [(qnt[:, pr, :], bqnt)],
                         gqn[:, 0:1], bgqn)
        qrt, bqrt = QRt[t % 2]
        for pr in range(4):
            b = 4 + pr % 2
            for c in range(2):
                self.mm(PS[b][0:64, :], WQR[:, c, pr, :], ZQN[c][0], c == 0, c == 1, (bWQ, ZQN[c][1]), (bPS[b],))
            rope64(PS[b][0:64, :], bPS[b], gqr, bgqr, ct, bct, st, bst, qrt[0:64, pr, :], bqrt)
        self.dma("sp", scr["QN"][t].rearrange("p (a b) -> p a b", a=4), qnt, reads=(bqnt,), stream="est", ring=4)
        self.dma("sp", scr["QR"][t].rearrange("p (a b) -> p a b", a=4), qrt[0:64], reads=(bqrt,), stream="est", ring=4)
        for k in range(8):
            self.mm(PS[0], WIN[:, k, 1280:1408], ht[:, k, :], k == 0, k == 7, (bWIN, bht), (bPS[0],))
        zk, bzk = ZKVN
        self.fm_norm([(PS[0], bPS[0])], 128, ones, bones, 2, 1.0 / 128, [(zk, bzk)])
        knt, bknt = KNt[t % 2]
        for pr in range(4):
            b = pr % 2
            self.mm(PS[b], WKN[:, pr, :], zk, True, True, (bWKV, bzk), (bPS[b],))
            self.fm_norm([(PS[b], bPS[b])], 128, bd64, bbd64, 2 + pr % 2, 1.0 / 64, [(knt[:, pr, :], bknt)],
                         gkn[:, 0:1], bgkn)
        self.dma("sp", scr["KN"][s, :, :, c0:c0 + TT], knt, reads=(bknt,), stream="est", ring=4)
        vtt, bvtt = VTt[t % 2]
        for j in range(4):
            b = 4 + j % 2
            self.mm(PS[b], zk[:, j * 128:(j + 1) * 128], WVV, True, True, (bWKV, bzk), (bPS[b],))
            self.cp("act" if j % 2 else "dve", vtt[:, j, :], PS[b], (bPS[b],), (bvtt,))
        self.dma("sp", scr["V"][s, :, ti * 4:ti * 4 + 4, :], vtt, reads=(bvtt,), stream="est", ring=4)
        for k in range(8):
            self.mm(PS[6][0:64, :], WIN[:, k, 1408:1472], ht[:, k, :], k == 0, k == 7, (bWIN, bht), (bPS[6],))
        krt, bkrt = KRt[t % 2]
        rope64(PS[6][0:64, :], bPS[6], gkr, bgkr, ct, bct, st, bst, krt[0:64, :], bkrt)
        self.dma("sp", scr["KR"][s, :, c0:c0 + TT], krt[0:64, :], reads=(bkrt,), stream="est", ring=4)

    self.norm1(C, 0, src(0))
    self.norm2(C, 0)
    for t in range(ntiles):
        if t + 1 < ntiles:
            self.norm1(C, t + 1, src(t + 1))
        e1_tile(t, t)
        if t + 1 < ntiles:
            self.norm2(C, t + 1)
    S.barrier()
    A.reset(m0)

    m0 = A.mark()
    stage = self.new_stage(A, 2, cols=512)
    WOC = A.alloc([128, 4, D], BF16)
    WOM = A.alloc([128, 8, D], BF16)
    bWOC, bWOM = Buf("woc"), Buf("wom")
    w_out = I["ev_w_out"][e]
    for cc in range(4):
        self.load_rows(stage, WOC[:, cc, :], bWOC, w_out[cc * 128:(cc + 1) * 128, :])
    for h in range(8):
        self.load_rows(stage, WOM[0:64, h, :], bWOM, w_out[512 + h * 64:512 + (h + 1) * 64, :])
    mask, bmask = self.load_const(A, self.c["c_mask"], [128, 128], BF16)
    sel, bsel = self.load_const(A, self.c["c_sel"], [65, 64], F32)
    R = self.resid_ctx(A)
    KH = A.alloc([128, 8, SQ], BF16)
    VA = A.alloc([128, NB, 8, 65], BF16)
    bKH, bVA = Buf("kh"), Buf("va")
    VST = [(A.alloc([128, 8, 512], BF16), Buf("vst")) for _ in range(1)]
    QHl = [(A.alloc([128, 8, TT], BF16), Buf("qhl")) for _ in range(2)]
    ATl = [(A.alloc([128, 4, TT], BF16), Buf("atl")) for _ in range(2)]
    PTl = [(A.alloc([128, TT], BF16), Buf("pt")) for _ in range(5)]
    OS = [(A.alloc([128, TT], F32), Buf("os")) for _ in range(2)]
    RD = [(A.alloc([128, TT], F32), Buf("rd")) for _ in range(2)]
    OT = [(A.alloc([128, 8, TT], BF16), Buf("ot")) for _ in range(2)]
    pcount = 0
    for s in range(ns):
        for h in range(8):
            self.dma("sp", KH[0:64, h, :], scr["KN"][s, (h % 2) * 64:(h % 2 + 1) * 64, h // 2, :], writes=(bKH,),
                     stream="kv", ring=4)
            self.dma("sp", KH[64:96, h, :], scr["KR"][s, 0:32, :], writes=(bKH,), stream="kv", ring=4)
        self.memset("pool", VA, 1.0, (bVA,))
        for g in range(NB // 8):
            vs, bvs = VST[0]
            self.dma("sp", vs, scr["V"][s, :, g * 8:(g + 1) * 8, :], writes=(bvs,), stream="kv", ring=4)
            self.cp("pool", VA[:, g * 8:(g + 1) * 8, :, 0:64], vs.rearrange("p b (h e) -> p b h e", e=64),
                    (bvs,), (bVA,))
        for ti in range(TPS):
            t = s * TPS + ti
            qh, bqh = QHl[t % 2]
            at, bat = ATl[t % 2]
            ot, bot = OT[t % 2]
            qn_s = scr["QN"][t].rearrange("p (a b) -> p a b", a=4)
            qr_s = scr["QR"][t].rearrange("p (a b) -> p a b", a=4)
            for h in range(8):
                self.dma("sp", qh[0:64, h, :], qn_s[(h % 2) * 64:(h % 2 + 1) * 64, h // 2, :], writes=(bqh,),
                         stream="ql", ring=6)
                self.dma("sp", qh[64:96, h, :], qr_s[(h % 2) * 32:(h % 2 + 1) * 32, h // 2, :], writes=(bqh,),
                         stream="ql", ring=6)
            self.dma("sp", at, scr["AT"][t].rearrange("p (a b) -> p a b", a=4), writes=(bat,), stream="ql", ring=6)
            nkb = 4 * ti + 4
            blocks = [(h, kb) for h in range(8) for kb in range(nkb)]
            state = {}

            def qk(i):
                nonlocal pcount
                h, kb = blocks[i]
                pr, hh = h // 2, h % 2
                n0 = max(0, kb - 4 * ti) * 128
                pb = (0, 1, 5)[pcount % 3]
                p, bp = PTl[pcount % 5]
                pcount += 1
                self.mm(PS[pb][:, n0:TT], KH[0:96, h, kb * 128:(kb + 1) * 128], qh[0:96, h, n0:TT], True, True,
                        (bKH, bqh), (bPS[pb],))
                self.act(p[:, n0:TT], PS[pb][:, n0:TT], AF.Exp, (bPS[pb],), (bp,))
                if kb >= 4 * ti:
                    self.tt("pool", p[:, n0:n0 + 128], p[:, n0:n0 + 128], mask, ALU.mult, (bp, bmask), (bp,))
                state[i] = (p, bp, n0)

            def pv(i):
                h, kb = blocks[i]
                p, bp, n0 = state.pop(i)
                po = 2 + h % 2
                self.mm(PS[po][0:65, n0:TT], VA[:, kb, h, :], p[:, n0:TT], kb == 0, kb == nkb - 1,
                        (bVA, bp), (bPS[po],))

            def epi1(h):
                po = 2 + h % 2
                os_, bos = OS[h % 2]
                self.cp("act", os_[0:65, :], PS[po][0:65, :], (bPS[po],), (bos,))

            def epi2(h):
                os_, bos = OS[h % 2]
                rd, brd = RD[h % 2]
                pd = 4
                self.mm(PS[pd][0:64, :], sel, os_[0:65, :], True, True, (bsel, bos), (bPS[pd],))
                self.recip(rd[0:64, :], PS[pd][0:64, :], (bPS[pd],), (brd,))
                self.tt("pool", ot[0:64, h, :], os_[0:64, :], rd[0:64, :], ALU.mult, (bos, brd), (bot,))

            qk(0)
            qk(1)
            pend = []
            for i in range(len(blocks)):
                if i + 2 < len(blocks):
                    qk(i + 2)
                pv(i)
                h, kb = blocks[i]
                for item in list(pend):
                    if item[1] <= i:
                        epi2(item[0])
                        pend.remove(item)
                if kb == nkb - 1:
                    epi1(h)
                    pend.append((h, i + 2))
            for item in pend:
                epi2(item[0])
            steps = [((lambda j, cc=cc, at=at: at[:, cc, j * 128:(j + 1) * 128]),
                      (lambda hf, cc=cc: WOC[:, cc, hf * 512:(hf + 1) * 512]), (bWOC, bat)) for cc in range(4)]
            steps += [((lambda j, h=h, ot=ot: ot[0:64, h, j * 128:(j + 1) * 128]),
                       (lambda hf, h=h: WOM[0:64, h, hf * 512:(hf + 1) * 512]), (bWOM, bot)) for h in range(8)]
            self.out_proj(R, t, (0, 1, 2, 3), steps, xin, xout, 1.0, banks=(6, 7))
    S.barrier()
    A.reset(m0)


Builder.even_phase = _even_phase


def _odd_phase(self, xin, xout, o, l):
    S, A = self.S_, self.arena
    PS, bPS = self.PS, self.bPS
    I = self.I
    ns, SQ, TPS = self.n_seq, self.S, self.TPS
    ntiles = self.NT // TT
    if not hasattr(self, "od_scr"):
        self.od_scr = dict(
            QT=self.scratch("s_rq", [ntiles, 128, 8 * TT], BF16),
            KT=self.scratch("s_rk", [ntiles, 128, 8 * TT], BF16),
            QX=self.scratch("s_rqx", [ntiles, 128, 8 * TT], BF16),
            V=self.scratch("s_rv", [ntiles, 128, 4 * 2048], BF16),
            SG=self.scratch("s_rsg", [ntiles, 128, 4 * 2048], BF16),
        )
    scr = self.od_scr
    w_in = I["od_w_in"][o]

    m0 = A.mark()
    stage = self.new_stage(A, 2)
    gmix, bgmix = self.load_col(A, I["mix_norm"][l], 8)
    gmk = A.alloc([128, 8], F32)
    bgmk = Buf("gmk")
    self.ts(gmk, gmix, 1.0 / 16.0, None, ALU.mult, None, (bgmix,), (bgmk,))
    WIN = A.alloc([128, 8, 6144], BF16)
    bWIN = Buf("win")
    for k in range(8):
        rows = w_in[k * 128:(k + 1) * 128, :]
        self.load_rows(stage, WIN[:, k, 0:1024], bWIN, rows[:, 0:1024], gmix[:, k:k + 1], bgmix)
        self.load_rows(stage, WIN[:, k, 1024:2048], bWIN, rows[:, 1024:2048], gmk[:, k:k + 1], bgmk)
        self.load_rows(stage, WIN[:, k, 2048:6144], bWIN, rows[:, 2048:6144], gmix[:, k:k + 1], bgmix)
    xi, bxi = self.load_const(A, self.c["c_xi"], [128, 4, 512], F32)
    C = self.norm_ctx(A, use_ln=False)
    self.temp_pool(A, 4, 1)
    CT = [(A.alloc([128, TT], F32), Buf("ct")) for _ in range(1)]
    ST = [(A.alloc([128, TT], F32), Buf("st")) for _ in range(1)]
    QTt = [(A.alloc([128, 8, TT], BF16), Buf("qt")) for _ in range(1)]
    KTt = [(A.alloc([128, 8, TT], BF16), Buf("kt")) for _ in range(1)]
    QXt = [(A.alloc([128, 8, TT], BF16), Buf("qx")) for _ in range(1)]
    Q32 = [(A.alloc([128, TT], F32), Buf("q32")) for _ in range(2)]
    VT = [(A.alloc([128, 2048], BF16), Buf("vt")) for _ in range(2)]
    SGT = [(A.alloc([128, 2048], BF16), Buf("sgt")) for _ in range(2)]

    def src(t):
        return lambda j: xin[t * TT + j * 128:t * TT + (j + 1) * 128, :]

    def r1_tile(t, slot):
        s, ti = t // TPS, t % TPS
        c0 = ti * TT
        ht, bht = C["HT"][slot % 2]
        ct, bct = CT[0]
        st, bst = ST[0]
        self.dma("sp", ct, self.CR[s, :, c0:c0 + TT], writes=(bct,), stream="tab", ring=4)
        self.dma("sp", st, self.SR[s, :, c0:c0 + TT], writes=(bst,), stream="tab", ring=4)
        qt, bqt = QTt[0]
        kt, bkt = KTt[0]
        qx, bqx = QXt[0]
        for which, dst, bdst in ((0, qt, bqt), (1, kt, bkt)):
            for h in range(4):
                col = which * 1024 + h * 256
                for dc in range(2):
                    for k in range(8):
                        self.mm(PS[dc], WIN[:, k, col + dc * 128:col + (dc + 1) * 128], ht[:, k, :], k == 0, k == 7,
                                (bWIN, bht), (bPS[dc],))
                T1, b1 = self.t32()
                T2, b2 = self.t32()
                T3, b3 = self.t32()
                T4, b4 = self.t32()
                self.tt("dve", T1, PS[0], ct, ALU.mult, (bPS[0], bct), (b1,))
                self.tt("dve", T2, PS[1], st, ALU.mult, (bPS[1], bst), (b2,))
                self.tt("dve", T3, PS[0], st, ALU.mult, (bPS[0], bst), (b3,))
                self.tt("dve", T4, PS[1], ct, ALU.mult, (bPS[1], bct), (b4,))
                if which == 0:
                    qa, bqa = Q32[0]
                    qb, bqb = Q32[1]
                    self.tt("pool", qa, T1, T2, ALU.subtract, (b1, b2), (bqa,))
                    self.tt("pool", qb, T3, T4, ALU.add, (b3, b4), (bqb,))
                    self.cp("act", dst[:, 2 * h, :], qa, (bqa,), (bdst,))
                    self.cp("act", dst[:, 2 * h + 1, :], qb, (bqb,), (bdst,))
                    self.tt("pool", qx[:, 2 * h, :], qa, xi[:, h, :], ALU.mult, (bqa, bxi), (bqx,))
                    self.tt("pool", qx[:, 2 * h + 1, :], qb, xi[:, h, :], ALU.mult, (bqb, bxi), (bqx,))
                else:
                    self.tt("pool", dst[:, 2 * h, :], T1, T2, ALU.subtract, (b1, b2), (bdst,))
                    self.tt("pool", dst[:, 2 * h + 1, :], T3, T4, ALU.add, (b3, b4), (bdst,))
        self.dma("sp", scr["QT"][t].rearrange("p (a b) -> p a b", a=8), qt, reads=(bqt,), stream="rst1", ring=4)
        self.dma("sp", scr["KT"][t].rearrange("p (a b) -> p a b", a=8), kt, reads=(bkt,), stream="rst1", ring=4)
        self.dma("sp", scr["QX"][t].rearrange("p (a b) -> p a b", a=8), qx, reads=(bqx,), stream="rst1", ring=4)
        for cj in range(4):
            vt, bvt = VT[cj % 2]
            sg, bsg = SGT[cj % 2]
            for h in range(4):
                b = 2 + h % 2
                for k in range(8):
                    self.mm(PS[b], ht[:, k, cj * 128:(cj + 1) * 128], WIN[:, k, 2048 + h * 512:2048 + (h + 1) * 512],
                            k == 0, k == 7, (bWIN, bht), (bPS[b],))
                self.cp("dve" if h % 2 else "act", vt[:, h * 512:(h + 1) * 512], PS[b], (bPS[b],), (bvt,))
            for h in range(4):
                b = 4 + h % 2
                for k in range(8):
                    self.mm(PS[b], ht[:, k, cj * 128:(cj + 1) * 128], WIN[:, k, 4096 + h * 512:4096 + (h + 1) * 512],
                            k == 0, k == 7, (bWIN, bht), (bPS[b],))
                self.act(sg[:, h * 512:(h + 1) * 512], PS[b], AF.Silu, (bPS[b],), (bsg,))
            self.dma("sp", scr["V"][t, :, cj * 2048:(cj + 1) * 2048], vt, reads=(bvt,), stream="rst2", ring=4)
            self.dma("sp", scr["SG"][t, :, cj * 2048:(cj + 1) * 2048], sg, reads=(bsg,), stream="rst2", ring=4)

    self.norm1(C, 0, src(0))
    self.norm2(C, 0)
    for t in range(ntiles):
        if t + 1 < ntiles:
            self.norm1(C, t + 1, src(t + 1))
        r1_tile(t, t)
        if t + 1 < ntiles:
            self.norm2(C, t + 1)
    S.barrier()
    A.reset(m0)

    m0 = A.mark()
    stage = self.new_stage(A, 2)
    WO = A.alloc([128, 16, D], BF16)
    bWO = Buf("wo")
    for c in range(16):
        self.load_rows(stage, WO[:, c, :], bWO, I["od_w_out"][o][c * 128:(c + 1) * 128, :])
    ident, bid = self.load_const(A, self.c["c_ident"], [128, 128], BF16)
    decay, bdec = self.load_const(A, self.c["c_decay"], [128, 4, 128], F32)
    zeta, bzeta = self.load_const(A, self.c["c_zeta"], [128, 4], F32)
    GNG = A.alloc([128, 2048], F32)
    GNB = A.alloc([128, 2048], F32)
    bGN = Buf("gn")
    self.dma("sp", GNG, I["od_gn_g"][o].rearrange("(o n) -> o n", o=1).partition_broadcast(128), writes=(bGN,),
             stream="misc", ring=4)
    self.dma("sp", GNB, I["od_gn_b"][o].rearrange("(o n) -> o n", o=1).partition_broadcast(128), writes=(bGN,),
             stream="misc", ring=4)
    R = self.resid_ctx(A)
    ST32 = A.alloc([128, 4, 2, 512], F32)
    STB = A.alloc([128, 4, 2, 512], BF16)
    bST = [Buf("st32_%d" % h) for h in range(4)]
    bSTB = [Buf("stb_%d" % h) for h in range(4)]
    QTl = [(A.alloc([128, 8, TT], BF16), Buf("qtl")) for _ in range(2)]
    KTl = [(A.alloc([128, 8, TT], BF16), Buf("ktl")) for _ in range(2)]
    QXl = [(A.alloc([128, 8, TT], BF16), Buf("qxl")) for _ in range(2)]
    Vl = [(A.alloc([128, 2048], BF16), Buf("vl")) for _ in range(2)]
    SGl = [(A.alloc([128, 2048], BF16), Buf("sgl")) for _ in range(2)]
    KZ = [(A.alloc([128, 256], BF16), Buf("kz")) for _ in range(2)]
    PTl = [(A.alloc([128, 128], BF16), Buf("ptl")) for _ in range(2)]
    O32 = [(A.alloc([128, 512], F32), Buf("o32")) for _ in range(3)]
    SQJ = (A.alloc([128, 512], BF16), Buf("sqj"))
    STAT = [(A.alloc([128, 8], F32), Buf("stat")) for _ in range(3)]
    OG = [(A.alloc([128, 2048], BF16), Buf("og")) for _ in range(2)]
    OGT = [(A.alloc([128, 16, TT], BF16), Buf("ogt")) for _ in range(2)]
    cnt = 0
    for s in range(ns):
        for h in range(4):
            self.memset("pool", ST32[:, h], 0.0, (bST[h],))
            self.memset("pool", STB[:, h], 0.0, (bSTB[h],))
        for ti in range(TPS):
            t = s * TPS + ti
            qt, bqt = QTl[t % 2]
            kt, bkt = KTl[t % 2]
            qx, bqx = QXl[t % 2]
            ogt, bogt = OGT[t % 2]
            self.dma("sp", qt, scr["QT"][t].rearrange("p (a b) -> p a b", a=8), writes=(bqt,), stream="rl", ring=4)
            self.dma("sp", kt, scr["KT"][t].rearrange("p (a b) -> p a b", a=8), writes=(bkt,), stream="rl", ring=4)
            self.dma("sp", qx, scr["QX"][t].rearrange("p (a b) -> p a b", a=8), writes=(bqx,), stream="rl", ring=4)
            items = [(cj, h) for cj in range(4) for h in range(4)]
            info = {}

            def stage_a(i):
                nonlocal cnt
                cj, h = items[i]
                cs = slice(cj * 128, (cj + 1) * 128)
                vl, bvl = Vl[cj % 2]
                sg, bsg = SGl[cj % 2]
                if h == 0:
                    self.dma("sp", vl, scr["V"][t, :, cj * 2048:(cj + 1) * 2048], writes=(bvl,), stream="rl2", ring=4)
                    self.dma("sp", sg, scr["SG"][t, :, cj * 2048:(cj + 1) * 2048], writes=(bsg,), stream="rl2", ring=4)
                cnt += 1
                k_ = cnt
                pz = 6 + k_ % 2
                ptz = PS[pz].bitcast(BF16)
                for dc in range(2):
                    self.S_.op("pe", lambda eng, ptz=ptz, kt=kt, h=h, dc=dc, cs=cs: eng.transpose(
                        out=ptz[:, dc * 128:(dc + 1) * 128], in_=kt[:, 2 * h + dc, cs], identity=ident),
                        reads=(bkt, bid), writes=(bPS[pz],))
                kz, bkz = KZ[k_ % 2]
                self.act(kz, ptz[:, 0:256], AF.Copy, (bPS[pz], bzeta), (bkz,), scale=zeta[:, h:h + 1])
                pb = 0 + k_ % 2
                for dc in range(2):
                    self.mm(PS[pb][:, 0:128], kt[:, 2 * h + dc, cs], qt[:, 2 * h + dc, cs], dc == 0, dc == 1,
                            (bkt, bqt), (bPS[pb],))
                p, bp = PTl[k_ % 2]
                self.tt("dve", p, PS[pb][:, 0:128], decay[:, h, :], ALU.mult, (bPS[pb], bdec), (bp,))
                info[i] = (k_, kz, bkz, p, bp)

            def stage_b(i):
                cj, h = items[i]
                cs = slice(cj * 128, (cj + 1) * 128)
                vl, bvl = Vl[cj % 2]
                sg, bsg = SGl[cj % 2]
                og, bog = OG[cj % 2]
                k_, kz, bkz, p, bp = info.pop(i)
                po = 2 + k_ % 2
                self.mm(PS[po], p, vl[:, h * 512:(h + 1) * 512], True, False, (bp, bvl), (bPS[po],))
                for dc in range(2):
                    self.mm(PS[po], qx[:, 2 * h + dc, cs], STB[:, h, dc, :], False, dc == 1, (bqx, bSTB[h]),
                            (bPS[po],))
                for dc in range(2):
                    pu = 4 + dc
                    self.mm(PS[pu], kz[:, dc * 128:(dc + 1) * 128], vl[:, h * 512:(h + 1) * 512], True, True,
                            (bkz, bvl), (bPS[pu],))
                    self.stt(ST32[:, h, dc, :], ST32[:, h, dc, :], float(self.gch[h]), PS[pu], ALU.mult, ALU.add,
                             (bPS[pu], bST[h]), (bST[h],))
                self.cp("pool", STB[:, h], ST32[:, h], (bST[h],), (bSTB[h],))
                o32, bo32 = O32[k_ % 3]
                stt_, bstat = STAT[k_ % 3]
                sq, bsq = SQJ
                self.act(o32, PS[po], AF.Copy, (bPS[po],), (bo32, bstat), accum_out=stt_[:, 0:1])
                self.act(sq, PS[po], AF.Square, (bPS[po],), (bsq, bstat), accum_out=stt_[:, 1:2])
                self.ts(stt_[:, 2:3], stt_[:, 0:1], 1.0 / 512, None, ALU.mult, None, (bstat,), (bstat,))
                self.tt("dve", stt_[:, 3:4], stt_[:, 2:3], stt_[:, 2:3], ALU.mult, (bstat,), (bstat,))
                self.stt(stt_[:, 4:5], stt_[:, 1:2], 1.0 / 512, stt_[:, 3:4], ALU.mult, ALU.subtract,
                         (bstat,), (bstat,))
                self.ts(stt_[:, 4:5], stt_[:, 4:5], EPS, None, ALU.add, None, (bstat,), (bstat,))
                self.act(stt_[:, 5:6], stt_[:, 4:5], AF.Sqrt, (bstat,), (bstat,))
                self.recip(stt_[:, 6:7], stt_[:, 5:6], (bstat,), (bstat,))
                self.ts(o32, o32, stt_[:, 2:3], stt_[:, 6:7], ALU.subtract, ALU.mult, (bo32, bstat), (bo32,))
                self.tt("pool", o32, o32, GNG[:, h * 512:(h + 1) * 512], ALU.mult, (bo32, bGN), (bo32,))
                self.tt("pool", o32, o32, GNB[:, h * 512:(h + 1) * 512], ALU.add, (bo32, bGN), (bo32,))
                self.tt("pool", og[:, h * 512:(h + 1) * 512], o32, sg[:, h * 512:(h + 1) * 512], ALU.mult,
                        (bo32, bsg), (bog,))

            def chunk_end(cj):
                cs = slice(cj * 128, (cj + 1) * 128)
                og, bog = OG[cj % 2]
                for g in range(2):
                    pz = 6 + g
                    ptz = PS[pz].bitcast(BF16).rearrange("p (c t) -> p c t", c=8)
                    for c in range(8):
                        self.S_.op("pe", lambda eng, ptz=ptz, og=og, c=c, g=g: eng.transpose(
                            out=ptz[:, c, :], in_=og[:, (g * 8 + c) * 128:(g * 8 + c + 1) * 128], identity=ident),
                            reads=(bog, bid), writes=(bPS[pz],))
                    self.cp("dve" if g else "act", ogt[:, g * 8:(g + 1) * 8, cs], ptz, (bPS[pz],), (bogt,))

            stage_a(0)
            pend = []
            for i in range(len(items)):
                if i + 1 < len(items):
                    stage_a(i + 1)
                stage_b(i)
                for it in list(pend):
                    if it[1] <= i:
                        chunk_end(it[0])
                        pend.remove(it)
                if items[i][1] == 3:
                    pend.append((items[i][0], i + 2))
            for it in pend:
                chunk_end(it[0])
            steps = [((lambda j, c=c, ogt=ogt: ogt[:, c, j * 128:(j + 1) * 128]),
                      (lambda hf, c=c: WO[:, c, hf * 512:(hf + 1) * 512]), (bWO, bogt)) for c in range(16)]
            self.out_proj(R, t, (0, 1, 2, 3), steps, xin, xout, 1.0, banks=(4, 5))
    S.barrier()
    A.reset(m0)


Builder.odd_phase = _odd_phase
```
